# Optimizing a Trainium2 kernel written in Bass

```python
import jax, jax.numpy as jnp
from jax import lax
import numpy as np

D_MODEL = 1024
BATCH = 16
SEQ = 4096
DEPTH = 1

HEAD_DIM = 64
N_FOX_HEADS = 8
N_DIL_HEADS = 8
FOX_WIDTH = N_FOX_HEADS * HEAD_DIM
DIL_WIDTH = N_DIL_HEADS * HEAD_DIM
IN_COLS = 3 * FOX_WIDTH + N_FOX_HEADS + 3 * DIL_WIDTH
Q_BLOCK = 128
DILATED_BRANCHES = ((128, 1), (512, 4), (2048, 16))
T5_NUM_BUCKETS = 32
T5_MAX_DISTANCE = 2048
N_EXPERTS = 256
TOP_K = 8
N_EXPERT_GROUPS = 8
TOP_K_GROUPS = 4
EXPERT_HIDDEN = 256
SHARED_HIDDEN = 256
ROUTED_SCALE = 2.5
DISPATCH_BLOCK = 128
N_MOD = 6
EPS = 1e-6

kernel_name = "hybrid_fox_dilated_moe_adaln"


def rmsnorm(x):
    xf = x.astype(jnp.float32)
    return (xf * lax.rsqrt(jnp.mean(xf * xf, axis=-1, keepdims=True) + EPS)).astype(x.dtype)


def _t5_bucket(dist):
    max_exact = T5_NUM_BUCKETS // 2
    d = np.maximum(dist, 1).astype(np.float32)
    large = max_exact + (np.log(d / max_exact) / np.log(T5_MAX_DISTANCE / max_exact)
                         * (T5_NUM_BUCKETS - max_exact)).astype(np.int32)
    large = np.minimum(large, T5_NUM_BUCKETS - 1)
    return np.where(dist < max_exact, dist, large).astype(np.int32)


def fox_attention(q, k, v, f_logit, b_forget):
    B, S, H, E = q.shape
    log_f = jax.nn.log_sigmoid(f_logit.astype(jnp.float32) + b_forget.astype(jnp.float32))
    F = jnp.cumsum(log_f, axis=1).transpose(0, 2, 1)
    scale = HEAD_DIM ** -0.5
    outs = []
    for i in range(S // Q_BLOCK):
        q0, q1 = i * Q_BLOCK, (i + 1) * Q_BLOCK
        s = jnp.einsum('bqhe,bkhe->bhqk', q[:, q0:q1], k[:, :q1]).astype(jnp.float32) * scale
        s = s + F[:, :, q0:q1, None] - F[:, :, None, :q1]
        causal = np.arange(q0, q1)[:, None] >= np.arange(q1)[None, :]
        p = jax.nn.softmax(jnp.where(causal, s, -jnp.inf), axis=-1)
        outs.append(jnp.einsum('bhqk,bkhe->bqhe', p.astype(v.dtype), v[:, :q1]))
    return jnp.concatenate(outs, axis=1)


def dilated_branch(q, k, v, rel_bias, window, dilation):
    B, S, H, E = q.shape
    blk = window // dilation
    span = blk * dilation
    L = -(-S // span) * span
    nb = L // span

    def to_blocks(t):
        t = jnp.pad(t, ((0, 0), (0, L - S), (0, 0), (0, 0)))
        t = t.reshape(B, L // dilation, dilation, H, E).transpose(0, 2, 1, 3, 4)
        return t.reshape(B, dilation, nb, blk, H, E)

    def with_prev(t):
        prev = jnp.pad(t[:, :, :-1], ((0, 0), (0, 0), (1, 0), (0, 0), (0, 0), (0, 0)))
        return jnp.concatenate([prev, t], axis=3)

    qb = to_blocks(q)
    kk = with_prev(to_blocks(k))
    vv = with_prev(to_blocks(v))
    rel = np.arange(blk)[:, None] + blk - np.arange(2 * blk)[None, :]
    band = (rel >= 0) & (rel <= blk)
    key_ok = (np.arange(nb)[:, None] * blk + np.arange(2 * blk)[None, :] - blk) >= 0
    mask = (band[None] & key_ok[:, None, :])[None, None, :, None]
    bucket = _t5_bucket(np.clip(rel, 0, blk) * dilation)
    bias = rel_bias[bucket].astype(jnp.float32).transpose(2, 0, 1)
    s = jnp.einsum('brnqhe,brnkhe->brnhqk', qb, kk).astype(jnp.float32) * (HEAD_DIM ** -0.5) + bias
    s = jnp.where(mask, s, -jnp.inf)
    lse = jax.nn.logsumexp(s, axis=-1)
    p = jnp.exp(s - lse[..., None])
    o = jnp.einsum('brnhqk,brnkhe->brnqhe', p.astype(vv.dtype), vv)
    o = o.reshape(B, dilation, L // dilation, H, E).transpose(0, 2, 1, 3, 4).reshape(B, L, H, E)[:, :S]
    lse = lse.transpose(0, 1, 2, 4, 3).reshape(B, dilation, L // dilation, H)
    lse = lse.transpose(0, 2, 1, 3).reshape(B, L, H)[:, :S]
    return o, lse


def dilated_mixture(q, k, v, rel_bias):
    outs, lses = [], []
    for window, dilation in DILATED_BRANCHES:
        o, lse = dilated_branch(q, k, v, rel_bias, window, dilation)
        outs.append(o)
        lses.append(lse)
    w = jax.nn.softmax(jnp.stack(lses, axis=-1), axis=-1)
    o = jnp.stack(outs, axis=-1)
    return jnp.sum(o * w[:, :, :, None, :].astype(o.dtype), axis=-1)


def hybrid_mixer(h, w_in, b_forget, g_fox_out, g_dil_out, w_out, rel_bias):
    B, S, _ = h.shape
    proj = h @ w_in
    o1, o2, o3, o4 = FOX_WIDTH, 2 * FOX_WIDTH, 3 * FOX_WIDTH, 3 * FOX_WIDTH + N_FOX_HEADS
    heads = lambda t, n: t.reshape(B, S, n, HEAD_DIM)
    q_f = heads(proj[..., :o1], N_FOX_HEADS)
    k_f = heads(proj[..., o1:o2], N_FOX_HEADS)
    v_f = heads(proj[..., o2:o3], N_FOX_HEADS)
    f_logit = proj[..., o3:o4]
    q_d = heads(proj[..., o4:o4 + DIL_WIDTH], N_DIL_HEADS)
    k_d = heads(proj[..., o4 + DIL_WIDTH:o4 + 2 * DIL_WIDTH], N_DIL_HEADS)
    v_d = heads(proj[..., o4 + 2 * DIL_WIDTH:], N_DIL_HEADS)
    y_fox = fox_attention(q_f, k_f, v_f, f_logit, b_forget).reshape(B, S, FOX_WIDTH)
    y_dil = dilated_mixture(q_d, k_d, v_d, rel_bias).reshape(B, S, DIL_WIDTH)
    merged = jnp.concatenate([rmsnorm(y_fox) * g_fox_out, rmsnorm(y_dil) * g_dil_out], axis=-1)
    return merged @ w_out


def swiglu(x, w_gate, w_up, w_down):
    return (jax.nn.silu(x @ w_gate) * (x @ w_up)) @ w_down


def moe(h, w_router, router_bias, w_exp_gate, w_exp_up, w_exp_down, w_sh_gate, w_sh_up, w_sh_down):
    B, S, D = h.shape
    N = B * S
    hf = h.reshape(N, D)
    scores = jax.nn.sigmoid((hf @ w_router).astype(jnp.float32))
    sel = scores + router_bias.astype(jnp.float32)
    grp = lax.top_k(sel.reshape(N, N_EXPERT_GROUPS, N_EXPERTS // N_EXPERT_GROUPS), 2)[0].sum(-1)
    _, gidx = lax.top_k(grp, TOP_K_GROUPS)
    gmask = jax.nn.one_hot(gidx, N_EXPERT_GROUPS, dtype=jnp.float32).sum(1) > 0
    emask = jnp.repeat(gmask, N_EXPERTS // N_EXPERT_GROUPS, axis=1)
    _, eidx = lax.top_k(jnp.where(emask, sel, -jnp.inf), TOP_K)
    wts = jnp.take_along_axis(scores, eidx, axis=1)
    wts = wts / jnp.sum(wts, axis=-1, keepdims=True) * ROUTED_SCALE

    NK = N * TOP_K
    flat_e = eidx.reshape(NK).astype(jnp.int32)
    flat_tok = (jnp.arange(NK, dtype=jnp.int32) // TOP_K)
    flat_w = wts.reshape(NK)
    order = jnp.argsort(flat_e)
    se, stok, sw = flat_e[order], flat_tok[order], flat_w[order]
    counts = jnp.bincount(flat_e, length=N_EXPERTS).astype(jnp.int32)
    padded = (counts + DISPATCH_BLOCK - 1) // DISPATCH_BLOCK * DISPATCH_BLOCK
    start = jnp.cumsum(counts) - counts
    pend = jnp.cumsum(padded)
    pstart = pend - padded
    dest = pstart[se] + (jnp.arange(NK, dtype=jnp.int32) - start[se])
    R = NK + N_EXPERTS * DISPATCH_BLOCK
    nblk = R // DISPATCH_BLOCK
    row_tok = jnp.full((R,), N, dtype=jnp.int32).at[dest].set(stok)
    row_w = jnp.zeros((R,), jnp.float32).at[dest].set(sw)
    block_e = jnp.minimum(jnp.searchsorted(pend, jnp.arange(nblk, dtype=jnp.int32) * DISPATCH_BLOCK,
                                           side='right'), N_EXPERTS - 1).astype(jnp.int32)
    h_pad = jnp.concatenate([hf, jnp.zeros((1, D), hf.dtype)], axis=0)

    def step(acc, blk):
        tok, w, e = blk
        xb = h_pad[tok]
        y = swiglu(xb, w_exp_gate[e], w_exp_up[e], w_exp_down[e])
        return acc.at[tok].add((y * w[:, None]).astype(acc.dtype)), None

    acc, _ = lax.scan(step, jnp.zeros((N + 1, D), hf.dtype),
                      (row_tok.reshape(nblk, DISPATCH_BLOCK), row_w.reshape(nblk, DISPATCH_BLOCK), block_e))
    routed = acc[:N]
    shared = swiglu(hf, w_sh_gate, w_sh_up, w_sh_down)
    return (routed + shared).reshape(B, S, D)


def setup_inputs(seed: int = 0) -> dict:
    key = jax.random.key(seed)
    ks = jax.random.split(key, 20)
    D, E, Hd = D_MODEL, N_EXPERTS, EXPERT_HIDDEN
    nrm = lambda k, shape, fan: jax.random.normal(k, shape, jnp.float32) * (fan ** -0.5)
    b_forget = (jnp.linspace(1.0, 6.0, N_FOX_HEADS, dtype=jnp.float32)[None, :]
                + 0.1 * jax.random.normal(ks[3], (DEPTH, N_FOX_HEADS), jnp.float32))
    return {
        "x": jax.random.normal(ks[0], (BATCH, SEQ, D), jnp.float32),
        "c": jax.random.normal(ks[1], (BATCH, D), jnp.float32),
        "w_in": nrm(ks[2], (DEPTH, D, IN_COLS), D),
        "b_forget": b_forget,
        "g_fox_out": 1.0 + 0.02 * jax.random.normal(ks[4], (DEPTH, FOX_WIDTH), jnp.float32),
        "g_dil_out": 1.0 + 0.02 * jax.random.normal(ks[5], (DEPTH, DIL_WIDTH), jnp.float32),
        "w_out": nrm(ks[6], (DEPTH, FOX_WIDTH + DIL_WIDTH, D), FOX_WIDTH + DIL_WIDTH),
        "w_ada": 0.5 * nrm(ks[7], (DEPTH, D, N_MOD * D), D),
        "b_ada": 0.02 * jax.random.normal(ks[8], (DEPTH, N_MOD * D), jnp.float32),
        "w_router": nrm(ks[9], (DEPTH, D, E), D),
        "router_bias": 0.01 * jax.random.normal(ks[10], (DEPTH, E), jnp.float32),
        "w_exp_gate": nrm(ks[11], (DEPTH, E, D, Hd), D),
        "w_exp_up": nrm(ks[12], (DEPTH, E, D, Hd), D),
        "w_exp_down": nrm(ks[13], (DEPTH, E, Hd, D), Hd),
        "w_sh_gate": nrm(ks[14], (DEPTH, D, SHARED_HIDDEN), D),
        "w_sh_up": nrm(ks[15], (DEPTH, D, SHARED_HIDDEN), D),
        "w_sh_down": nrm(ks[16], (DEPTH, SHARED_HIDDEN, D), SHARED_HIDDEN),
        "rel_bias": 0.5 * jax.random.normal(ks[17], (T5_NUM_BUCKETS, N_DIL_HEADS), jnp.float32),
        "g_final": 1.0 + 0.02 * jax.random.normal(ks[18], (D,), jnp.float32),
    }


def reference(x, c, w_in, b_forget, g_fox_out, g_dil_out, w_out, w_ada, b_ada, w_router, router_bias,
              w_exp_gate, w_exp_up, w_exp_down, w_sh_gate, w_sh_up, w_sh_down, rel_bias, g_final):
    c_act = jax.nn.silu(c)
    for l in range(DEPTH):
        mod = c_act @ w_ada[l] + b_ada[l]
        shift1, scale1, gate1, shift2, scale2, gate2 = [m[:, None, :] for m in jnp.split(mod, N_MOD, axis=-1)]
        h = rmsnorm(x) * (1.0 + scale1) + shift1
        x = x + gate1 * hybrid_mixer(h, w_in[l], b_forget[l], g_fox_out[l], g_dil_out[l], w_out[l], rel_bias)
        h = rmsnorm(x) * (1.0 + scale2) + shift2
        x = x + gate2 * moe(h, w_router[l], router_bias[l], w_exp_gate[l], w_exp_up[l], w_exp_down[l],
                            w_sh_gate[l], w_sh_up[l], w_sh_down[l])
    return rmsnorm(x) * g_final
```

```python
import numpy as np
import ml_dtypes
from contextlib import ExitStack
import concourse.bass as bass
import concourse.mybir as mybir
from concourse.bass_utils import run_bass_kernel_spmd

F32 = mybir.dt.float32
BF16 = mybir.dt.bfloat16
I32 = mybir.dt.int32
U32 = mybir.dt.uint32
AF = mybir.ActivationFunctionType
ALU = mybir.AluOpType

S = 4096
D = 1024
NSEQ = 2
NT = S // 128
NG = S // 512
NTOK = NSEQ * S
NTILE = NTOK // 128
INC = 3080
NE = 256
TOPK = 8
BLK = 256
NBLK = NTOK * TOPK // BLK + NE
RROWS = NBLK * BLK
EPS = 1e-6
NEG = -30000.0
DIL = ((128, 1), (512, 4), (2048, 16))
LIMIT = 30000


class Buf:
    def __init__(self, name, dram=False):
        self.name = name
        self.w = {}
        self.r = {}
        self.sem = None
        self.cnt = 0
        self.dram = dram


class TT:
    def __init__(self, t, name, dram=False):
        self.t = t
        self.buf = Buf(name, dram)

    def __getitem__(self, k):
        return self.t[k]


class KB:
    def __init__(self, nc, es):
        self.nc = nc
        self.es = es
        self.engs = {'pe': nc.tensor, 'dve': nc.vector, 'act': nc.scalar, 'pool': nc.gpsimd, 'sp': nc.sync}
        self.esem = {}
        self.ecnt = {}
        self.known = {e: {} for e in self.engs}
        self.nsem = 0
        self.nins = {e: 0 for e in self.engs}
        self.cur_es = es
        self.dmabufs = []
        self.free_sems = []
        self.phase_bufs = [[]]
        for e in self.engs:
            self._epoch(e)

    def phase(self):
        kb = self

        class _Ph:
            def __enter__(self_):
                self_.es = ExitStack()
                self_.prev = kb.cur_es
                kb.cur_es = self_.es
                kb.phase_bufs.append([])
                return self_

            def __exit__(self_, *a):
                kb.barrier()
                for b in kb.phase_bufs.pop():
                    if b.sem is not None:
                        kb.free_sems.append((b.sem, b.cnt))
                        if b in kb.dmabufs:
                            kb.dmabufs.remove(b)
                        b.sem = None
                kb.cur_es = self_.prev
                self_.es.close()
                return False
        return _Ph()

    def barrier(self):
        evs = {}
        for e in self.engs:
            if self.ecnt[e] > 0:
                evs[self.esem[e]] = self.ecnt[e]
        for b in self.dmabufs:
            if b.sem is not None and b.cnt > 0 and b.name != "precast":
                evs[b.sem] = b.cnt
        for e in self.engs:
            kn = self.known[e]
            for sem, val in evs.items():
                if sem == self.esem[e]:
                    continue
                if kn.get(sem, 0) < val:
                    self.engs[e].wait_ge(sem, val)
                    kn[sem] = val

    def newsem(self, name):
        self.nsem += 1
        return self.es.enter_context(self.nc.semaphore(f"{name}_{self.nsem}"))

    def _epoch(self, e):
        self.esem[e] = self.newsem("e" + e)
        self.ecnt[e] = 0

    def sb(self, name, shape, dt):
        t = TT(self.cur_es.enter_context(self.nc.sbuf_tensor(name, list(shape), dt)), name)
        self.phase_bufs[-1].append(t.buf)
        return t

    def ps(self, name, shape, dt):
        return TT(self.es.enter_context(self.nc.psum_tensor(name, list(shape), dt)), name)

    def dram(self, name, shape, dt, kind="Internal"):
        return TT(self.nc.dram_tensor(name, list(shape), dt, kind=kind), name, dram=True)

    def _wait(self, e, reads, writes):
        deps = {}

        def add(sem, val, src, raw):
            if src == e:
                if e == 'pe' or e == 'sp':
                    return
                if not raw:
                    return
            k = sem
            if deps.get(k, 0) < val:
                deps[k] = val
        for b in reads:
            b = b.buf if isinstance(b, TT) else b
            for sem, (val, src) in b.w.items():
                add(sem, val, src, True)
        for b in writes:
            b = b.buf if isinstance(b, TT) else b
            for sem, (val, src) in b.w.items():
                add(sem, val, src, False)
            for sem, (val, src) in b.r.items():
                add(sem, val, src, False)
        kn = self.known[e]
        for sem, val in deps.items():
            if kn.get(sem, 0) < val:
                self.engs[e].wait_ge(sem, val)
                kn[sem] = val

    def _post(self, ev, reads, writes):
        sem, val, src = ev
        for b in writes:
            b = b.buf if isinstance(b, TT) else b
            if b.dram:
                b.w[sem] = (val, src)
            else:
                b.w = {sem: (val, src)}
                b.r = {}
        for b in reads:
            b = b.buf if isinstance(b, TT) else b
            b.r[sem] = (val, src)

    def op(self, e, fn, reads=(), writes=()):
        self._wait(e, reads, writes)
        if self.ecnt[e] >= LIMIT:
            self._epoch(e)
        ins = fn(self.engs[e])
        self.ecnt[e] += 1
        self.nins[e] += 1
        ins.then_inc(self.esem[e], 1)
        self._post((self.esem[e], self.ecnt[e], e), reads, writes)
        return ins

    def dma(self, q, out, in_, reads=(), writes=(), sembuf=None, fn=None, **kw):
        self._wait(q, reads, writes)
        sb_ = sembuf.buf if isinstance(sembuf, TT) else sembuf
        if sb_.sem is None or sb_.cnt >= LIMIT:
            if sb_.sem is None and self.free_sems and self.free_sems[-1][1] < LIMIT // 2:
                sb_.sem, sb_.cnt = self.free_sems.pop()
            else:
                sb_.sem = self.newsem("d" + sb_.name[:8])
                sb_.cnt = 0
            if sb_ not in self.dmabufs:
                self.dmabufs.append(sb_)
        if fn is None:
            ins = self.engs[q].dma_start(out=out, in_=in_, **kw)
        else:
            ins = fn(self.engs[q])
        sb_.cnt += 16
        self.nins[q] += 1
        ins.then_inc(sb_.sem, 16)
        self._post((sb_.sem, sb_.cnt, 'dma'), reads, writes)
        return ins

    def wait_all(self, e, bufs):
        self._wait(e, bufs, ())


def bcast_rows(ap_row, nparts=128):
    n = ap_row.shape[-1]
    return bass.AP(ap_row.tensor, ap_row.offset, [[0, nparts], [1, n]])


def build(debug=False):
    nc = bass.Bass("TRN2", target_bir_lowering=False)
    es = ExitStack()
    kb = KB(nc, es)
    ein = lambda name, shape, dt=F32: TT(nc.dram_tensor(name, list(shape), dt, kind="ExternalInput"), name, dram=True)
    okind = "ExternalOutput" if debug else "Internal"

    def record(f, *a):
        rec = []
        orig_op, orig_dma = kb.op, kb.dma
        kb.op = lambda *args, **kw: rec.append((orig_op, args, kw))
        kb.dma = lambda *args, **kw: rec.append((orig_dma, args, kw))
        try:
            f(*a)
        finally:
            kb.op, kb.dma = orig_op, orig_dma
        return rec

    def emit_interleaved(A, B):
        ia = ib = 0
        while ia < len(A) or ib < len(B):
            if ib >= len(B) or (ia < len(A) and ia * len(B) <= ib * len(A)):
                fn, args, kw = A[ia]
                ia += 1
            else:
                fn, args, kw = B[ib]
                ib += 1
            fn(*args, **kw)

    def emit_merged(streams):
        streams = [st for st in streams if st]
        pos = [0] * len(streams)
        total = sum(len(st) for st in streams)
        for _ in range(total):
            best = min((i for i in range(len(streams)) if pos[i] < len(streams[i])),
                       key=lambda i: pos[i] / len(streams[i]))
            fn, args, kw = streams[best][pos[best]]
            pos[best] += 1
            fn(*args, **kw)


    x_in = ein("x", [NSEQ, S, D])
    cT_in = ein("cT", [128, 8, NSEQ])
    w_in_d = ein("w_in", [D, INC])
    bfg_in = ein("b_forget", [8, 1])
    gmix_in = ein("g_mix", [128, 8])
    w_out_d = ein("w_out", [D, D])
    w_ada_d = ein("w_ada", [D, 6 * D])
    b_ada_d = ein("b_ada", [1, 6 * D])
    b_adaT_d = ein("b_adaT", [128, 48])
    ident_d = ein("ident_in", [128, 128], BF16)
    relb_d = ein("rel_bias", [32, 8])
    oh_d = ein("oh_in", [32, 3, 384])
    negm_d = ein("negm_in", [8, 3, 384])
    J_d = ein("J_in", [128, 128])
    tri_d = ein("tri_in", [128, 128], BF16)
    w_r_d = ein("w_router", [D, NE])
    rbias_d = ein("router_bias", [1, NE])
    wsg_d = ein("w_sh_gate", [D, 256])
    wsu_d = ein("w_sh_up", [D, 256])
    wsd_d = ein("w_sh_down", [256, D])
    wA_d = ein("w_exp_a", [NE * 128, 2048])
    wB_d = ein("w_exp_b", [NE * 128, 2048])
    wD_d = ein("w_exp_d", [NE * 128, 2048])
    gfin_d = ein("g_final", [1, D])
    lst_d = ein("lst_in", [128, 128], BF16)
    iota_d = ein("iota_in", [128, NE])
    bvals_d = ein("bvals_in", [128, 6])

    out_d = TT(nc.dram_tensor("out", [NSEQ, S, D], F32, kind="ExternalOutput"), "out", dram=True)

    QK = [kb.dram(f"QK{s}", [16, 128, S], BF16, kind=okind) for s in range(NSEQ)]
    VV = [kb.dram(f"VV{s}", [S, 1024], BF16, kind=okind) for s in range(NSEQ)]
    GP = [kb.dram(f"GP{s}", [8, 3, S], BF16, kind=okind) for s in range(NSEQ)]
    EXT = kb.dram("EXT", [8, 3, 384], F32)
    YF = [kb.dram(f"YF{s}", [S, 512], F32, kind=okind) for s in range(NSEQ)]
    OD = [kb.dram(f"OD{s}", [3, S, 8, 65], F32, kind=okind) for s in range(NSEQ)]
    H2 = kb.dram("H2", [NTOK, D], BF16)
    X2 = kb.dram("X2", [NTOK, D], F32, kind=okind)
    HS = kb.dram("HS", [RROWS, D], BF16)
    WGUb = kb.dram("WGUb", [NE * 128, 4096], BF16)
    WDb = kb.dram("WDb", [NE * 128, 2048], BF16)
    YS = kb.dram("YS", [RROWS, D], BF16)

    ident = kb.sb("ident", [128, 128], BF16)
    kb.dma('sp', ident[:], ident_d[:], writes=[ident], sembuf=ident)
    epsc = kb.sb("epsc", [128, 1], F32)
    kb.op('dve', lambda e: e.memset(epsc[:], EPS), writes=[epsc])
    cact = kb.sb("cact", [128, 8, NSEQ], F32)
    kb.dma('sp', cact[:], cT_in[:], writes=[cact], sembuf=cact)
    kb.op('act', lambda e: e.activation(out=cact[:], in_=cact[:], func=AF.Silu), reads=[cact], writes=[cact])
    badT = kb.sb("badT", [128, 48], F32)
    kb.dma('sp', badT[:], b_adaT_d[:], writes=[badT], sembuf=badT)
    bfg = kb.sb("bfg", [8, 1], F32)
    kb.dma('sp', bfg[:], bfg_in[:], writes=[bfg], sembuf=bfg)
    kb.op('dve', lambda e: e.tensor_scalar(out=bfg[:], in0=bfg[:], scalar1=-1.0, scalar2=None, op0=ALU.mult),
          reads=[bfg], writes=[bfg])

    psum = [kb.ps(f"ps{i}", [128, 512], F32) for i in range(8)]

    precast_buf = Buf("precast")
    PCH = 256
    precast_jobs = [(dst, c0, src, r0) for r0 in range(0, NE * 128, PCH)
                    for dst, c0, src in ((WGUb, 0, wA_d), (WGUb, 2048, wB_d), (WDb, 0, wD_d))]

    def precast(n):
        for _ in range(n):
            if precast_jobs:
                dst, c0, src, r0 = precast_jobs.pop(0)
                kb.dma('pool', dst[r0:r0 + PCH, c0:c0 + 2048], src[r0:r0 + PCH, :], writes=[dst],
                       sembuf=precast_buf)

    modT = {m: kb.sb(f"modT{m}", [128, 8, NSEQ], F32) for m in (0, 1)}
    gate_b = {(m, s): kb.sb(f"gate{m}_{s}", [128, 1024], F32) for m in (2, 3, 4, 5) for s in range(NSEQ)}
    ph0 = kb.phase()
    ph0.__enter__()
    wa = [kb.sb(f"wa{i}", [128, 8, 1024], F32) for i in range(2)]
    cbc = kb.sb("cbc", [128, NSEQ, 8, 128], F32)
    for s in range(NSEQ):
        for kc in range(8):
            kb.op('dve', lambda e, s=s, kc=kc: e.tensor_copy(out=cbc[:, s, kc, :],
                                                              in_=cact[:, kc, s:s + 1].to_broadcast([128, 128])),
                  reads=[cact], writes=[cbc])
    wi = 0
    for m in range(6):
        wt = wa[wi % 2]
        wi += 1
        kb.dma('sp', wt[:], w_ada_d[:, m * 1024:(m + 1) * 1024].rearrange("(kc p) n -> p kc n", p=128),
               writes=[wt], sembuf=wt)
        if m in (0, 1):
            pt = psum[wi % 2]
            for ncn in range(8):
                for kc in range(8):
                    kb.op('pe', lambda e, ncn=ncn, kc=kc, wt=wt, pt=pt: e.matmul(
                        pt[:, ncn * 2:(ncn + 1) * 2], lhsT=wt[:, kc, ncn * 128:(ncn + 1) * 128],
                        rhs=cact[:, kc, :], start=(kc == 0), stop=(kc == 7)),
                        reads=[wt, cact], writes=[pt])
            mt = modT[m]
            kb.op('dve', lambda e, pt=pt, mt=mt, m=m: e.tensor_tensor(
                out=mt[:], in0=pt[:, 0:16].rearrange("p (a b) -> p a b", b=NSEQ),
                in1=badT[:, m * 8:(m + 1) * 8].unsqueeze(2).to_broadcast([128, 8, NSEQ]), op=ALU.add),
                reads=[pt, badT], writes=[mt])
            if m == 1:
                kb.op('dve', lambda e, mt=mt: e.tensor_scalar(out=mt[:], in0=mt[:], scalar1=1.0, scalar2=None,
                                                             op0=ALU.add), reads=[mt], writes=[mt])
        else:
            brow = kb.sb(f"brow{m}", [128, 1024], F32)
            kb.dma('sp', brow[:], bcast_rows(b_ada_d[0:1, m * 1024:(m + 1) * 1024]), writes=[brow], sembuf=brow)
            for s in range(NSEQ):
                gb = gate_b[(m, s)]
                for half in range(2):
                    pt = psum[2 + half]
                    for kc in range(8):
                        kb.op('pe', lambda e, kc=kc, wt=wt, pt=pt, s=s, half=half: e.matmul(
                            pt[:, :], lhsT=cbc[:, s, kc, :], rhs=wt[:, kc, half * 512:(half + 1) * 512],
                            start=(kc == 0), stop=(kc == 7)), reads=[wt, cbc], writes=[pt])
                    kb.op('dve', lambda e, pt=pt, gb=gb, half=half, brow=brow: e.tensor_tensor(
                        out=gb[:, half * 512:(half + 1) * 512], in0=pt[:, :],
                        in1=brow[:, half * 512:(half + 1) * 512], op=ALU.add),
                        reads=[pt, brow], writes=[gb])
                if m == 4:
                    kb.op('dve', lambda e, gb=gb: e.tensor_scalar(out=gb[:], in0=gb[:], scalar1=1.0, scalar2=None,
                                                                 op0=ALU.add), reads=[gb], writes=[gb])

    ph0.__exit__(None, None, None)

    phA = kb.phase()
    phA.__enter__()
    w_in_bf = kb.sb("w_in_bf", [128, 8, INC], BF16)
    for kc in range(8):
        for hf in range(2):
            kb.dma('pool', w_in_bf[:, kc, hf * 1540:(hf + 1) * 1540],
                   w_in_d[kc * 128:(kc + 1) * 128, hf * 1540:(hf + 1) * 1540],
                   writes=[w_in_bf], sembuf=w_in_bf)
    xp = [kb.sb(f"xp{i}", [128, D], F32) for i in range(3)]
    junk = kb.sb("junk", [128, D], BF16)
    xn = [kb.sb(f"xn{i}", [128, D], BF16) for i in range(2)]
    ssq = [kb.sb(f"ssq{i}", [128, 1], F32) for i in range(2)]
    std = [kb.sb(f"std{i}", [128, 1], F32) for i in range(2)]
    rstd = [kb.sb(f"rstd{i}", [128, 1], F32) for i in range(2)]
    hT = [kb.sb(f"hT{i}", [128, 8, 512], BF16) for i in range(2)]
    qkst = [kb.sb(f"qkst{i}", [128, 16, 512], BF16) for i in range(2)]
    vst = [kb.sb(f"vst{i}", [128, 4, 1024], BF16) for i in range(2)]
    ef = [kb.sb(f"ef{i}", [8, 512], F32) for i in range(2)]
    lf = [kb.sb(f"lf{i}", [8, 512], F32) for i in range(2)]
    Gt = [kb.sb(f"Gt{i}", [8, 512], F32) for i in range(2)]
    r1 = kb.sb("r1", [8, 512], F32)
    r2 = kb.sb("r2", [8, 512], F32)
    gpt = [kb.sb(f"gpt{i}", [8, 3, 512], BF16) for i in range(2)]
    ones8 = kb.sb("ones8", [8, 512], F32)
    kb.op('dve', lambda e: e.memset(ones8[:], 1.0), writes=[ones8])
    carry = kb.sb("carry", [8, 1], F32)

    qk_cols = [0 + 128 * i for i in range(4)] + [512 + 128 * i for i in range(4)] + \
              [1544 + 128 * i for i in range(4)] + [2056 + 128 * i for i in range(4)]
    qk_isq = [True] * 4 + [False] * 4 + [True] * 4 + [False] * 4
    st = {"tcount": 0, "evac": 0}

    def A1(gi):
        s, g = gi // NG, gi % NG
        gcount = gi
        tcount = gi * 4
        hTg = hT[gcount % 2]
        for j in range(4):
            t = g * 4 + j
            xt = xp[tcount % 3]
            i2 = tcount % 2
            kb.dma('sp', xt[:], x_in[s, t * 128:(t + 1) * 128, :], writes=[xt], sembuf=xt)
            kb.op('act', lambda e, xt=xt, i2=i2: e.activation(out=junk[:], in_=xt[:], func=AF.Square,
                                                               accum_out=ssq[i2][:]),
                  reads=[xt], writes=[junk, ssq[i2]])
            kb.op('act', lambda e, i2=i2: e.activation(out=std[i2][:], in_=ssq[i2][:], func=AF.Sqrt,
                                                       scale=1.0 / D, bias=epsc[:]),
                  reads=[ssq[i2], epsc], writes=[std[i2]])
            kb.op('dve', lambda e, i2=i2: e.reciprocal(out=rstd[i2][:], in_=std[i2][:]),
                  reads=[std[i2]], writes=[rstd[i2]])
            kb.op('act', lambda e, xt=xt, i2=i2: e.activation(out=xn[i2][:], in_=xt[:], func=AF.Identity,
                                                               scale=rstd[i2][:]),
                  reads=[xt, rstd[i2]], writes=[xn[i2]])
            tp = psum[tcount % 2]
            tpv = tp[:].bitcast(BF16)
            for kc in range(8):
                kb.op('pe', lambda e, kc=kc, i2=i2, tpv=tpv: e.transpose(
                    out=tpv[:, kc * 128:(kc + 1) * 128], in_=xn[i2][:, kc * 128:(kc + 1) * 128],
                    identity=ident[:]), reads=[xn[i2], ident], writes=[tp])
            for kc in range(8):
                kb.op('dve', lambda e, kc=kc, tpv=tpv, j=j, hTg=hTg, s=s: e.tensor_scalar(
                    out=hTg[:, kc, j * 128:(j + 1) * 128], in0=tpv[:, kc * 128:(kc + 1) * 128],
                    scalar1=modT[1][:, kc, s:s + 1], scalar2=modT[0][:, kc, s:s + 1],
                    op0=ALU.mult, op1=ALU.add), reads=[tp, modT[1], modT[0]], writes=[hTg])
            tcount += 1

    def A2(gi):
        nonlocal evac
        s, g = gi // NG, gi % NG
        gcount = gi
        hTg = hT[gcount % 2]
        if g == 0:
            kb.op('dve', lambda e: e.memset(carry[:], 0.0), writes=[carry])
        i2 = gcount % 2
        pf = psum[2]
        for kc in range(8):
            kb.op('pe', lambda e, kc=kc, pf=pf, hTg=hTg: e.matmul(
                pf[0:8, :], lhsT=w_in_bf[:, kc, 1536:1544], rhs=hTg[:, kc, :],
                start=(kc == 0), stop=(kc == 7)), reads=[w_in_bf, hTg], writes=[pf])
        kb.op('act', lambda e, pf=pf, i2=i2: e.activation(out=ef[i2][:], in_=pf[0:8, :], func=AF.Exp,
                                                           scale=-1.0, bias=bfg[:]),
              reads=[pf, bfg], writes=[ef[i2]])
        kb.op('act', lambda e, i2=i2: e.activation(out=lf[i2][:], in_=ef[i2][:], func=AF.Ln, bias=1.0),
              reads=[ef[i2]], writes=[lf[i2]])
        kb.op('dve', lambda e, i2=i2: e.tensor_tensor_scan(out=Gt[i2][:], data0=ones8[:], data1=lf[i2][:],
                                                            initial=carry[:], op0=ALU.mult, op1=ALU.add),
              reads=[ones8, lf[i2], carry], writes=[Gt[i2]])
        kb.op('dve', lambda e, i2=i2: e.tensor_copy(out=carry[:], in_=Gt[i2][:, 511:512]),
              reads=[Gt[i2]], writes=[carry])
        gp_ = gpt[i2]
        kb.op('dve', lambda e, i2=i2, gp_=gp_: e.tensor_copy(out=gp_[:, 0, :], in_=Gt[i2][:]),
              reads=[Gt[i2]], writes=[gp_])
        kb.op('dve', lambda e, i2=i2, gp_=gp_: e.tensor_tensor(out=r1[:], in0=Gt[i2][:], in1=gp_[:, 0, :],
                                                                op=ALU.subtract),
              reads=[Gt[i2], gp_], writes=[r1])
        kb.op('dve', lambda e, gp_=gp_: e.tensor_copy(out=gp_[:, 1, :], in_=r1[:]), reads=[r1], writes=[gp_])
        kb.op('dve', lambda e, gp_=gp_: e.tensor_tensor(out=r2[:], in0=r1[:], in1=gp_[:, 1, :],
                                                        op=ALU.subtract), reads=[r1, gp_], writes=[r2])
        kb.op('dve', lambda e, gp_=gp_: e.tensor_copy(out=gp_[:, 2, :], in_=r2[:]), reads=[r2], writes=[gp_])
        kb.dma('pool', GP[s][:, :, g * 512:(g + 1) * 512], gp_[:], reads=[gp_], writes=[GP[s]], sembuf=gp_)
        qs = qkst[gcount % 2]
        for ci in range(16):
            pq = psum[4 + (ci % 4)]
            c0 = qk_cols[ci]
            for kc in range(8):
                kb.op('pe', lambda e, kc=kc, pq=pq, c0=c0, hTg=hTg: e.matmul(
                    pq[:, :], lhsT=w_in_bf[:, kc, c0:c0 + 128], rhs=hTg[:, kc, :],
                    start=(kc == 0), stop=(kc == 7)), reads=[w_in_bf, hTg], writes=[pq])
            sc = 0.125 if qk_isq[ci] else 1.0
            if evac % 2 == 0:
                kb.op('act', lambda e, pq=pq, qs=qs, ci=ci, sc=sc: e.activation(
                    out=qs[:, ci, :], in_=pq[:, :], func=AF.Copy, scale=sc), reads=[pq], writes=[qs])
            else:
                kb.op('dve', lambda e, pq=pq, qs=qs, ci=ci, sc=sc: e.tensor_scalar(
                    out=qs[:, ci, :], in0=pq[:, :], scalar1=sc, scalar2=None, op0=ALU.mult),
                    reads=[pq], writes=[qs])
            evac += 1
        kb.dma('pool', QK[s][:, :, g * 512:(g + 1) * 512].rearrange("c p t -> p c t"), qs[:],
               reads=[qs], writes=[QK[s]], sembuf=qs)
        vs_ = vst[gcount % 2]
        for j in range(4):
            for hf, c0 in enumerate((1024, 2568)):
                pv = psum[2 + ((j * 2 + hf) % 2)] if False else psum[4 + ((j * 2 + hf) % 4)]
                for kc in range(8):
                    kb.op('pe', lambda e, kc=kc, pv=pv, c0=c0, hTg=hTg, j=j: e.matmul(
                        pv[:, :], lhsT=hTg[:, kc, j * 128:(j + 1) * 128], rhs=w_in_bf[:, kc, c0:c0 + 512],
                        start=(kc == 0), stop=(kc == 7)), reads=[w_in_bf, hTg], writes=[pv])
                if evac % 2 == 0:
                    kb.op('act', lambda e, pv=pv, vs_=vs_, j=j, hf=hf: e.activation(
                        out=vs_[:, j, hf * 512:(hf + 1) * 512], in_=pv[:, :], func=AF.Copy),
                        reads=[pv], writes=[vs_])
                else:
                    kb.op('dve', lambda e, pv=pv, vs_=vs_, j=j, hf=hf: e.tensor_copy(
                        out=vs_[:, j, hf * 512:(hf + 1) * 512], in_=pv[:, :]), reads=[pv], writes=[vs_])
                evac += 1
        kb.dma('pool', VV[s][g * 512:(g + 1) * 512, :].rearrange("(j p) n -> p j n", p=128), vs_[:],
               reads=[vs_], writes=[VV[s]], sembuf=vs_)

    evac = 0
    A1(0)
    for gi in range(NSEQ * NG):
        Aa = record(A1, gi + 1) if gi + 1 < NSEQ * NG else []
        Bb = record(A2, gi)
        emit_merged([Aa, Bb])
        precast(8)
    phA.__exit__(None, None, None)

    phB = kb.phase()
    phB.__enter__()
    tri = kb.sb("tri", [128, 128], BF16)
    kb.dma('sp', tri[:], tri_d[:], writes=[tri], sembuf=tri)
    Tt = kb.sb("Tt", [128, 24, 256], F32)
    rb = kb.sb("rb", [32, 8], F32)
    kb.dma('sp', rb[:], relb_d[:], writes=[rb], sembuf=rb)
    oh = kb.sb("oh", [32, 3, 384], F32)
    kb.dma('sp', oh[:], oh_d[:], writes=[oh], sembuf=oh)
    negm = kb.sb("negm", [8, 3, 384], F32)
    kb.dma('sp', negm[:], negm_d[:], writes=[negm], sembuf=negm)
    Jm = kb.sb("Jm", [128, 128], F32)
    kb.dma('sp', Jm[:], J_d[:], writes=[Jm], sembuf=Jm)
    extsb = kb.sb("extsb", [8, 3, 384], F32)
    for di in range(3):
        pt = psum[di % 2]
        kb.op('pe', lambda e, di=di, pt=pt: e.matmul(pt[0:8, 0:384], lhsT=rb[:, :], rhs=oh[:, di, :],
                                                      start=True, stop=True), reads=[rb, oh], writes=[pt])
        kb.op('dve', lambda e, di=di, pt=pt: e.tensor_tensor(out=extsb[:, di, :], in0=pt[0:8, 0:384],
                                                              in1=negm[:, di, :], op=ALU.add),
              reads=[pt, negm], writes=[extsb])
    kb.dma('sp', EXT[:], extsb[:], reads=[extsb], writes=[EXT], sembuf=extsb)
    hk = [kb.sb(f"hk{i}", [128, 256], F32) for i in range(2)]
    for di in range(3):
        for h in range(8):
            hkt = hk[(di * 8 + h) % 2]
            kb.dma('sp', hkt[:], bass.AP(EXT.t, (h * 3 + di) * 384, [[1, 128], [1, 256]]),
                   reads=[EXT], writes=[hkt], sembuf=hkt)
            pt = psum[(di * 8 + h) % 2]
            kb.op('pe', lambda e, hkt=hkt, pt=pt: e.matmul(pt[:, 0:256], lhsT=Jm[:, :], rhs=hkt[:, :],
                                                            start=True, stop=True), reads=[Jm, hkt], writes=[pt])
            kb.op('dve', lambda e, pt=pt, di=di, h=h: e.tensor_copy(out=Tt[:, di * 8 + h, :], in_=pt[:, 0:256]),
                  reads=[pt], writes=[Tt])

    KT = [kb.sb(f"KT{i}", [70, S], BF16) for i in range(2)]
    QT = [kb.sb(f"QT{i}", [70, S], BF16) for i in range(2)]
    KTd = [kb.sb(f"KTd{i}", [64, S], BF16) for i in range(2)]
    QTd = [kb.sb(f"QTd{i}", [64, S], BF16) for i in range(2)]
    Vf = [kb.sb(f"Vf{i}", [128, 32, 65], BF16) for i in range(2)]
    _vd = [kb.sb(f"Vd_{di}", [128, 32, 65], BF16) for di in range(3)]
    Vd = [_vd, _vd]
    for i in range(2):
        kb.op('pool', lambda e, i=i: e.memset(KT[i][64:70, :], -1.0), writes=[KT[i]])
        kb.op('pool', lambda e, i=i: e.memset(QT[i][64:70, :], 1.0), writes=[QT[i]])
        kb.op('pool', lambda e, i=i: e.memset(Vf[i][:, :, 64:65], 1.0), writes=[Vf[i]])
        if i == 0:
            for di in range(3):
                kb.op('pool', lambda e, i=i, di=di: e.memset(Vd[i][di][:, :, 64:65], 1.0), writes=[Vd[i][di]])
    Pt = [kb.sb(f"Pt{i}", [128, 512], BF16) for i in range(3)]
    Ptd = [kb.sb(f"Ptd{i}", [128, 256], BF16) for i in range(3)]
    Ssb = [kb.sb(f"Ssb{i}", [128, 256], F32) for i in range(3)]
    yst = [kb.sb(f"yst{i}", [128, 4, 64], F32) for i in range(2)]
    rcf = [kb.sb(f"rcf{i}", [128, 1], F32) for i in range(4)]
    ost = [kb.sb(f"ost{i}", [128, 32, 65], F32) for i in range(2)]
    hcount = 0
    ycount = 0
    ocount = 0
    for s in range(NSEQ):
        for h in range(8):
            hb = hcount % 2
            hcount += 1
            po = (h % 2) * 64
            kt_, qt_, ktd, qtd, vf = KT[hb], QT[hb], KTd[hb], QTd[hb], Vf[hb]
            kb.dma('sp', kt_[0:64, :], QK[s][4 + h // 2, po:po + 64, :], reads=[QK[s]], writes=[kt_], sembuf=kt_)
            kb.dma('sp', kt_[67:70, :], GP[s][h, :, :], reads=[GP[s]], writes=[kt_], sembuf=kt_)
            kb.dma('sp', qt_[0:64, :], QK[s][h // 2, po:po + 64, :], reads=[QK[s]], writes=[qt_], sembuf=qt_)
            kb.dma('sp', qt_[64:67, :], GP[s][h, :, :], reads=[GP[s]], writes=[qt_], sembuf=qt_)
            kb.dma('sp', vf[:, :, 0:64], VV[s][:, h * 64:(h + 1) * 64].rearrange("(n p) e -> p n e", p=128),
                   reads=[VV[s]], writes=[vf], sembuf=vf)
            kb.dma('sp', ktd[:, :], QK[s][12 + h // 2, po:po + 64, :], reads=[QK[s]], writes=[ktd], sembuf=ktd)
            kb.dma('sp', qtd[:, :], QK[s][8 + h // 2, po:po + 64, :], reads=[QK[s]], writes=[qtd], sembuf=qtd)
            for di, (win, d) in enumerate(DIL):
                NB = 32 // d
                vdt = Vd[hb][di]
                src = VV[s][:, 512 + h * 64:512 + (h + 1) * 64].rearrange("(n p r) e -> r p n e", p=128, r=d)
                for r in range(d):
                    kb.dma('sp', vdt[:, r * NB:(r + 1) * NB, 0:64], src[r], reads=[VV[s]], writes=[vdt], sembuf=vdt)
            for qg in range(8):
                nkt = 4 * (qg + 1)
                Oq = [psum[4 + i] for i in range(4)]

                def fox_s(kt, qg=qg):
                    j = kt - 4 * qg
                    c_lo = 128 * j if j > 0 else 0
                    Sp = psum[kt % 3]
                    Pk = Pt[kt % 3]
                    kb.op('pe', lambda e: e.matmul(
                        Sp[:, c_lo:512], lhsT=kt_[0:70, kt * 128:(kt + 1) * 128],
                        rhs=qt_[0:70, qg * 512 + c_lo:(qg + 1) * 512], start=True, stop=True),
                        reads=[kt_, qt_], writes=[Sp])
                    kb.op('act', lambda e: e.activation(
                        out=Pk[:, c_lo:512], in_=Sp[:, c_lo:512], func=AF.Exp), reads=[Sp], writes=[Pk])
                    if j >= 0:
                        kb.op('dve', lambda e: e.tensor_tensor(
                            out=Pk[:, c_lo:c_lo + 128], in0=Pk[:, c_lo:c_lo + 128], in1=tri[:, :], op=ALU.mult),
                            reads=[Pk, tri], writes=[Pk])

                def fox_pv(kt, qg=qg, Oq=Oq):
                    j = kt - 4 * qg
                    Pk = Pt[kt % 3]
                    for i in range(max(j, 0), 4):
                        kb.op('pe', lambda e, i=i: e.matmul(
                            Oq[i][:, 0:65], lhsT=Pk[:, i * 128:(i + 1) * 128], rhs=vf[:, kt, :],
                            start=(kt == 0), stop=(kt == 4 * qg + i)), reads=[Pk, vf], writes=[Oq[i]])
                for kt in range(nkt):
                    fox_s(kt)
                    if kt > 0:
                        fox_pv(kt - 1)
                fox_pv(nkt - 1)
                ys = yst[ycount % 2]
                ycount += 1
                for i in range(4):
                    kb.op('dve', lambda e, i=i: e.reciprocal(out=rcf[i][:], in_=Oq[i][:, 64:65]),
                          reads=[Oq[i]], writes=[rcf[i]])
                    kb.op('dve', lambda e, i=i, ys=ys: e.tensor_scalar(
                        out=ys[:, i, :], in0=Oq[i][:, 0:64], scalar1=rcf[i][:], scalar2=None, op0=ALU.mult),
                        reads=[Oq[i], rcf[i]], writes=[ys])
                kb.dma('pool', YF[s][qg * 512:(qg + 1) * 512, h * 64:(h + 1) * 64].rearrange("(i p) e -> p i e", p=128),
                       ys[:], reads=[ys], writes=[YF[s]], sembuf=ys)
            units = []
            for di, (win, d) in enumerate(DIL):
                NB = 32 // d
                for r in range(d):
                    for n in range(NB):
                        units.append((di, d, NB, r, n))
            Od = [psum[4], psum[5]]
            ostmap = {}

            def dil_s(ui, u):
                di, d, NB, r, n = u
                ncol = 256 if n < NB - 1 else 128
                st = n * 128 * d + r
                Sp = psum[ui % 3]
                Sb = Ssb[ui % 3]
                Pk = Ptd[ui % 3]
                kb.op('pe', lambda e: e.matmul(
                    Sp[:, 0:ncol], lhsT=ktd[0:64, st:st + 127 * d + 1:d], rhs=qtd[0:64, st:st + (ncol - 1) * d + 1:d],
                    start=True, stop=True), reads=[ktd, qtd], writes=[Sp])
                kb.op('dve', lambda e: e.tensor_tensor(
                    out=Sb[:, 0:ncol], in0=Sp[:, 0:ncol], in1=Tt[:, di * 8 + h, 0:ncol], op=ALU.add),
                    reads=[Sp, Tt], writes=[Sb])
                kb.op('act', lambda e: e.activation(
                    out=Pk[:, 0:ncol], in_=Sb[:, 0:ncol], func=AF.Exp), reads=[Sb], writes=[Pk])

            def dil_pv(ui, u):
                nonlocal ocount
                di, d, NB, r, n = u
                Pk = Ptd[ui % 3]
                vdt = Vd[hb][di]
                if n == 0:
                    ostmap[(di, r)] = ost[ocount % 2]
                    ocount += 1
                os_ = ostmap[(di, r)]
                kb.op('pe', lambda e: e.matmul(
                    Od[n % 2][:, 0:65], lhsT=Pk[:, 0:128], rhs=vdt[:, r * NB + n, :],
                    start=(n == 0), stop=True), reads=[Pk, vdt], writes=[Od[n % 2]])
                if n < NB - 1:
                    kb.op('pe', lambda e: e.matmul(
                        Od[(n + 1) % 2][:, 0:65], lhsT=Pk[:, 128:256], rhs=vdt[:, r * NB + n, :],
                        start=True, stop=False), reads=[Pk, vdt], writes=[Od[(n + 1) % 2]])
                kb.op('act', lambda e: e.activation(
                    out=os_[:, n, :], in_=Od[n % 2][:, 0:65], func=AF.Copy), reads=[Od[n % 2]], writes=[os_])
                if n == NB - 1:
                    dstv = OD[s][di, :, h, :].rearrange("(n p r) e -> r p n e", p=128, r=d)
                    kb.dma('pool', dstv[r], os_[:, 0:NB, :], reads=[os_], writes=[OD[s]], sembuf=os_)
            for ui, u in enumerate(units):
                dil_s(ui, u)
                if ui > 0:
                    dil_pv(ui - 1, units[ui - 1])
            dil_pv(len(units) - 1, units[-1])
    phB.__exit__(None, None, None)

    wts_all = kb.sb("wts_all", [128, NTILE, 8], F32)
    dest_all = kb.sb("dest_all", [128, NTILE, 8], I32)
    cnt_b = kb.sb("cnt_b", [128, NE], F32)
    idx_all = kb.sb("idx_all", [128, NBLK], I32)
    idx4_all = kb.sb("idx4_all", [128, NBLK], I32)
    ones_bf = kb.sb("ones_bf", [128, 128], BF16)
    kb.op('dve', lambda e: e.memset(ones_bf[:], 1.0), writes=[ones_bf])
    BIG = 1.0e4
    phCD = kb.phase()
    phCD.__enter__()
    M_all = kb.sb("M_all", [128, NTILE, NE], BF16)
    i8f_all = kb.sb("i8f_all", [128, NTILE, 8], F32)

    phC = kb.phase()
    phC.__enter__()
    w_out_bf = kb.sb("w_out_bf", [128, 8, D], BF16)
    wr_bf = kb.sb("wr_bf", [128, 8, NE], BF16)
    wsgu_bf = kb.sb("wsgu_bf", [128, 8, 512], BF16)
    wsd_bf = kb.sb("wsd_bf", [128, 2, D], BF16)
    for kc in range(8):
        rs = slice(kc * 128, (kc + 1) * 128)
        kb.dma('pool', w_out_bf[:, kc, :], w_out_d[rs, :], writes=[w_out_bf], sembuf=w_out_bf)
        kb.dma('pool', wr_bf[:, kc, :], w_r_d[rs, :], writes=[wr_bf], sembuf=wr_bf)
        kb.dma('pool', wsgu_bf[:, kc, 0:256], wsg_d[rs, :], writes=[wsgu_bf], sembuf=wsgu_bf)
        kb.dma('pool', wsgu_bf[:, kc, 256:512], wsu_d[rs, :], writes=[wsgu_bf], sembuf=wsgu_bf)
    for hc in range(2):
        kb.dma('pool', wsd_bf[:, hc, :], wsd_d[hc * 128:(hc + 1) * 128, :], writes=[wsd_bf], sembuf=wsd_bf)
    gmix = kb.sb("gmix", [128, 8], F32)
    kb.dma('sp', gmix[:], gmix_in[:], writes=[gmix], sembuf=gmix)
    rbias_b = kb.sb("rbias_b", [128, NE], F32)
    kb.dma('sp', rbias_b[:], bcast_rows(rbias_d[0:1, :]), writes=[rbias_b], sembuf=rbias_b)
    ym = [kb.sb(f"ym{i}", [128, D], F32) for i in range(2)]
    odt = [kb.sb(f"odt{i}", [128, 3, 520], F32) for i in range(2)]
    xq = [kb.sb(f"xq{i}", [128, D], F32) for i in range(3)]
    osum = kb.sb("osum", [128, 520], F32)
    rcd = kb.sb("rcd", [128, 8], F32)
    junkc = kb.sb("junkc", [128, D], BF16)
    junkc2 = kb.sb("junkc2", [128, D], BF16)
    ss2 = kb.sb("ss2", [128, 2], F32)
    sd2 = kb.sb("sd2", [128, 2], F32)
    rs2 = kb.sb("rs2", [128, 2], F32)
    ymn = kb.sb("ymn", [128, D], BF16)
    ymT = kb.sb("ymT", [128, 8, 128], BF16)
    tmpc = kb.sb("tmpc", [128, 512], F32)
    tmpc2 = kb.sb("tmpc2", [128, 512], F32)
    ss1 = kb.sb("ss1", [128, 1], F32)
    sd1 = kb.sb("sd1", [128, 1], F32)
    rs1 = kb.sb("rs1", [128, 1], F32)
    h2f = kb.sb("h2f", [128, D], F32)
    h2tok = [kb.sb(f"h2tok{i}", [128, D], BF16) for i in range(2)]
    h2Ts = [kb.sb(f"h2T{i}", [128, 8, 128], BF16) for i in range(2)]
    scores = kb.sb("scores", [128, NE], F32)
    sel = kb.sb("sel", [128, NE], F32)
    msel = kb.sb("msel", [128, NE], F32)
    wd_ = kb.sb("wd_", [128, NE], F32)
    m8 = kb.sb("m8", [128, 8, 8], F32)
    grp = kb.sb("grp", [128, 8], F32)
    g8 = kb.sb("g8", [128, 8], F32)
    gm = kb.sb("gm", [128, 8], F32)
    pen = kb.sb("pen", [128, 8], F32)
    t8 = kb.sb("t8", [128, 8], F32)
    w8 = kb.sb("w8", [128, 8], F32)
    i8u = kb.sb("i8u", [128, 8], U32)
    wsum = kb.sb("wsum", [128, 1], F32)
    rws = kb.sb("rws", [128, 1], F32)
    sg = kb.sb("sg", [128, 256], F32)
    actT = kb.sb("actT", [128, 256], BF16)
    kb.op('dve', lambda e: e.memset(cnt_b[:], 0.0), writes=[cnt_b])
    def S1a(tg):
        s = tg // NT
        t = tg % NT
        rows = slice(t * 128, (t + 1) * 128)
        grow = slice(tg * 128, (tg + 1) * 128)
        i2 = tg % 2
        h2T_ = h2Ts[i2]
        ymt, odd, xt = ym[i2], odt[i2], xq[tg % 3]
        kb.dma('sp', ymt[:, 0:512], YF[s][rows, :], reads=[YF[s]], writes=[ymt], sembuf=ymt)
        kb.dma('sp', odd[:], OD[s][:, rows, :, :].rearrange("d t h e -> t d (h e)"), reads=[OD[s]], writes=[odd],
               sembuf=odd)
        kb.dma('sp', xt[:], x_in[s, rows, :], writes=[xt], sembuf=xt)
        kb.op('dve', lambda e, odd=odd: e.tensor_tensor(out=osum[:], in0=odd[:, 0, :], in1=odd[:, 1, :], op=ALU.add),
              reads=[odd], writes=[osum])
        kb.op('dve', lambda e, odd=odd: e.tensor_tensor(out=osum[:], in0=osum[:], in1=odd[:, 2, :], op=ALU.add),
              reads=[odd, osum], writes=[osum])
        osv = osum[:].rearrange("p (h e) -> p h e", e=65)
        kb.op('dve', lambda e, osv=osv: e.reciprocal(out=rcd[:], in_=osv[:, :, 64]), reads=[osum], writes=[rcd])
        kb.op('dve', lambda e, osv=osv, ymt=ymt: e.tensor_tensor(
            out=ymt[:, 512:1024].rearrange("p (h e) -> p h e", e=64), in0=osv[:, :, 0:64],
            in1=rcd[:].unsqueeze(2).to_broadcast([128, 8, 64]), op=ALU.mult), reads=[osum, rcd], writes=[ymt])
        for hf in range(2):
            kb.op('act', lambda e, hf=hf, ymt=ymt: e.activation(
                out=junkc[:, 0:512], in_=ymt[:, hf * 512:(hf + 1) * 512], func=AF.Square,
                accum_out=ss2[:, hf:hf + 1]), reads=[ymt], writes=[junkc, ss2])
        kb.op('act', lambda e: e.activation(out=sd2[:], in_=ss2[:], func=AF.Sqrt, scale=1.0 / 512, bias=epsc[:]),
              reads=[ss2, epsc], writes=[sd2])
        kb.op('dve', lambda e: e.reciprocal(out=rs2[:], in_=sd2[:]), reads=[sd2], writes=[rs2])
        for hf in range(2):
            kb.op('act', lambda e, hf=hf, ymt=ymt: e.activation(
                out=ymn[:, hf * 512:(hf + 1) * 512], in_=ymt[:, hf * 512:(hf + 1) * 512], func=AF.Identity,
                scale=rs2[:, hf:hf + 1]), reads=[ymt, rs2], writes=[ymn])
        tp = psum[0]
        tpv = tp[:].bitcast(BF16)
        for kc in range(8):
            kb.op('pe', lambda e, kc=kc, tpv=tpv: e.transpose(
                out=tpv[:, kc * 128:(kc + 1) * 128], in_=ymn[:, kc * 128:(kc + 1) * 128], identity=ident[:]),
                reads=[ymn, ident], writes=[tp])
        kb.op('dve', lambda e, tpv=tpv: e.tensor_tensor(
            out=ymT[:], in0=tpv.rearrange("p (k t) -> p k t", t=128),
            in1=gmix[:].unsqueeze(2).to_broadcast([128, 8, 128]), op=ALU.mult), reads=[tp, gmix], writes=[ymT])
        for hf in range(2):
            po_ = psum[4 + hf]
            for kc in range(8):
                kb.op('pe', lambda e, kc=kc, hf=hf, po_=po_: e.matmul(
                    po_[:, :], lhsT=ymT[:, kc, :], rhs=w_out_bf[:, kc, hf * 512:(hf + 1) * 512],
                    start=(kc == 0), stop=(kc == 7)), reads=[ymT, w_out_bf], writes=[po_])
            g1 = gate_b[(2, s)]
            kb.op('dve', lambda e, hf=hf, po_=po_, g1=g1: e.tensor_tensor(
                out=tmpc[:], in0=po_[:, :], in1=g1[:, hf * 512:(hf + 1) * 512], op=ALU.mult),
                reads=[po_, g1], writes=[tmpc])
            kb.op('dve', lambda e, hf=hf, xt=xt: e.tensor_tensor(
                out=xt[:, hf * 512:(hf + 1) * 512], in0=xt[:, hf * 512:(hf + 1) * 512], in1=tmpc[:], op=ALU.add),
                reads=[xt, tmpc], writes=[xt])

    def S1b(tg):
        s = tg // NT
        grow = slice(tg * 128, (tg + 1) * 128)
        i2 = tg % 2
        xt = xq[tg % 3]
        h2T_ = h2Ts[i2]
        kb.op('act', lambda e, xt=xt: e.activation(out=junkc2[:], in_=xt[:], func=AF.Square, accum_out=ss1[:]),
              reads=[xt], writes=[junkc2, ss1])
        kb.op('act', lambda e: e.activation(out=sd1[:], in_=ss1[:], func=AF.Sqrt, scale=1.0 / D, bias=epsc[:]),
              reads=[ss1, epsc], writes=[sd1])
        kb.op('dve', lambda e: e.reciprocal(out=rs1[:], in_=sd1[:]), reads=[sd1], writes=[rs1])
        sc2, sh2 = gate_b[(4, s)], gate_b[(3, s)]
        kb.op('dve', lambda e, xt=xt, sc2=sc2: e.scalar_tensor_tensor(
            out=h2f[:], in0=xt[:], scalar=rs1[:], in1=sc2[:], op0=ALU.mult, op1=ALU.mult),
            reads=[xt, rs1, sc2], writes=[h2f])
        h2t = h2tok[i2]
        kb.op('dve', lambda e, h2t=h2t, sh2=sh2: e.tensor_tensor(out=h2t[:], in0=h2f[:], in1=sh2[:], op=ALU.add),
              reads=[h2f, sh2], writes=[h2t])
        kb.dma('sp', H2[grow, :], h2t[:], reads=[h2t], writes=[H2], sembuf=h2t)
        tp2 = psum[7]
        tp2v = tp2[:].bitcast(BF16)
        for kc in range(8):
            kb.op('pe', lambda e, kc=kc, tp2v=tp2v, h2t=h2t: e.transpose(
                out=tp2v[:, kc * 128:(kc + 1) * 128], in_=h2t[:, kc * 128:(kc + 1) * 128], identity=ident[:]),
                reads=[h2t, ident], writes=[tp2])
        kb.op('act', lambda e, tp2v=tp2v: e.activation(out=h2T_[:].rearrange("p k t -> p (k t)"), in_=tp2v,
                                                       func=AF.Copy), reads=[tp2], writes=[h2T_])

    def S2(tg):
        s = tg // NT
        grow = slice(tg * 128, (tg + 1) * 128)
        i2 = tg % 2
        xt = xq[tg % 3]
        h2T_ = h2Ts[i2]
        pr = psum[3]
        for kc in range(8):
            kb.op('pe', lambda e, kc=kc, pr=pr: e.matmul(pr[:, 0:NE], lhsT=h2T_[:, kc, :], rhs=wr_bf[:, kc, :],
                                                         start=(kc == 0), stop=(kc == 7)),
                  reads=[h2T_, wr_bf], writes=[pr])
        kb.op('act', lambda e, pr=pr: e.activation(out=scores[:], in_=pr[:, 0:NE], func=AF.Sigmoid),
              reads=[pr], writes=[scores])
        kb.op('dve', lambda e: e.tensor_tensor(out=sel[:], in0=scores[:], in1=rbias_b[:], op=ALU.add),
              reads=[scores, rbias_b], writes=[sel])
        for g in range(8):
            kb.op('dve', lambda e, g=g: e.max(out=m8[:, g, :], in_=sel[:, g * 32:(g + 1) * 32]),
                  reads=[sel], writes=[m8])
        kb.op('dve', lambda e: e.tensor_tensor(out=grp[:], in0=m8[:, :, 0], in1=m8[:, :, 1], op=ALU.add),
              reads=[m8], writes=[grp])
        kb.op('dve', lambda e: e.max(out=g8[:], in_=grp[:]), reads=[grp], writes=[g8])
        kb.op('dve', lambda e: e.tensor_scalar(out=gm[:], in0=grp[:], scalar1=g8[:, 3:4], scalar2=None,
                                               op0=ALU.is_ge), reads=[grp, g8], writes=[gm])
        kb.op('dve', lambda e: e.tensor_scalar(out=pen[:], in0=gm[:], scalar1=BIG, scalar2=-BIG, op0=ALU.mult,
                                               op1=ALU.add), reads=[gm], writes=[pen])
        kb.op('dve', lambda e: e.tensor_tensor(
            out=msel[:].rearrange("p (g e) -> p g e", e=32), in0=sel[:].rearrange("p (g e) -> p g e", e=32),
            in1=gm[:].unsqueeze(2).to_broadcast([128, 8, 32]), op=ALU.mult), reads=[sel, gm], writes=[msel])
        kb.op('dve', lambda e: e.tensor_tensor(
            out=msel[:].rearrange("p (g e) -> p g e", e=32), in0=msel[:].rearrange("p (g e) -> p g e", e=32),
            in1=pen[:].unsqueeze(2).to_broadcast([128, 8, 32]), op=ALU.add), reads=[msel, pen], writes=[msel])
        kb.op('dve', lambda e: e.max(out=t8[:], in_=msel[:]), reads=[msel], writes=[t8])
        kb.op('dve', lambda e, tg=tg: e.tensor_scalar(out=M_all[:, tg, :], in0=msel[:], scalar1=t8[:, 7:8],
                                                      scalar2=None, op0=ALU.is_ge),
              reads=[msel, t8], writes=[M_all])
        kb.op('dve', lambda e, tg=tg: e.tensor_tensor(out=wd_[:], in0=scores[:], in1=M_all[:, tg, :], op=ALU.mult),
              reads=[scores, M_all], writes=[wd_])
        kb.op('dve', lambda e: e.max(out=w8[:], in_=wd_[:]), reads=[wd_], writes=[w8])
        kb.op('dve', lambda e: e.max_index(out=i8u[:], in_max=w8[:], in_values=wd_[:]), reads=[w8, wd_],
              writes=[i8u])
        kb.op('dve', lambda e: e.tensor_reduce(out=wsum[:], in_=w8[:], axis=mybir.AxisListType.X, op=ALU.add),
              reads=[w8], writes=[wsum])
        kb.op('dve', lambda e: e.reciprocal(out=rws[:], in_=wsum[:]), reads=[wsum], writes=[rws])
        kb.op('dve', lambda e, tg=tg: e.tensor_scalar(out=wts_all[:, tg, :], in0=w8[:], scalar1=rws[:],
                                                      scalar2=2.5, op0=ALU.mult, op1=ALU.mult),
              reads=[w8, rws], writes=[wts_all])
        kb.op('dve', lambda e, tg=tg: e.tensor_copy(out=i8f_all[:, tg, :], in_=i8u[:]), reads=[i8u],
              writes=[i8f_all])
        kb.op('pe', lambda e, tg=tg, pr=pr: e.matmul(pr[:, 256:512], lhsT=ones_bf[:, :], rhs=M_all[:, tg, :],
                                                     start=True, stop=True),
              reads=[ones_bf, M_all], writes=[pr])
        kb.op('dve', lambda e, pr=pr: e.tensor_tensor(out=cnt_b[:], in0=cnt_b[:], in1=pr[:, 256:512], op=ALU.add),
              reads=[pr, cnt_b], writes=[cnt_b])
        pg = psum[6]
        for gi in range(4):
            for kc in range(8):
                kb.op('pe', lambda e, gi=gi, kc=kc, pg=pg: e.matmul(
                    pg[:, gi * 128:(gi + 1) * 128], lhsT=wsgu_bf[:, kc, gi * 128:(gi + 1) * 128], rhs=h2T_[:, kc, :],
                    start=(kc == 0), stop=(kc == 7)), reads=[wsgu_bf, h2T_], writes=[pg])
        kb.op('act', lambda e, pg=pg: e.activation(out=sg[:], in_=pg[:, 0:256], func=AF.Silu), reads=[pg],
              writes=[sg])
        kb.op('dve', lambda e, pg=pg: e.tensor_tensor(out=actT[:], in0=sg[:], in1=pg[:, 256:512], op=ALU.mult),
              reads=[sg, pg], writes=[actT])
        for hf in range(2):
            po_ = psum[1 + hf]
            for hc in range(2):
                kb.op('pe', lambda e, hc=hc, hf=hf, po_=po_: e.matmul(
                    po_[:, :], lhsT=actT[:, hc * 128:(hc + 1) * 128], rhs=wsd_bf[:, hc, hf * 512:(hf + 1) * 512],
                    start=(hc == 0), stop=(hc == 1)), reads=[actT, wsd_bf], writes=[po_])
            g2 = gate_b[(5, s)]
            kb.op('dve', lambda e, hf=hf, po_=po_, g2=g2: e.tensor_tensor(
                out=tmpc2[:], in0=po_[:, :], in1=g2[:, hf * 512:(hf + 1) * 512], op=ALU.mult),
                reads=[po_, g2], writes=[tmpc2])
            kb.op('dve', lambda e, hf=hf, xt=xt: e.tensor_tensor(
                out=xt[:, hf * 512:(hf + 1) * 512], in0=xt[:, hf * 512:(hf + 1) * 512], in1=tmpc2[:], op=ALU.add),
                reads=[xt, tmpc2], writes=[xt])
        kb.dma('sp', X2[grow, :], xt[:], reads=[xt], writes=[X2], sembuf=xt)
    S1a(0)
    S1a(1)
    S1b(0)
    for tg in range(NTILE):
        A = record(S1a, tg + 2) if tg + 2 < NTILE else []
        B = record(S1b, tg + 1) if tg + 1 < NTILE else []
        C = record(S2, tg)
        emit_merged([A, B, C])
        precast(4)
    precast(1000)
    phC.__exit__(None, None, None)

    phD = kb.phase()
    phD.__enter__()
    lst = kb.sb("lst", [128, 128], BF16)
    kb.dma('sp', lst[:], lst_d[:], writes=[lst], sembuf=lst)
    iota = kb.sb("iota", [128, NE], F32)
    kb.dma('sp', iota[:], iota_d[:], writes=[iota], sembuf=iota)
    bvals = kb.sb("bvals", [128, 6], F32)
    kb.dma('sp', bvals[:], bvals_d[:], writes=[bvals], sembuf=bvals)
    cnt_i = kb.sb("cnt_i", [128, NE], I32)
    nb_i = kb.sb("nb_i", [128, NE], I32)
    nb_f = kb.sb("nb_f", [128, NE], F32)
    Cc = kb.sb("Cc", [128, NE], F32)
    ones256 = kb.sb("ones256", [128, NE], F32)
    base = kb.sb("base", [128, NE], F32)
    junkd = kb.sb("junkd", [128, NE], F32)
    be_f = kb.sb("be_f", [128, 6], F32)
    be_i = kb.sb("be_i", [128, 6], I32)
    Dm = kb.sb("Dm", [128, NE], F32)
    dest_f = kb.sb("dest_f", [128, 8], F32)
    h2s = [kb.sb(f"h2s{i}", [128, D], BF16) for i in range(3)]
    kb.op('dve', lambda e: e.memset(ones256[:], 1.0), writes=[ones256])
    kb.op('dve', lambda e: e.tensor_copy(out=cnt_i[:], in_=cnt_b[:]), reads=[cnt_b], writes=[cnt_i])
    kb.op('dve', lambda e: e.tensor_scalar(out=cnt_i[:], in0=cnt_i[:], scalar1=BLK - 1, scalar2=None, op0=ALU.add),
          reads=[cnt_i], writes=[cnt_i])
    kb.op('dve', lambda e: e.tensor_scalar(out=nb_i[:], in0=cnt_i[:], scalar1=8, scalar2=None,
                                           op0=ALU.arith_shift_right), reads=[cnt_i], writes=[nb_i])
    kb.op('dve', lambda e: e.tensor_copy(out=nb_f[:], in_=nb_i[:]), reads=[nb_i], writes=[nb_f])
    kb.op('dve', lambda e: e.tensor_tensor_scan(out=Cc[:], data0=ones256[:], data1=nb_f[:], initial=0.0,
                                                op0=ALU.mult, op1=ALU.add), reads=[ones256, nb_f], writes=[Cc])
    kb.op('dve', lambda e: e.tensor_tensor(out=base[:], in0=Cc[:], in1=nb_f[:], op=ALU.subtract),
          reads=[Cc, nb_f], writes=[base])
    kb.op('dve', lambda e: e.tensor_scalar(out=base[:], in0=base[:], scalar1=float(BLK), scalar2=None, op0=ALU.mult),
          reads=[base], writes=[base])
    for tg in range(NTILE):
        pp = psum[tg % 2]
        pc = psum[2 + tg % 2]
        kb.op('pe', lambda e, tg=tg, pp=pp: e.matmul(pp[:, 0:NE], lhsT=lst[:, :], rhs=M_all[:, tg, :],
                                                     start=True, stop=True), reads=[lst, M_all], writes=[pp])
        kb.op('pe', lambda e, tg=tg, pc=pc: e.matmul(pc[:, 0:NE], lhsT=ones_bf[:, :], rhs=M_all[:, tg, :],
                                                     start=True, stop=True), reads=[ones_bf, M_all], writes=[pc])
        kb.op('dve', lambda e, pp=pp: e.tensor_tensor(out=Dm[:], in0=pp[:, 0:NE], in1=base[:], op=ALU.add),
              reads=[pp, base], writes=[Dm])
        kb.op('dve', lambda e, pc=pc: e.tensor_tensor(out=base[:], in0=pc[:, 0:NE], in1=base[:], op=ALU.add),
              reads=[pc, base], writes=[base])
        for k in range(8):
            kb.op('dve', lambda e, k=k, tg=tg: e.scalar_tensor_tensor(
                out=junkd[:], in0=iota[:], scalar=i8f_all[:, tg, k:k + 1], in1=Dm[:], op0=ALU.is_equal,
                op1=ALU.mult, accum_out=dest_f[:, k:k + 1]), reads=[iota, i8f_all, Dm], writes=[junkd, dest_f])
        kb.op('dve', lambda e, tg=tg: e.tensor_copy(out=dest_all[:, tg, :], in_=dest_f[:]), reads=[dest_f],
              writes=[dest_all])
        hs_ = h2s[tg % 3]
        kb.dma('sp', hs_[:], H2[tg * 128:(tg + 1) * 128, :], reads=[H2], writes=[hs_], sembuf=hs_)
        for k in range(8):
            kb.dma('pool', None, None, reads=[hs_, dest_all], writes=[HS], sembuf=hs_,
                   fn=lambda e, k=k, tg=tg, hs_=hs_: e.indirect_dma_start(
                       out=HS[:, :], out_offset=bass.IndirectOffsetOnAxis(ap=dest_all[:, tg, k:k + 1], axis=0),
                       in_=hs_[:, :], in_offset=None))
    be_bc = kb.sb("be_bc", [128, NBLK], F32)
    for bb in range(NBLK):
        kb.op('dve', lambda e, bb=bb: e.tensor_scalar(out=junkd[:], in0=Cc[:], scalar1=float(bb), scalar2=0.0,
                                                      op0=ALU.is_le, op1=ALU.add, accum_out=be_bc[:, bb:bb + 1]),
              reads=[Cc], writes=[junkd, be_bc])
    same = kb.sb("same", [128, NBLK], F32)
    bex = kb.sb("bex", [128, NBLK], F32)
    for lag, dst in ((3, idx_all), (4, idx4_all)):
        kb.op('dve', lambda e, lag=lag: e.memset(same[:, 0:lag], 0.0), writes=[same])
        kb.op('dve', lambda e, lag=lag: e.tensor_tensor(out=same[:, lag:NBLK], in0=be_bc[:, lag:NBLK],
                                                        in1=be_bc[:, 0:NBLK - lag], op=ALU.is_equal),
              reads=[be_bc], writes=[same])
        kb.op('dve', lambda e: e.scalar_tensor_tensor(out=bex[:], in0=same[:], scalar=float(2 * NE), in1=be_bc[:],
                                                      op0=ALU.mult, op1=ALU.add), reads=[same, be_bc], writes=[bex])
        kb.op('dve', lambda e: e.tensor_scalar(out=bex[:], in0=bex[:], scalar1=128.0, scalar2=bvals[:, 0:1],
                                               op0=ALU.mult, op1=ALU.add), reads=[bex, bvals], writes=[bex])
        kb.op('dve', lambda e, dst=dst: e.tensor_copy(out=dst[:], in_=bex[:]), reads=[bex], writes=[dst])
    phD.__exit__(None, None, None)
    phCD.__exit__(None, None, None)

    phE = kb.phase()
    phE.__enter__()
    wGU = [kb.sb(f"wGU{i}", [128, 4096], BF16) for i in range(3)]
    wDn = [kb.sb(f"wDn{i}", [128, 2048], BF16) for i in range(4)]
    xb = [kb.sb(f"xb{i}", [128, D], BF16) for i in range(6)]
    xbT = [kb.sb(f"xbT{i}", [128, 8, 256], BF16) for i in range(2)]
    sge = [kb.sb(f"sge{i}", [128, 256], F32) for i in range(4)]
    actk = [kb.sb(f"actk{i}", [128, 256], BF16) for i in range(4)]
    acte = [kb.sb(f"acte{i}", [128, 2, 2, 128], BF16) for i in range(2)]
    yb = [kb.sb(f"yb{i}", [128, 2, D], BF16) for i in range(2)]
    bnd_reg = nc.gpsimd.alloc_register("bnd_reg")
    nc.gpsimd.reg_mov(bnd_reg, NE * 128 - 1)

    def gather(dst, src_d, idxt, sl):
        kb.dma('pool', None, None, reads=[idxt, src_d], writes=[dst], sembuf=dst,
               fn=lambda e: e.indirect_dma_start(
                   out=dst[:, :], out_offset=None, in_=src_d[:, :],
                   in_offset=bass.IndirectOffsetOnAxis(ap=idxt[:, sl:sl + 1], axis=0),
                   bounds_check=bnd_reg, oob_is_err=False))

    def issue_w(sl):
        gather(wGU[sl % 3], WGUb, idx_all, sl)
        gather(wDn[sl % 4], WDb, idx4_all, sl)

    def issue_x(sl):
        for sb in range(2):
            xt_ = xb[(2 * sl + sb) % 6]
            r0 = sl * BLK + sb * 128
            kb.dma('sp', xt_[:], HS[r0:r0 + 128, :], reads=[HS], writes=[xt_], sembuf=xt_)

    def e_T(sl):
        xT = xbT[sl % 2]
        for sb in range(2):
            xt_ = xb[(2 * sl + sb) % 6]
            tp = psum[sb]
            tpv = tp[:].bitcast(BF16)
            for kc in range(8):
                kb.op('pe', lambda e, kc=kc: e.transpose(
                    out=tpv[:, kc * 128:(kc + 1) * 128], in_=xt_[:, kc * 128:(kc + 1) * 128], identity=ident[:]),
                    reads=[xt_, ident], writes=[tp])
            src = tpv.rearrange("p (k t) -> p k t", t=128)
            if sb == 0:
                kb.op('dve', lambda e: e.tensor_copy(out=xT[:, :, 0:128], in_=src), reads=[tp], writes=[xT])
            else:
                kb.op('act', lambda e: e.activation(out=xT[:, :, 128:256], in_=src, func=AF.Copy),
                      reads=[tp], writes=[xT])

    def e_GU(sl):
        xT = xbT[sl % 2]
        for sb in range(2):
            pg = psum[2 + sb]
            for kc in range(8):
                wt_ = wGU[sl % 3]
                kb.op('pe', lambda e, kc=kc, wt_=wt_: e.matmul(
                    pg[:, :], lhsT=xT[:, kc, sb * 128:(sb + 1) * 128], rhs=wt_[:, kc * 512:(kc + 1) * 512],
                    start=(kc == 0), stop=(kc == 7)), reads=[wt_, xT], writes=[pg])
            sg_ = sge[(2 * sl + sb) % 4]
            ak_ = actk[(2 * sl + sb) % 4]
            kb.op('act', lambda e: e.activation(out=sg_[:], in_=pg[:, 0:256], func=AF.Silu), reads=[pg],
                  writes=[sg_])
            kb.op('dve', lambda e: e.tensor_tensor(out=ak_[:], in0=sg_[:], in1=pg[:, 256:512], op=ALU.mult),
                  reads=[sg_, pg], writes=[ak_])

    def e_AT(sl):
        ta = psum[4]
        tav = ta[:].bitcast(BF16)
        for sb in range(2):
            ak_ = actk[(2 * sl + sb) % 4]
            for hc in range(2):
                c0 = (sb * 2 + hc) * 128
                kb.op('pe', lambda e, hc=hc, c0=c0: e.transpose(
                    out=tav[:, c0:c0 + 128], in_=ak_[:, hc * 128:(hc + 1) * 128], identity=ident[:]),
                    reads=[ak_, ident], writes=[ta])
        ac_ = acte[sl % 2]
        kb.op('dve', lambda e: e.tensor_copy(out=ac_[:].rearrange("p a b t -> p (a b t)"), in_=tav[:, 0:512]),
              reads=[ta], writes=[ac_])

    pocnt = [0]

    def e_DN(sl):
        wd = wDn[sl % 4][:].rearrange("p (h n) -> p h n", n=D)
        ac_ = acte[sl % 2]
        yb_ = yb[sl % 2]
        for sb in range(2):
            for hf in range(2):
                po_ = psum[5 + pocnt[0] % 3]
                pocnt[0] += 1
                for hc in range(2):
                    kb.op('pe', lambda e, hc=hc: e.matmul(
                        po_[:, :], lhsT=ac_[:, sb, hc, :], rhs=wd[:, hc, hf * 512:(hf + 1) * 512],
                        start=(hc == 0), stop=(hc == 1)), reads=[ac_, wDn[sl % 4]], writes=[po_])
                if hf == 0:
                    kb.op('dve', lambda e: e.tensor_copy(out=yb_[:, sb, 0:512], in_=po_[:, :]),
                          reads=[po_], writes=[yb_])
                else:
                    kb.op('act', lambda e: e.activation(out=yb_[:, sb, 512:1024], in_=po_[:, :], func=AF.Copy),
                          reads=[po_], writes=[yb_])
        kb.dma('sp', YS[sl * BLK:(sl + 1) * BLK, :].rearrange("(sb p) n -> p sb n", p=128), yb_[:],
               reads=[yb_], writes=[YS], sembuf=yb_)

    issue_w(0)
    issue_w(1)
    issue_x(0)
    issue_x(1)
    e_T(0)
    for sl in range(NBLK):
        if sl + 2 < NBLK:
            issue_w(sl + 2)
            issue_x(sl + 2)
        if sl + 1 < NBLK:
            e_T(sl + 1)
        if sl > 0:
            e_AT(sl - 1)
        e_GU(sl)
        if sl > 0:
            e_DN(sl - 1)
    e_AT(NBLK - 1)
    e_DN(NBLK - 1)
    phE.__exit__(None, None, None)

    phF = kb.phase()
    phF.__enter__()
    gfin_b = kb.sb("gfin_b", [128, D], F32)
    kb.dma('sp', gfin_b[:], bcast_rows(gfin_d[0:1, :]), writes=[gfin_b], sembuf=gfin_b)
    x2t = [kb.sb(f"x2t{i}", [128, D], F32) for i in range(2)]
    yk = [kb.sb(f"yk{i}", [128, D], BF16) for i in range(4)]
    acc = kb.sb("acc", [128, D], F32)
    tmpf = kb.sb("tmpf", [128, D], F32)
    junkf = kb.sb("junkf", [128, D], BF16)
    ssf = kb.sb("ssf", [128, 1], F32)
    sdf = kb.sb("sdf", [128, 1], F32)
    rsf = kb.sb("rsf", [128, 1], F32)
    ot = [kb.sb(f"ot{i}", [128, D], F32) for i in range(2)]
    gi_ = 0
    for tg in range(NTILE):
        s = tg // NT
        t = tg % NT
        xt = x2t[tg % 2]
        kb.dma('sp', xt[:], X2[tg * 128:(tg + 1) * 128, :], reads=[X2], writes=[xt], sembuf=xt)
        for k in range(8):
            yk_ = yk[gi_ % 4]
            gi_ += 1
            kb.dma('pool', None, None, reads=[YS, dest_all], writes=[yk_], sembuf=yk_,
                   fn=lambda e, k=k, tg=tg, yk_=yk_: e.indirect_dma_start(
                       out=yk_[:, :], out_offset=None, in_=YS[:, :],
                       in_offset=bass.IndirectOffsetOnAxis(ap=dest_all[:, tg, k:k + 1], axis=0)))
            if k == 0:
                kb.op('dve', lambda e, k=k, tg=tg, yk_=yk_: e.tensor_scalar(
                    out=acc[:], in0=yk_[:], scalar1=wts_all[:, tg, k:k + 1], scalar2=None, op0=ALU.mult),
                    reads=[yk_, wts_all], writes=[acc])
            else:
                kb.op('dve', lambda e, k=k, tg=tg, yk_=yk_: e.scalar_tensor_tensor(
                    out=acc[:], in0=yk_[:], scalar=wts_all[:, tg, k:k + 1], in1=acc[:], op0=ALU.mult, op1=ALU.add),
                    reads=[yk_, wts_all, acc], writes=[acc])
        g2 = gate_b[(5, s)]
        kb.op('dve', lambda e, g2=g2: e.tensor_tensor(out=tmpf[:], in0=acc[:], in1=g2[:], op=ALU.mult),
              reads=[acc, g2], writes=[tmpf])
        kb.op('dve', lambda e, xt=xt: e.tensor_tensor(out=xt[:], in0=xt[:], in1=tmpf[:], op=ALU.add),
              reads=[xt, tmpf], writes=[xt])
        kb.op('act', lambda e, xt=xt: e.activation(out=junkf[:], in_=xt[:], func=AF.Square, accum_out=ssf[:]),
              reads=[xt], writes=[junkf, ssf])
        kb.op('act', lambda e: e.activation(out=sdf[:], in_=ssf[:], func=AF.Sqrt, scale=1.0 / D, bias=epsc[:]),
              reads=[ssf, epsc], writes=[sdf])
        kb.op('dve', lambda e: e.reciprocal(out=rsf[:], in_=sdf[:]), reads=[sdf], writes=[rsf])
        o_ = ot[tg % 2]
        kb.op('dve', lambda e, xt=xt, o_=o_: e.scalar_tensor_tensor(
            out=o_[:], in0=xt[:], scalar=rsf[:], in1=gfin_b[:], op0=ALU.mult, op1=ALU.mult),
            reads=[xt, rsf, gfin_b], writes=[o_])
        kb.dma('sp', out_d[s, t * 128:(t + 1) * 128, :], o_[:], reads=[o_], writes=[out_d], sembuf=o_)
    phF.__exit__(None, None, None)
    allb = [out_d]
    for e in ('sp',):
        kb.wait_all(e, allb)
    return nc, kb, es


_CACHE = {}


def relayout_experts(inputs):
    g = inputs["w_exp_gate"][0].reshape(NE, 8, 128, 256).transpose(0, 2, 1, 3)
    u = inputs["w_exp_up"][0].reshape(NE, 8, 128, 256).transpose(0, 2, 1, 3)
    gu = np.concatenate([g, u], axis=3)
    wa = np.ascontiguousarray(gu[:, :, 0:4, :]).reshape(NE * 128, 2048)
    wb = np.ascontiguousarray(gu[:, :, 4:8, :]).reshape(NE * 128, 2048)
    wd = np.ascontiguousarray(inputs["w_exp_down"][0].reshape(NE, 2, 128, D).transpose(0, 2, 1, 3)).reshape(NE * 128, 2048)
    return {"_wa": wa, "_wb": wb, "_wd": wd}


def kernel(**inputs):
    inputs = {k: np.asarray(v) for k, v in inputs.items()}
    if 'nc' not in _CACHE:
        _CACHE['nc'] = build(False)
    nc = _CACHE['nc'][0]
    inputs.update(relayout_experts(inputs))
    in_maps = [make_inputs(c, **inputs) for c in range(8)]
    res = run_bass_kernel_spmd(nc, in_maps, core_ids=list(range(8)))
    out = np.concatenate([np.asarray(r["out"]) for r in res.results], axis=0)
    return out.astype(np.float32)


def _t5_bucket(dist):
    max_exact = 16
    dd = np.maximum(dist, 1).astype(np.float32)
    large = max_exact + (np.log(dd / max_exact) / np.log(2048 / max_exact) * (32 - max_exact)).astype(np.int32)
    large = np.minimum(large, 31)
    return np.where(dist < max_exact, dist, large).astype(np.int32)


def _consts():
    oh = np.zeros((32, 3, 384), np.float32)
    negm = np.full((8, 3, 384), NEG, np.float32)
    for di, (win, d) in enumerate(DIL):
        rel = np.arange(129)
        bk = _t5_bucket(rel * d)
        oh[bk, di, 127 + rel] = 1.0
        negm[:, di, 127:127 + 129] = 0.0
    J = np.zeros((128, 128), np.float32)
    J[np.arange(128), 127 - np.arange(128)] = 1.0
    tri = (np.arange(128)[None, :] >= np.arange(128)[:, None]).astype(ml_dtypes.bfloat16)
    lst = (np.arange(128)[:, None] < np.arange(128)[None, :]).astype(ml_dtypes.bfloat16)
    iota = np.tile(np.arange(NE, dtype=np.float32)[None, :], (128, 1))
    bvals = (np.arange(128)[:, None] + 128 * np.arange(6)[None, :]).astype(np.float32)
    return {"oh_in": oh, "negm_in": negm, "J_in": J, "tri_in": tri, "lst_in": lst, "iota_in": iota,
            "bvals_in": bvals}


CONSTS = _consts()


def make_inputs(core, x, c, w_in, b_forget, g_fox_out, g_dil_out, w_out, w_ada, b_ada, **kw):
    f = np.ascontiguousarray
    xs = f(x[NSEQ * core:NSEQ * (core + 1)])
    cs = c[NSEQ * core:NSEQ * (core + 1)]
    cT = f(cs.T.reshape(8, 128, NSEQ).transpose(1, 0, 2))
    gm = np.concatenate([g_fox_out[0], g_dil_out[0]])
    return {
        "x": xs, "cT": cT, "w_in": f(w_in[0]), "b_forget": f(b_forget[0].reshape(8, 1)),
        "g_mix": f(gm.reshape(8, 128).T), "w_out": f(w_out[0]), "w_ada": f(w_ada[0]),
        "b_ada": f(b_ada[0].reshape(1, -1)), "b_adaT": f(b_ada[0].reshape(48, 128).T),
        "ident_in": np.eye(128, dtype=ml_dtypes.bfloat16),
        "rel_bias": f(kw["rel_bias"]), **CONSTS,
        "w_router": f(kw["w_router"][0]), "router_bias": f(kw["router_bias"][0].reshape(1, -1)),
        "w_sh_gate": f(kw["w_sh_gate"][0]), "w_sh_up": f(kw["w_sh_up"][0]), "w_sh_down": f(kw["w_sh_down"][0]),
        "w_exp_a": kw["_wa"], "w_exp_b": kw["_wb"], "w_exp_d": kw["_wd"],
        "g_final": f(kw["g_final"].reshape(1, -1)),
    }
```

```python
import numpy as np
import ml_dtypes
from contextlib import ExitStack
import concourse.bass as bass
import concourse.mybir as mybir
from concourse.bass_utils import run_bass_kernel_spmd

F32 = mybir.dt.float32
BF16 = mybir.dt.bfloat16
I32 = mybir.dt.int32
U32 = mybir.dt.uint32
AF = mybir.ActivationFunctionType
ALU = mybir.AluOpType

S = 4096
D = 1024
NSEQ = 2
NT = S // 128
NG = S // 512
NTOK = NSEQ * S
NTILE = NTOK // 128
INC = 3080
NE = 256
TOPK = 8
BLK = 256
NBLK = NTOK * TOPK // BLK + NE
RROWS = NBLK * BLK
EPS = 1e-6
NEG = -30000.0
DIL = ((128, 1), (512, 4), (2048, 16))
LIMIT = 30000


class Buf:
    def __init__(self, name, dram=False):
        self.name = name
        self.w = {}
        self.r = {}
        self.sem = None
        self.cnt = 0
        self.dram = dram


class TT:
    def __init__(self, t, name, dram=False):
        self.t = t
        self.buf = Buf(name, dram)

    def __getitem__(self, k):
        return self.t[k]


class KB:
    def __init__(self, nc, es):
        self.nc = nc
        self.es = es
        self.engs = {'pe': nc.tensor, 'dve': nc.vector, 'act': nc.scalar, 'pool': nc.gpsimd, 'sp': nc.sync}
        self.esem = {}
        self.ecnt = {}
        self.known = {e: {} for e in self.engs}
        self.nsem = 0
        self.nins = {e: 0 for e in self.engs}
        self.cur_es = es
        self.dmabufs = []
        self.free_sems = []
        self.phase_bufs = [[]]
        for e in self.engs:
            self._epoch(e)

    def phase(self):
        kb = self

        class _Ph:
            def __enter__(self_):
                self_.es = ExitStack()
                self_.prev = kb.cur_es
                kb.cur_es = self_.es
                kb.phase_bufs.append([])
                return self_

            def __exit__(self_, *a):
                kb.barrier()
                for b in kb.phase_bufs.pop():
                    if b.sem is not None:
                        kb.free_sems.append((b.sem, b.cnt))
                        if b in kb.dmabufs:
                            kb.dmabufs.remove(b)
                        b.sem = None
                kb.cur_es = self_.prev
                self_.es.close()
                return False
        return _Ph()

    def barrier(self):
        evs = {}
        for e in self.engs:
            if self.ecnt[e] > 0:
                evs[self.esem[e]] = self.ecnt[e]
        for b in self.dmabufs:
            if b.sem is not None and b.cnt > 0 and b.name != "precast":
                evs[b.sem] = b.cnt
        for e in self.engs:
            kn = self.known[e]
            for sem, val in evs.items():
                if sem == self.esem[e]:
                    continue
                if kn.get(sem, 0) < val:
                    self.engs[e].wait_ge(sem, val)
                    kn[sem] = val

    def newsem(self, name):
        self.nsem += 1
        return self.es.enter_context(self.nc.semaphore(f"{name}_{self.nsem}"))

    def _epoch(self, e):
        self.esem[e] = self.newsem("e" + e)
        self.ecnt[e] = 0

    def sb(self, name, shape, dt):
        t = TT(self.cur_es.enter_context(self.nc.sbuf_tensor(name, list(shape), dt)), name)
        self.phase_bufs[-1].append(t.buf)
        return t

    def ps(self, name, shape, dt):
        return TT(self.es.enter_context(self.nc.psum_tensor(name, list(shape), dt)), name)

    def dram(self, name, shape, dt, kind="Internal"):
        return TT(self.nc.dram_tensor(name, list(shape), dt, kind=kind), name, dram=True)

    def _wait(self, e, reads, writes):
        deps = {}

        def add(sem, val, src, raw):
            if src == e:
                if e == 'pe' or e == 'sp':
                    return
                if not raw:
                    return
            k = sem
            if deps.get(k, 0) < val:
                deps[k] = val
        for b in reads:
            b = b.buf if isinstance(b, TT) else b
            for sem, (val, src) in b.w.items():
                add(sem, val, src, True)
        for b in writes:
            b = b.buf if isinstance(b, TT) else b
            for sem, (val, src) in b.w.items():
                add(sem, val, src, False)
            for sem, (val, src) in b.r.items():
                add(sem, val, src, False)
        kn = self.known[e]
        for sem, val in deps.items():
            if kn.get(sem, 0) < val:
                self.engs[e].wait_ge(sem, val)
                kn[sem] = val

    def _post(self, ev, reads, writes):
        sem, val, src = ev
        for b in writes:
            b = b.buf if isinstance(b, TT) else b
            if b.dram:
                b.w[sem] = (val, src)
            else:
                b.w = {sem: (val, src)}
                b.r = {}
        for b in reads:
            b = b.buf if isinstance(b, TT) else b
            b.r[sem] = (val, src)

    def op(self, e, fn, reads=(), writes=()):
        self._wait(e, reads, writes)
        if self.ecnt[e] >= LIMIT:
            self._epoch(e)
        ins = fn(self.engs[e])
        self.ecnt[e] += 1
        self.nins[e] += 1
        ins.then_inc(self.esem[e], 1)
        self._post((self.esem[e], self.ecnt[e], e), reads, writes)
        return ins

    def dma(self, q, out, in_, reads=(), writes=(), sembuf=None, fn=None, **kw):
        self._wait(q, reads, writes)
        sb_ = sembuf.buf if isinstance(sembuf, TT) else sembuf
        if sb_.sem is None or sb_.cnt >= LIMIT:
            if sb_.sem is None and self.free_sems and self.free_sems[-1][1] < LIMIT // 2:
                sb_.sem, sb_.cnt = self.free_sems.pop()
            else:
                sb_.sem = self.newsem("d" + sb_.name[:8])
                sb_.cnt = 0
            if sb_ not in self.dmabufs:
                self.dmabufs.append(sb_)
        if fn is None:
            ins = self.engs[q].dma_start(out=out, in_=in_, **kw)
        else:
            ins = fn(self.engs[q])
        sb_.cnt += 16
        self.nins[q] += 1
        ins.then_inc(sb_.sem, 16)
        self._post((sb_.sem, sb_.cnt, 'dma'), reads, writes)
        return ins

    def wait_all(self, e, bufs):
        self._wait(e, bufs, ())


def bcast_rows(ap_row, nparts=128):
    n = ap_row.shape[-1]
    return bass.AP(ap_row.tensor, ap_row.offset, [[0, nparts], [1, n]])


def build(debug=False):
    nc = bass.Bass("TRN2", target_bir_lowering=False)
    es = ExitStack()
    kb = KB(nc, es)
    ein = lambda name, shape, dt=F32: TT(nc.dram_tensor(name, list(shape), dt, kind="ExternalInput"), name, dram=True)
    okind = "ExternalOutput" if debug else "Internal"

    def record(f, *a):
        rec = []
        orig_op, orig_dma = kb.op, kb.dma
        kb.op = lambda *args, **kw: rec.append((orig_op, args, kw))
        kb.dma = lambda *args, **kw: rec.append((orig_dma, args, kw))
        try:
            f(*a)
        finally:
            kb.op, kb.dma = orig_op, orig_dma
        return rec

    def emit_interleaved(A, B):
        ia = ib = 0
        while ia < len(A) or ib < len(B):
            if ib >= len(B) or (ia < len(A) and ia * len(B) <= ib * len(A)):
                fn, args, kw = A[ia]
                ia += 1
            else:
                fn, args, kw = B[ib]
                ib += 1
            fn(*args, **kw)

    def emit_merged(streams):
        streams = [st for st in streams if st]
        pos = [0] * len(streams)
        total = sum(len(st) for st in streams)
        for _ in range(total):
            best = min((i for i in range(len(streams)) if pos[i] < len(streams[i])),
                       key=lambda i: pos[i] / len(streams[i]))
            fn, args, kw = streams[best][pos[best]]
            pos[best] += 1
            fn(*args, **kw)


    x_in = ein("x", [NSEQ, S, D])
    cT_in = ein("cT", [128, 8, NSEQ])
    w_in_d = ein("w_in", [D, INC])
    bfg_in = ein("b_forget", [8, 1])
    gmix_in = ein("g_mix", [128, 8])
    w_out_d = ein("w_out", [D, D])
    w_ada_d = ein("w_ada", [D, 6 * D])
    b_ada_d = ein("b_ada", [1, 6 * D])
    b_adaT_d = ein("b_adaT", [128, 48])
    ident_d = ein("ident_in", [128, 128], BF16)
    relb_d = ein("rel_bias", [32, 8])
    oh_d = ein("oh_in", [32, 3, 384])
    negm_d = ein("negm_in", [8, 3, 384])
    J_d = ein("J_in", [128, 128])
    tri_d = ein("tri_in", [128, 128], BF16)
    w_r_d = ein("w_router", [D, NE])
    rbias_d = ein("router_bias", [1, NE])
    wsg_d = ein("w_sh_gate", [D, 256])
    wsu_d = ein("w_sh_up", [D, 256])
    wsd_d = ein("w_sh_down", [256, D])
    wA_d = ein("w_exp_a", [NE * 128, 2048])
    wB_d = ein("w_exp_b", [NE * 128, 2048])
    wD_d = ein("w_exp_d", [NE * 128, 2048])
    gfin_d = ein("g_final", [1, D])
    lst_d = ein("lst_in", [128, 128], BF16)
    iota_d = ein("iota_in", [128, NE])
    bvals_d = ein("bvals_in", [128, 6])

    out_d = TT(nc.dram_tensor("out", [NSEQ, S, D], F32, kind="ExternalOutput"), "out", dram=True)

    QK = [kb.dram(f"QK{s}", [16, 128, S], BF16, kind=okind) for s in range(NSEQ)]
    VV = [kb.dram(f"VV{s}", [S, 1024], BF16, kind=okind) for s in range(NSEQ)]
    GP = [kb.dram(f"GP{s}", [8, 3, S], BF16, kind=okind) for s in range(NSEQ)]
    EXT = kb.dram("EXT", [8, 3, 384], F32)
    YF = [kb.dram(f"YF{s}", [S, 512], F32, kind=okind) for s in range(NSEQ)]
    OD = [kb.dram(f"OD{s}", [3, S, 8, 65], F32, kind=okind) for s in range(NSEQ)]
    H2 = kb.dram("H2", [NTOK, D], BF16)
    X2 = kb.dram("X2", [NTOK, D], F32, kind=okind)
    HS = kb.dram("HS", [RROWS, D], BF16)
    WAb = kb.dram("WAb", [NE * 128, 2048], BF16)
    WBb = kb.dram("WBb", [NE * 128, 2048], BF16)
    WDb = kb.dram("WDb", [NE * 128, 2048], BF16)
    YS = kb.dram("YS", [RROWS, D], BF16)

    ident = kb.sb("ident", [128, 128], BF16)
    kb.dma('sp', ident[:], ident_d[:], writes=[ident], sembuf=ident)
    epsc = kb.sb("epsc", [128, 1], F32)
    kb.op('dve', lambda e: e.memset(epsc[:], EPS), writes=[epsc])
    cact = kb.sb("cact", [128, 8, NSEQ], F32)
    kb.dma('sp', cact[:], cT_in[:], writes=[cact], sembuf=cact)
    kb.op('act', lambda e: e.activation(out=cact[:], in_=cact[:], func=AF.Silu), reads=[cact], writes=[cact])
    badT = kb.sb("badT", [128, 48], F32)
    kb.dma('sp', badT[:], b_adaT_d[:], writes=[badT], sembuf=badT)
    bfg = kb.sb("bfg", [8, 1], F32)
    kb.dma('sp', bfg[:], bfg_in[:], writes=[bfg], sembuf=bfg)
    kb.op('dve', lambda e: e.tensor_scalar(out=bfg[:], in0=bfg[:], scalar1=-1.0, scalar2=None, op0=ALU.mult),
          reads=[bfg], writes=[bfg])

    psum = [kb.ps(f"ps{i}", [128, 512], F32) for i in range(8)]

    precast_buf = Buf("precast")
    PCH = 256
    precast_jobs = [(dst, src, r0) for r0 in range(0, NE * 128, PCH)
                    for dst, src in ((WAb, wA_d), (WBb, wB_d), (WDb, wD_d))]

    def precast(n):
        for _ in range(n):
            if precast_jobs:
                dst, src, r0 = precast_jobs.pop(0)
                kb.dma('pool', dst[r0:r0 + PCH, :], src[r0:r0 + PCH, :], writes=[dst], sembuf=precast_buf)

    modT = {m: kb.sb(f"modT{m}", [128, 8, NSEQ], F32) for m in (0, 1)}
    gate_b = {(m, s): kb.sb(f"gate{m}_{s}", [128, 1024], F32) for m in (2, 3, 4, 5) for s in range(NSEQ)}
    ph0 = kb.phase()
    ph0.__enter__()
    wa = [kb.sb(f"wa{i}", [128, 8, 1024], F32) for i in range(2)]
    cbc = kb.sb("cbc", [128, NSEQ, 8, 128], F32)
    for s in range(NSEQ):
        for kc in range(8):
            kb.op('dve', lambda e, s=s, kc=kc: e.tensor_copy(out=cbc[:, s, kc, :],
                                                              in_=cact[:, kc, s:s + 1].to_broadcast([128, 128])),
                  reads=[cact], writes=[cbc])
    wi = 0
    for m in range(6):
        wt = wa[wi % 2]
        wi += 1
        kb.dma('sp', wt[:], w_ada_d[:, m * 1024:(m + 1) * 1024].rearrange("(kc p) n -> p kc n", p=128),
               writes=[wt], sembuf=wt)
        if m in (0, 1):
            pt = psum[wi % 2]
            for ncn in range(8):
                for kc in range(8):
                    kb.op('pe', lambda e, ncn=ncn, kc=kc, wt=wt, pt=pt: e.matmul(
                        pt[:, ncn * 2:(ncn + 1) * 2], lhsT=wt[:, kc, ncn * 128:(ncn + 1) * 128],
                        rhs=cact[:, kc, :], start=(kc == 0), stop=(kc == 7)),
                        reads=[wt, cact], writes=[pt])
            mt = modT[m]
            kb.op('dve', lambda e, pt=pt, mt=mt, m=m: e.tensor_tensor(
                out=mt[:], in0=pt[:, 0:16].rearrange("p (a b) -> p a b", b=NSEQ),
                in1=badT[:, m * 8:(m + 1) * 8].unsqueeze(2).to_broadcast([128, 8, NSEQ]), op=ALU.add),
                reads=[pt, badT], writes=[mt])
            if m == 1:
                kb.op('dve', lambda e, mt=mt: e.tensor_scalar(out=mt[:], in0=mt[:], scalar1=1.0, scalar2=None,
                                                             op0=ALU.add), reads=[mt], writes=[mt])
        else:
            brow = kb.sb(f"brow{m}", [128, 1024], F32)
            kb.dma('sp', brow[:], bcast_rows(b_ada_d[0:1, m * 1024:(m + 1) * 1024]), writes=[brow], sembuf=brow)
            for s in range(NSEQ):
                gb = gate_b[(m, s)]
                for half in range(2):
                    pt = psum[2 + half]
                    for kc in range(8):
                        kb.op('pe', lambda e, kc=kc, wt=wt, pt=pt, s=s, half=half: e.matmul(
                            pt[:, :], lhsT=cbc[:, s, kc, :], rhs=wt[:, kc, half * 512:(half + 1) * 512],
                            start=(kc == 0), stop=(kc == 7)), reads=[wt, cbc], writes=[pt])
                    kb.op('dve', lambda e, pt=pt, gb=gb, half=half, brow=brow: e.tensor_tensor(
                        out=gb[:, half * 512:(half + 1) * 512], in0=pt[:, :],
                        in1=brow[:, half * 512:(half + 1) * 512], op=ALU.add),
                        reads=[pt, brow], writes=[gb])
                if m == 4:
                    kb.op('dve', lambda e, gb=gb: e.tensor_scalar(out=gb[:], in0=gb[:], scalar1=1.0, scalar2=None,
                                                                 op0=ALU.add), reads=[gb], writes=[gb])

    ph0.__exit__(None, None, None)

    phA = kb.phase()
    phA.__enter__()
    w_in_bf = kb.sb("w_in_bf", [128, 8, INC], BF16)
    for kc in range(8):
        for hf in range(2):
            kb.dma('pool', w_in_bf[:, kc, hf * 1540:(hf + 1) * 1540],
                   w_in_d[kc * 128:(kc + 1) * 128, hf * 1540:(hf + 1) * 1540],
                   writes=[w_in_bf], sembuf=w_in_bf)
    xp = [kb.sb(f"xp{i}", [128, D], F32) for i in range(3)]
    junk = kb.sb("junk", [128, D], BF16)
    xn = [kb.sb(f"xn{i}", [128, D], BF16) for i in range(2)]
    ssq = [kb.sb(f"ssq{i}", [128, 1], F32) for i in range(2)]
    std = [kb.sb(f"std{i}", [128, 1], F32) for i in range(2)]
    rstd = [kb.sb(f"rstd{i}", [128, 1], F32) for i in range(2)]
    hT = [kb.sb(f"hT{i}", [128, 8, 512], BF16) for i in range(2)]
    qkst = [kb.sb(f"qkst{i}", [128, 16, 512], BF16) for i in range(2)]
    vst = [kb.sb(f"vst{i}", [128, 4, 1024], BF16) for i in range(2)]
    ef = [kb.sb(f"ef{i}", [8, 512], F32) for i in range(2)]
    lf = [kb.sb(f"lf{i}", [8, 512], F32) for i in range(2)]
    Gt = [kb.sb(f"Gt{i}", [8, 512], F32) for i in range(2)]
    r1 = kb.sb("r1", [8, 512], F32)
    r2 = kb.sb("r2", [8, 512], F32)
    gpt = [kb.sb(f"gpt{i}", [8, 3, 512], BF16) for i in range(2)]
    ones8 = kb.sb("ones8", [8, 512], F32)
    kb.op('dve', lambda e: e.memset(ones8[:], 1.0), writes=[ones8])
    carry = kb.sb("carry", [8, 1], F32)

    qk_cols = [0 + 128 * i for i in range(4)] + [512 + 128 * i for i in range(4)] + \
              [1544 + 128 * i for i in range(4)] + [2056 + 128 * i for i in range(4)]
    qk_isq = [True] * 4 + [False] * 4 + [True] * 4 + [False] * 4
    st = {"tcount": 0, "evac": 0}

    def A1(gi):
        s, g = gi // NG, gi % NG
        gcount = gi
        tcount = gi * 4
        hTg = hT[gcount % 2]
        for j in range(4):
            t = g * 4 + j
            xt = xp[tcount % 3]
            i2 = tcount % 2
            kb.dma('sp', xt[:], x_in[s, t * 128:(t + 1) * 128, :], writes=[xt], sembuf=xt)
            kb.op('act', lambda e, xt=xt, i2=i2: e.activation(out=junk[:], in_=xt[:], func=AF.Square,
                                                               accum_out=ssq[i2][:]),
                  reads=[xt], writes=[junk, ssq[i2]])
            kb.op('act', lambda e, i2=i2: e.activation(out=std[i2][:], in_=ssq[i2][:], func=AF.Sqrt,
                                                       scale=1.0 / D, bias=epsc[:]),
                  reads=[ssq[i2], epsc], writes=[std[i2]])
            kb.op('dve', lambda e, i2=i2: e.reciprocal(out=rstd[i2][:], in_=std[i2][:]),
                  reads=[std[i2]], writes=[rstd[i2]])
            kb.op('act', lambda e, xt=xt, i2=i2: e.activation(out=xn[i2][:], in_=xt[:], func=AF.Identity,
                                                               scale=rstd[i2][:]),
                  reads=[xt, rstd[i2]], writes=[xn[i2]])
            tp = psum[tcount % 2]
            tpv = tp[:].bitcast(BF16)
            for kc in range(8):
                kb.op('pe', lambda e, kc=kc, i2=i2, tpv=tpv: e.transpose(
                    out=tpv[:, kc * 128:(kc + 1) * 128], in_=xn[i2][:, kc * 128:(kc + 1) * 128],
                    identity=ident[:]), reads=[xn[i2], ident], writes=[tp])
            for kc in range(8):
                kb.op('dve', lambda e, kc=kc, tpv=tpv, j=j, hTg=hTg, s=s: e.tensor_scalar(
                    out=hTg[:, kc, j * 128:(j + 1) * 128], in0=tpv[:, kc * 128:(kc + 1) * 128],
                    scalar1=modT[1][:, kc, s:s + 1], scalar2=modT[0][:, kc, s:s + 1],
                    op0=ALU.mult, op1=ALU.add), reads=[tp, modT[1], modT[0]], writes=[hTg])
            tcount += 1

    def A2(gi):
        nonlocal evac
        s, g = gi // NG, gi % NG
        gcount = gi
        hTg = hT[gcount % 2]
        if g == 0:
            kb.op('dve', lambda e: e.memset(carry[:], 0.0), writes=[carry])
        i2 = gcount % 2
        pf = psum[2]
        for kc in range(8):
            kb.op('pe', lambda e, kc=kc, pf=pf, hTg=hTg: e.matmul(
                pf[0:8, :], lhsT=w_in_bf[:, kc, 1536:1544], rhs=hTg[:, kc, :],
                start=(kc == 0), stop=(kc == 7)), reads=[w_in_bf, hTg], writes=[pf])
        kb.op('act', lambda e, pf=pf, i2=i2: e.activation(out=ef[i2][:], in_=pf[0:8, :], func=AF.Exp,
                                                           scale=-1.0, bias=bfg[:]),
              reads=[pf, bfg], writes=[ef[i2]])
        kb.op('act', lambda e, i2=i2: e.activation(out=lf[i2][:], in_=ef[i2][:], func=AF.Ln, bias=1.0),
              reads=[ef[i2]], writes=[lf[i2]])
        kb.op('dve', lambda e, i2=i2: e.tensor_tensor_scan(out=Gt[i2][:], data0=ones8[:], data1=lf[i2][:],
                                                            initial=carry[:], op0=ALU.mult, op1=ALU.add),
              reads=[ones8, lf[i2], carry], writes=[Gt[i2]])
        kb.op('dve', lambda e, i2=i2: e.tensor_copy(out=carry[:], in_=Gt[i2][:, 511:512]),
              reads=[Gt[i2]], writes=[carry])
        gp_ = gpt[i2]
        kb.op('dve', lambda e, i2=i2, gp_=gp_: e.tensor_copy(out=gp_[:, 0, :], in_=Gt[i2][:]),
              reads=[Gt[i2]], writes=[gp_])
        kb.op('dve', lambda e, i2=i2, gp_=gp_: e.tensor_tensor(out=r1[:], in0=Gt[i2][:], in1=gp_[:, 0, :],
                                                                op=ALU.subtract),
              reads=[Gt[i2], gp_], writes=[r1])
        kb.op('dve', lambda e, gp_=gp_: e.tensor_copy(out=gp_[:, 1, :], in_=r1[:]), reads=[r1], writes=[gp_])
        kb.op('dve', lambda e, gp_=gp_: e.tensor_tensor(out=r2[:], in0=r1[:], in1=gp_[:, 1, :],
                                                        op=ALU.subtract), reads=[r1, gp_], writes=[r2])
        kb.op('dve', lambda e, gp_=gp_: e.tensor_copy(out=gp_[:, 2, :], in_=r2[:]), reads=[r2], writes=[gp_])
        kb.dma('pool', GP[s][:, :, g * 512:(g + 1) * 512], gp_[:], reads=[gp_], writes=[GP[s]], sembuf=gp_)
        qs = qkst[gcount % 2]
        for ci in range(16):
            pq = psum[4 + (ci % 4)]
            c0 = qk_cols[ci]
            for kc in range(8):
                kb.op('pe', lambda e, kc=kc, pq=pq, c0=c0, hTg=hTg: e.matmul(
                    pq[:, :], lhsT=w_in_bf[:, kc, c0:c0 + 128], rhs=hTg[:, kc, :],
                    start=(kc == 0), stop=(kc == 7)), reads=[w_in_bf, hTg], writes=[pq])
            sc = 0.125 if qk_isq[ci] else 1.0
            if evac % 2 == 0:
                kb.op('act', lambda e, pq=pq, qs=qs, ci=ci, sc=sc: e.activation(
                    out=qs[:, ci, :], in_=pq[:, :], func=AF.Copy, scale=sc), reads=[pq], writes=[qs])
            else:
                kb.op('dve', lambda e, pq=pq, qs=qs, ci=ci, sc=sc: e.tensor_scalar(
                    out=qs[:, ci, :], in0=pq[:, :], scalar1=sc, scalar2=None, op0=ALU.mult),
                    reads=[pq], writes=[qs])
            evac += 1
        kb.dma('pool', QK[s][:, :, g * 512:(g + 1) * 512].rearrange("c p t -> p c t"), qs[:],
               reads=[qs], writes=[QK[s]], sembuf=qs)
        vs_ = vst[gcount % 2]
        for j in range(4):
            for hf, c0 in enumerate((1024, 2568)):
                pv = psum[2 + ((j * 2 + hf) % 2)] if False else psum[4 + ((j * 2 + hf) % 4)]
                for kc in range(8):
                    kb.op('pe', lambda e, kc=kc, pv=pv, c0=c0, hTg=hTg, j=j: e.matmul(
                        pv[:, :], lhsT=hTg[:, kc, j * 128:(j + 1) * 128], rhs=w_in_bf[:, kc, c0:c0 + 512],
                        start=(kc == 0), stop=(kc == 7)), reads=[w_in_bf, hTg], writes=[pv])
                if evac % 2 == 0:
                    kb.op('act', lambda e, pv=pv, vs_=vs_, j=j, hf=hf: e.activation(
                        out=vs_[:, j, hf * 512:(hf + 1) * 512], in_=pv[:, :], func=AF.Copy),
                        reads=[pv], writes=[vs_])
                else:
                    kb.op('dve', lambda e, pv=pv, vs_=vs_, j=j, hf=hf: e.tensor_copy(
                        out=vs_[:, j, hf * 512:(hf + 1) * 512], in_=pv[:, :]), reads=[pv], writes=[vs_])
                evac += 1
        kb.dma('pool', VV[s][g * 512:(g + 1) * 512, :].rearrange("(j p) n -> p j n", p=128), vs_[:],
               reads=[vs_], writes=[VV[s]], sembuf=vs_)

    evac = 0
    A1(0)
    for gi in range(NSEQ * NG):
        Aa = record(A1, gi + 1) if gi + 1 < NSEQ * NG else []
        Bb = record(A2, gi)
        emit_merged([Aa, Bb])
        precast(4)
    phA.__exit__(None, None, None)

    phB = kb.phase()
    phB.__enter__()
    tri = kb.sb("tri", [128, 128], BF16)
    kb.dma('sp', tri[:], tri_d[:], writes=[tri], sembuf=tri)
    Tt = kb.sb("Tt", [128, 24, 256], F32)
    rb = kb.sb("rb", [32, 8], F32)
    kb.dma('sp', rb[:], relb_d[:], writes=[rb], sembuf=rb)
    oh = kb.sb("oh", [32, 3, 384], F32)
    kb.dma('sp', oh[:], oh_d[:], writes=[oh], sembuf=oh)
    negm = kb.sb("negm", [8, 3, 384], F32)
    kb.dma('sp', negm[:], negm_d[:], writes=[negm], sembuf=negm)
    Jm = kb.sb("Jm", [128, 128], F32)
    kb.dma('sp', Jm[:], J_d[:], writes=[Jm], sembuf=Jm)
    extsb = kb.sb("extsb", [8, 3, 384], F32)
    for di in range(3):
        pt = psum[di % 2]
        kb.op('pe', lambda e, di=di, pt=pt: e.matmul(pt[0:8, 0:384], lhsT=rb[:, :], rhs=oh[:, di, :],
                                                      start=True, stop=True), reads=[rb, oh], writes=[pt])
        kb.op('dve', lambda e, di=di, pt=pt: e.tensor_tensor(out=extsb[:, di, :], in0=pt[0:8, 0:384],
                                                              in1=negm[:, di, :], op=ALU.add),
              reads=[pt, negm], writes=[extsb])
    kb.dma('sp', EXT[:], extsb[:], reads=[extsb], writes=[EXT], sembuf=extsb)
    hk = [kb.sb(f"hk{i}", [128, 256], F32) for i in range(2)]
    for di in range(3):
        for h in range(8):
            hkt = hk[(di * 8 + h) % 2]
            kb.dma('sp', hkt[:], bass.AP(EXT.t, (h * 3 + di) * 384, [[1, 128], [1, 256]]),
                   reads=[EXT], writes=[hkt], sembuf=hkt)
            pt = psum[(di * 8 + h) % 2]
            kb.op('pe', lambda e, hkt=hkt, pt=pt: e.matmul(pt[:, 0:256], lhsT=Jm[:, :], rhs=hkt[:, :],
                                                            start=True, stop=True), reads=[Jm, hkt], writes=[pt])
            kb.op('dve', lambda e, pt=pt, di=di, h=h: e.tensor_copy(out=Tt[:, di * 8 + h, :], in_=pt[:, 0:256]),
                  reads=[pt], writes=[Tt])

    KT = [kb.sb(f"KT{i}", [70, S], BF16) for i in range(2)]
    QT = [kb.sb(f"QT{i}", [70, S], BF16) for i in range(2)]
    KTd = [kb.sb(f"KTd{i}", [64, S], BF16) for i in range(2)]
    QTd = [kb.sb(f"QTd{i}", [64, S], BF16) for i in range(2)]
    Vf = [kb.sb(f"Vf{i}", [128, 32, 65], BF16) for i in range(2)]
    _vd = [kb.sb(f"Vd_{di}", [128, 32, 65], BF16) for di in range(3)]
    Vd = [_vd, _vd]
    for i in range(2):
        kb.op('pool', lambda e, i=i: e.memset(KT[i][64:70, :], -1.0), writes=[KT[i]])
        kb.op('pool', lambda e, i=i: e.memset(QT[i][64:70, :], 1.0), writes=[QT[i]])
        kb.op('pool', lambda e, i=i: e.memset(Vf[i][:, :, 64:65], 1.0), writes=[Vf[i]])
        if i == 0:
            for di in range(3):
                kb.op('pool', lambda e, i=i, di=di: e.memset(Vd[i][di][:, :, 64:65], 1.0), writes=[Vd[i][di]])
    Pt = [kb.sb(f"Pt{i}", [128, 512], BF16) for i in range(3)]
    Ptd = [kb.sb(f"Ptd{i}", [128, 256], BF16) for i in range(3)]
    Ssb = [kb.sb(f"Ssb{i}", [128, 256], F32) for i in range(3)]
    yst = [kb.sb(f"yst{i}", [128, 4, 64], F32) for i in range(2)]
    rcf = [kb.sb(f"rcf{i}", [128, 1], F32) for i in range(4)]
    ost = [kb.sb(f"ost{i}", [128, 32, 65], F32) for i in range(2)]
    hcount = 0
    ycount = 0
    ocount = 0
    for s in range(NSEQ):
        for h in range(8):
            hb = hcount % 2
            hcount += 1
            po = (h % 2) * 64
            kt_, qt_, ktd, qtd, vf = KT[hb], QT[hb], KTd[hb], QTd[hb], Vf[hb]
            kb.dma('sp', kt_[0:64, :], QK[s][4 + h // 2, po:po + 64, :], reads=[QK[s]], writes=[kt_], sembuf=kt_)
            kb.dma('sp', kt_[67:70, :], GP[s][h, :, :], reads=[GP[s]], writes=[kt_], sembuf=kt_)
            kb.dma('sp', qt_[0:64, :], QK[s][h // 2, po:po + 64, :], reads=[QK[s]], writes=[qt_], sembuf=qt_)
            kb.dma('sp', qt_[64:67, :], GP[s][h, :, :], reads=[GP[s]], writes=[qt_], sembuf=qt_)
            kb.dma('sp', vf[:, :, 0:64], VV[s][:, h * 64:(h + 1) * 64].rearrange("(n p) e -> p n e", p=128),
                   reads=[VV[s]], writes=[vf], sembuf=vf)
            kb.dma('sp', ktd[:, :], QK[s][12 + h // 2, po:po + 64, :], reads=[QK[s]], writes=[ktd], sembuf=ktd)
            kb.dma('sp', qtd[:, :], QK[s][8 + h // 2, po:po + 64, :], reads=[QK[s]], writes=[qtd], sembuf=qtd)
            for di, (win, d) in enumerate(DIL):
                NB = 32 // d
                vdt = Vd[hb][di]
                src = VV[s][:, 512 + h * 64:512 + (h + 1) * 64].rearrange("(n p r) e -> r p n e", p=128, r=d)
                for r in range(d):
                    kb.dma('sp', vdt[:, r * NB:(r + 1) * NB, 0:64], src[r], reads=[VV[s]], writes=[vdt], sembuf=vdt)
            def FOX():
                nonlocal ycount
                for qg in range(8):
                    nkt = 4 * (qg + 1)
                    Oq = [psum[4 + i] for i in range(4)]

                    def fox_s(kt, qg=qg):
                        j = kt - 4 * qg
                        c_lo = 128 * j if j > 0 else 0
                        Sp = psum[kt % 2]
                        Pk = Pt[kt % 3]
                        kb.op('pe', lambda e: e.matmul(
                            Sp[:, c_lo:512], lhsT=kt_[0:70, kt * 128:(kt + 1) * 128],
                            rhs=qt_[0:70, qg * 512 + c_lo:(qg + 1) * 512], start=True, stop=True),
                            reads=[kt_, qt_], writes=[Sp])
                        kb.op('act', lambda e: e.activation(
                            out=Pk[:, c_lo:512], in_=Sp[:, c_lo:512], func=AF.Exp), reads=[Sp], writes=[Pk])
                        if j >= 0:
                            kb.op('dve', lambda e: e.tensor_tensor(
                                out=Pk[:, c_lo:c_lo + 128], in0=Pk[:, c_lo:c_lo + 128], in1=tri[:, :], op=ALU.mult),
                                reads=[Pk, tri], writes=[Pk])

                    def fox_pv(kt, qg=qg, Oq=Oq):
                        j = kt - 4 * qg
                        Pk = Pt[kt % 3]
                        for i in range(max(j, 0), 4):
                            kb.op('pe', lambda e, i=i: e.matmul(
                                Oq[i][:, 0:65], lhsT=Pk[:, i * 128:(i + 1) * 128], rhs=vf[:, kt, :],
                                start=(kt == 0), stop=(kt == 4 * qg + i)), reads=[Pk, vf], writes=[Oq[i]])
                    for kt in range(nkt):
                        fox_s(kt)
                        if kt > 0:
                            fox_pv(kt - 1)
                    fox_pv(nkt - 1)
                    ys = yst[ycount % 2]
                    ycount += 1
                    for i in range(4):
                        kb.op('dve', lambda e, i=i: e.reciprocal(out=rcf[i][:], in_=Oq[i][:, 64:65]),
                              reads=[Oq[i]], writes=[rcf[i]])
                        kb.op('dve', lambda e, i=i, ys=ys: e.tensor_scalar(
                            out=ys[:, i, :], in0=Oq[i][:, 0:64], scalar1=rcf[i][:], scalar2=None, op0=ALU.mult),
                            reads=[Oq[i], rcf[i]], writes=[ys])
                    kb.dma('pool', YF[s][qg * 512:(qg + 1) * 512, h * 64:(h + 1) * 64].rearrange("(i p) e -> p i e", p=128),
                           ys[:], reads=[ys], writes=[YF[s]], sembuf=ys)
                    if qg % 2 == 0:
                        precast(1)

            def DILF():
                nonlocal ocount
                units = []
                for di, (win, d) in enumerate(DIL):
                    NB = 32 // d
                    for r in range(d):
                        for n in range(NB):
                            units.append((di, d, NB, r, n))
                OdB = psum[3]
                ostmap = {}

                def dil_s(ui, u):
                    di, d, NB, r, n = u
                    ncol = 256 if n < NB - 1 else 128
                    st = n * 128 * d + r
                    Sp = psum[2]
                    Sb = Ssb[ui % 3]
                    Pk = Ptd[ui % 3]
                    kb.op('pe', lambda e: e.matmul(
                        Sp[:, 0:ncol], lhsT=ktd[0:64, st:st + 127 * d + 1:d], rhs=qtd[0:64, st:st + (ncol - 1) * d + 1:d],
                        start=True, stop=True), reads=[ktd, qtd], writes=[Sp])
                    kb.op('dve', lambda e: e.tensor_tensor(
                        out=Sb[:, 0:ncol], in0=Sp[:, 0:ncol], in1=Tt[:, di * 8 + h, 0:ncol], op=ALU.add),
                        reads=[Sp, Tt], writes=[Sb])
                    kb.op('act', lambda e: e.activation(
                        out=Pk[:, 0:ncol], in_=Sb[:, 0:ncol], func=AF.Exp), reads=[Sb], writes=[Pk])

                def dil_pv(ui, u):
                    nonlocal ocount
                    di, d, NB, r, n = u
                    Pk = Ptd[ui % 3]
                    vdt = Vd[hb][di]
                    if n == 0:
                        ostmap[(di, r)] = ost[ocount % 2]
                        ocount += 1
                    os_ = ostmap[(di, r)]
                    kb.op('pe', lambda e: e.matmul(
                        OdB[:, (n % 2) * 128:(n % 2) * 128 + 65], lhsT=Pk[:, 0:128], rhs=vdt[:, r * NB + n, :],
                        start=(n == 0), stop=True, skip_group_check=True), reads=[Pk, vdt], writes=[OdB])
                    if n < NB - 1:
                        kb.op('pe', lambda e: e.matmul(
                            OdB[:, ((n + 1) % 2) * 128:((n + 1) % 2) * 128 + 65], lhsT=Pk[:, 128:256],
                            rhs=vdt[:, r * NB + n, :], start=True, stop=False, skip_group_check=True),
                            reads=[Pk, vdt], writes=[OdB])
                    kb.op('act', lambda e: e.activation(
                        out=os_[:, n, :], in_=OdB[:, (n % 2) * 128:(n % 2) * 128 + 65], func=AF.Copy), reads=[OdB], writes=[os_])
                    if n == NB - 1:
                        dstv = OD[s][di, :, h, :].rearrange("(n p r) e -> r p n e", p=128, r=d)
                        kb.dma('pool', dstv[r], os_[:, 0:NB, :], reads=[os_], writes=[OD[s]], sembuf=os_)
                        if r % 2 == 0:
                            precast(1)
                for ui, u in enumerate(units):
                    dil_s(ui, u)
                    if ui > 0:
                        dil_pv(ui - 1, units[ui - 1])
                dil_pv(len(units) - 1, units[-1])
            emit_merged([record(FOX), record(DILF)])
    phB.__exit__(None, None, None)

    wts_all = kb.sb("wts_all", [128, NTILE, 8], F32)
    dest_all = kb.sb("dest_all", [128, NTILE, 8], I32)
    cnt_b = kb.sb("cnt_b", [128, NE], F32)
    idx_all = kb.sb("idx_all", [128, NBLK], I32)
    idx4_all = kb.sb("idx4_all", [128, NBLK], I32)
    ones_bf = kb.sb("ones_bf", [128, 128], BF16)
    kb.op('dve', lambda e: e.memset(ones_bf[:], 1.0), writes=[ones_bf])
    BIG = 1.0e4
    phCD = kb.phase()
    phCD.__enter__()
    M_all = kb.sb("M_all", [128, NTILE, NE], BF16)
    i8f_all = kb.sb("i8f_all", [128, NTILE, 8], F32)

    phC = kb.phase()
    phC.__enter__()
    w_out_bf = kb.sb("w_out_bf", [128, 8, D], BF16)
    wr_bf = kb.sb("wr_bf", [128, 8, NE], BF16)
    wsgu_bf = kb.sb("wsgu_bf", [128, 8, 512], BF16)
    wsd_bf = kb.sb("wsd_bf", [128, 2, D], BF16)
    for kc in range(8):
        rs = slice(kc * 128, (kc + 1) * 128)
        kb.dma('pool', w_out_bf[:, kc, :], w_out_d[rs, :], writes=[w_out_bf], sembuf=w_out_bf)
        kb.dma('pool', wr_bf[:, kc, :], w_r_d[rs, :], writes=[wr_bf], sembuf=wr_bf)
        kb.dma('pool', wsgu_bf[:, kc, 0:256], wsg_d[rs, :], writes=[wsgu_bf], sembuf=wsgu_bf)
        kb.dma('pool', wsgu_bf[:, kc, 256:512], wsu_d[rs, :], writes=[wsgu_bf], sembuf=wsgu_bf)
    for hc in range(2):
        kb.dma('pool', wsd_bf[:, hc, :], wsd_d[hc * 128:(hc + 1) * 128, :], writes=[wsd_bf], sembuf=wsd_bf)
    gmix = kb.sb("gmix", [128, 8], F32)
    kb.dma('sp', gmix[:], gmix_in[:], writes=[gmix], sembuf=gmix)
    rbias_b = kb.sb("rbias_b", [128, NE], F32)
    kb.dma('sp', rbias_b[:], bcast_rows(rbias_d[0:1, :]), writes=[rbias_b], sembuf=rbias_b)
    ym = [kb.sb(f"ym{i}", [128, D], F32) for i in range(2)]
    odt = [kb.sb(f"odt{i}", [128, 3, 520], F32) for i in range(2)]
    xq = [kb.sb(f"xq{i}", [128, D], F32) for i in range(3)]
    osum = kb.sb("osum", [128, 520], F32)
    rcd = kb.sb("rcd", [128, 8], F32)
    junkc = kb.sb("junkc", [128, D], BF16)
    junkc2 = kb.sb("junkc2", [128, D], BF16)
    ss2 = kb.sb("ss2", [128, 2], F32)
    sd2 = kb.sb("sd2", [128, 2], F32)
    rs2 = kb.sb("rs2", [128, 2], F32)
    ymn = kb.sb("ymn", [128, D], BF16)
    ymT = kb.sb("ymT", [128, 8, 128], BF16)
    tmpc = kb.sb("tmpc", [128, 512], F32)
    tmpc2 = kb.sb("tmpc2", [128, 512], F32)
    ss1 = kb.sb("ss1", [128, 1], F32)
    sd1 = kb.sb("sd1", [128, 1], F32)
    rs1 = kb.sb("rs1", [128, 1], F32)
    h2f = kb.sb("h2f", [128, D], F32)
    h2tok = [kb.sb(f"h2tok{i}", [128, D], BF16) for i in range(2)]
    h2Ts = [kb.sb(f"h2T{i}", [128, 8, 128], BF16) for i in range(2)]
    scores = kb.sb("scores", [128, NE], F32)
    sel = kb.sb("sel", [128, NE], F32)
    msel = kb.sb("msel", [128, NE], F32)
    wd_ = kb.sb("wd_", [128, NE], F32)
    m8 = kb.sb("m8", [128, 8, 8], F32)
    grp = kb.sb("grp", [128, 8], F32)
    g8 = kb.sb("g8", [128, 8], F32)
    gm = kb.sb("gm", [128, 8], F32)
    pen = kb.sb("pen", [128, 8], F32)
    t8 = kb.sb("t8", [128, 8], F32)
    w8 = kb.sb("w8", [128, 8], F32)
    i8u = kb.sb("i8u", [128, 8], U32)
    wsum = kb.sb("wsum", [128, 1], F32)
    rws = kb.sb("rws", [128, 1], F32)
    sg = kb.sb("sg", [128, 256], F32)
    actT = kb.sb("actT", [128, 256], BF16)
    kb.op('dve', lambda e: e.memset(cnt_b[:], 0.0), writes=[cnt_b])
    def S1a(tg):
        s = tg // NT
        t = tg % NT
        rows = slice(t * 128, (t + 1) * 128)
        grow = slice(tg * 128, (tg + 1) * 128)
        i2 = tg % 2
        h2T_ = h2Ts[i2]
        ymt, odd, xt = ym[i2], odt[i2], xq[tg % 3]
        kb.dma('sp', ymt[:, 0:512], YF[s][rows, :], reads=[YF[s]], writes=[ymt], sembuf=ymt)
        kb.dma('sp', odd[:], OD[s][:, rows, :, :].rearrange("d t h e -> t d (h e)"), reads=[OD[s]], writes=[odd],
               sembuf=odd)
        kb.dma('sp', xt[:], x_in[s, rows, :], writes=[xt], sembuf=xt)
        kb.op('dve', lambda e, odd=odd: e.tensor_tensor(out=osum[:], in0=odd[:, 0, :], in1=odd[:, 1, :], op=ALU.add),
              reads=[odd], writes=[osum])
        kb.op('dve', lambda e, odd=odd: e.tensor_tensor(out=osum[:], in0=osum[:], in1=odd[:, 2, :], op=ALU.add),
              reads=[odd, osum], writes=[osum])
        osv = osum[:].rearrange("p (h e) -> p h e", e=65)
        kb.op('dve', lambda e, osv=osv: e.reciprocal(out=rcd[:], in_=osv[:, :, 64]), reads=[osum], writes=[rcd])
        kb.op('dve', lambda e, osv=osv, ymt=ymt: e.tensor_tensor(
            out=ymt[:, 512:1024].rearrange("p (h e) -> p h e", e=64), in0=osv[:, :, 0:64],
            in1=rcd[:].unsqueeze(2).to_broadcast([128, 8, 64]), op=ALU.mult), reads=[osum, rcd], writes=[ymt])
        for hf in range(2):
            kb.op('act', lambda e, hf=hf, ymt=ymt: e.activation(
                out=junkc[:, 0:512], in_=ymt[:, hf * 512:(hf + 1) * 512], func=AF.Square,
                accum_out=ss2[:, hf:hf + 1]), reads=[ymt], writes=[junkc, ss2])
        kb.op('act', lambda e: e.activation(out=sd2[:], in_=ss2[:], func=AF.Sqrt, scale=1.0 / 512, bias=epsc[:]),
              reads=[ss2, epsc], writes=[sd2])
        kb.op('dve', lambda e: e.reciprocal(out=rs2[:], in_=sd2[:]), reads=[sd2], writes=[rs2])
        for hf in range(2):
            kb.op('act', lambda e, hf=hf, ymt=ymt: e.activation(
                out=ymn[:, hf * 512:(hf + 1) * 512], in_=ymt[:, hf * 512:(hf + 1) * 512], func=AF.Identity,
                scale=rs2[:, hf:hf + 1]), reads=[ymt, rs2], writes=[ymn])
        tp = psum[0]
        tpv = tp[:].bitcast(BF16)
        for kc in range(8):
            kb.op('pe', lambda e, kc=kc, tpv=tpv: e.transpose(
                out=tpv[:, kc * 128:(kc + 1) * 128], in_=ymn[:, kc * 128:(kc + 1) * 128], identity=ident[:]),
                reads=[ymn, ident], writes=[tp])
        kb.op('dve', lambda e, tpv=tpv: e.tensor_tensor(
            out=ymT[:], in0=tpv.rearrange("p (k t) -> p k t", t=128),
            in1=gmix[:].unsqueeze(2).to_broadcast([128, 8, 128]), op=ALU.mult), reads=[tp, gmix], writes=[ymT])
        for hf in range(2):
            po_ = psum[4 + hf]
            for kc in range(8):
                kb.op('pe', lambda e, kc=kc, hf=hf, po_=po_: e.matmul(
                    po_[:, :], lhsT=ymT[:, kc, :], rhs=w_out_bf[:, kc, hf * 512:(hf + 1) * 512],
                    start=(kc == 0), stop=(kc == 7)), reads=[ymT, w_out_bf], writes=[po_])
            g1 = gate_b[(2, s)]
            kb.op('dve', lambda e, hf=hf, po_=po_, g1=g1: e.tensor_tensor(
                out=tmpc[:], in0=po_[:, :], in1=g1[:, hf * 512:(hf + 1) * 512], op=ALU.mult),
                reads=[po_, g1], writes=[tmpc])
            kb.op('dve', lambda e, hf=hf, xt=xt: e.tensor_tensor(
                out=xt[:, hf * 512:(hf + 1) * 512], in0=xt[:, hf * 512:(hf + 1) * 512], in1=tmpc[:], op=ALU.add),
                reads=[xt, tmpc], writes=[xt])

    def S1b(tg):
        s = tg // NT
        grow = slice(tg * 128, (tg + 1) * 128)
        i2 = tg % 2
        xt = xq[tg % 3]
        h2T_ = h2Ts[i2]
        kb.op('act', lambda e, xt=xt: e.activation(out=junkc2[:], in_=xt[:], func=AF.Square, accum_out=ss1[:]),
              reads=[xt], writes=[junkc2, ss1])
        kb.op('act', lambda e: e.activation(out=sd1[:], in_=ss1[:], func=AF.Sqrt, scale=1.0 / D, bias=epsc[:]),
              reads=[ss1, epsc], writes=[sd1])
        kb.op('dve', lambda e: e.reciprocal(out=rs1[:], in_=sd1[:]), reads=[sd1], writes=[rs1])
        sc2, sh2 = gate_b[(4, s)], gate_b[(3, s)]
        kb.op('dve', lambda e, xt=xt, sc2=sc2: e.scalar_tensor_tensor(
            out=h2f[:], in0=xt[:], scalar=rs1[:], in1=sc2[:], op0=ALU.mult, op1=ALU.mult),
            reads=[xt, rs1, sc2], writes=[h2f])
        h2t = h2tok[i2]
        kb.op('dve', lambda e, h2t=h2t, sh2=sh2: e.tensor_tensor(out=h2t[:], in0=h2f[:], in1=sh2[:], op=ALU.add),
              reads=[h2f, sh2], writes=[h2t])
        kb.dma('pool', H2[grow, :], h2t[:], reads=[h2t], writes=[H2], sembuf=h2t)
        tp2 = psum[7]
        tp2v = tp2[:].bitcast(BF16)
        for kc in range(8):
            kb.op('pe', lambda e, kc=kc, tp2v=tp2v, h2t=h2t: e.transpose(
                out=tp2v[:, kc * 128:(kc + 1) * 128], in_=h2t[:, kc * 128:(kc + 1) * 128], identity=ident[:]),
                reads=[h2t, ident], writes=[tp2])
        kb.op('act', lambda e, tp2v=tp2v: e.activation(out=h2T_[:].rearrange("p k t -> p (k t)"), in_=tp2v,
                                                       func=AF.Copy), reads=[tp2], writes=[h2T_])

    def S2(tg):
        s = tg // NT
        grow = slice(tg * 128, (tg + 1) * 128)
        i2 = tg % 2
        xt = xq[tg % 3]
        h2T_ = h2Ts[i2]
        pr = psum[3]
        for kc in range(8):
            kb.op('pe', lambda e, kc=kc, pr=pr: e.matmul(pr[:, 0:NE], lhsT=h2T_[:, kc, :], rhs=wr_bf[:, kc, :],
                                                         start=(kc == 0), stop=(kc == 7)),
                  reads=[h2T_, wr_bf], writes=[pr])
        kb.op('act', lambda e, pr=pr: e.activation(out=scores[:], in_=pr[:, 0:NE], func=AF.Sigmoid),
              reads=[pr], writes=[scores])
        kb.op('dve', lambda e: e.tensor_tensor(out=sel[:], in0=scores[:], in1=rbias_b[:], op=ALU.add),
              reads=[scores, rbias_b], writes=[sel])
        for g in range(8):
            kb.op('dve', lambda e, g=g: e.max(out=m8[:, g, :], in_=sel[:, g * 32:(g + 1) * 32]),
                  reads=[sel], writes=[m8])
        kb.op('dve', lambda e: e.tensor_tensor(out=grp[:], in0=m8[:, :, 0], in1=m8[:, :, 1], op=ALU.add),
              reads=[m8], writes=[grp])
        kb.op('dve', lambda e: e.max(out=g8[:], in_=grp[:]), reads=[grp], writes=[g8])
        kb.op('dve', lambda e: e.tensor_scalar(out=gm[:], in0=grp[:], scalar1=g8[:, 3:4], scalar2=None,
                                               op0=ALU.is_ge), reads=[grp, g8], writes=[gm])
        kb.op('dve', lambda e: e.tensor_scalar(out=pen[:], in0=gm[:], scalar1=BIG, scalar2=-BIG, op0=ALU.mult,
                                               op1=ALU.add), reads=[gm], writes=[pen])
        kb.op('dve', lambda e: e.tensor_tensor(
            out=msel[:].rearrange("p (g e) -> p g e", e=32), in0=sel[:].rearrange("p (g e) -> p g e", e=32),
            in1=gm[:].unsqueeze(2).to_broadcast([128, 8, 32]), op=ALU.mult), reads=[sel, gm], writes=[msel])
        kb.op('dve', lambda e: e.tensor_tensor(
            out=msel[:].rearrange("p (g e) -> p g e", e=32), in0=msel[:].rearrange("p (g e) -> p g e", e=32),
            in1=pen[:].unsqueeze(2).to_broadcast([128, 8, 32]), op=ALU.add), reads=[msel, pen], writes=[msel])
        kb.op('dve', lambda e: e.max(out=t8[:], in_=msel[:]), reads=[msel], writes=[t8])
        kb.op('dve', lambda e, tg=tg: e.tensor_scalar(out=M_all[:, tg, :], in0=msel[:], scalar1=t8[:, 7:8],
                                                      scalar2=None, op0=ALU.is_ge),
              reads=[msel, t8], writes=[M_all])
        kb.op('dve', lambda e, tg=tg: e.tensor_tensor(out=wd_[:], in0=scores[:], in1=M_all[:, tg, :], op=ALU.mult),
              reads=[scores, M_all], writes=[wd_])
        kb.op('dve', lambda e: e.max(out=w8[:], in_=wd_[:]), reads=[wd_], writes=[w8])
        kb.op('dve', lambda e: e.max_index(out=i8u[:], in_max=w8[:], in_values=wd_[:]), reads=[w8, wd_],
              writes=[i8u])
        kb.op('dve', lambda e: e.tensor_reduce(out=wsum[:], in_=w8[:], axis=mybir.AxisListType.X, op=ALU.add),
              reads=[w8], writes=[wsum])
        kb.op('dve', lambda e: e.reciprocal(out=rws[:], in_=wsum[:]), reads=[wsum], writes=[rws])
        kb.op('dve', lambda e, tg=tg: e.tensor_scalar(out=wts_all[:, tg, :], in0=w8[:], scalar1=rws[:],
                                                      scalar2=2.5, op0=ALU.mult, op1=ALU.mult),
              reads=[w8, rws], writes=[wts_all])
        kb.op('dve', lambda e, tg=tg: e.tensor_copy(out=i8f_all[:, tg, :], in_=i8u[:]), reads=[i8u],
              writes=[i8f_all])
        kb.op('pe', lambda e, tg=tg, pr=pr: e.matmul(pr[:, 256:512], lhsT=ones_bf[:, :], rhs=M_all[:, tg, :],
                                                     start=True, stop=True),
              reads=[ones_bf, M_all], writes=[pr])
        kb.op('dve', lambda e, pr=pr: e.tensor_tensor(out=cnt_b[:], in0=cnt_b[:], in1=pr[:, 256:512], op=ALU.add),
              reads=[pr, cnt_b], writes=[cnt_b])
        pg = psum[6]
        for gi in range(4):
            for kc in range(8):
                kb.op('pe', lambda e, gi=gi, kc=kc, pg=pg: e.matmul(
                    pg[:, gi * 128:(gi + 1) * 128], lhsT=wsgu_bf[:, kc, gi * 128:(gi + 1) * 128], rhs=h2T_[:, kc, :],
                    start=(kc == 0), stop=(kc == 7)), reads=[wsgu_bf, h2T_], writes=[pg])
        kb.op('act', lambda e, pg=pg: e.activation(out=sg[:], in_=pg[:, 0:256], func=AF.Silu), reads=[pg],
              writes=[sg])
        kb.op('dve', lambda e, pg=pg: e.tensor_tensor(out=actT[:], in0=sg[:], in1=pg[:, 256:512], op=ALU.mult),
              reads=[sg, pg], writes=[actT])
        for hf in range(2):
            po_ = psum[1 + hf]
            for hc in range(2):
                kb.op('pe', lambda e, hc=hc, hf=hf, po_=po_: e.matmul(
                    po_[:, :], lhsT=actT[:, hc * 128:(hc + 1) * 128], rhs=wsd_bf[:, hc, hf * 512:(hf + 1) * 512],
                    start=(hc == 0), stop=(hc == 1)), reads=[actT, wsd_bf], writes=[po_])
            g2 = gate_b[(5, s)]
            kb.op('dve', lambda e, hf=hf, po_=po_, g2=g2: e.tensor_tensor(
                out=tmpc2[:], in0=po_[:, :], in1=g2[:, hf * 512:(hf + 1) * 512], op=ALU.mult),
                reads=[po_, g2], writes=[tmpc2])
            kb.op('dve', lambda e, hf=hf, xt=xt: e.tensor_tensor(
                out=xt[:, hf * 512:(hf + 1) * 512], in0=xt[:, hf * 512:(hf + 1) * 512], in1=tmpc2[:], op=ALU.add),
                reads=[xt, tmpc2], writes=[xt])
        kb.dma('pool', X2[grow, :], xt[:], reads=[xt], writes=[X2], sembuf=xt)
    S1a(0)
    S1a(1)
    S1b(0)
    for tg in range(NTILE):
        A = record(S1a, tg + 2) if tg + 2 < NTILE else []
        B = record(S1b, tg + 1) if tg + 1 < NTILE else []
        C = record(S2, tg)
        emit_merged([A, B, C])
        precast(2)
    precast(1000)
    phC.__exit__(None, None, None)

    phD = kb.phase()
    phD.__enter__()
    lst = kb.sb("lst", [128, 128], BF16)
    kb.dma('sp', lst[:], lst_d[:], writes=[lst], sembuf=lst)
    iota = kb.sb("iota", [128, NE], F32)
    kb.dma('sp', iota[:], iota_d[:], writes=[iota], sembuf=iota)
    bvals = kb.sb("bvals", [128, 6], F32)
    kb.dma('sp', bvals[:], bvals_d[:], writes=[bvals], sembuf=bvals)
    cnt_i = kb.sb("cnt_i", [128, NE], I32)
    nb_i = kb.sb("nb_i", [128, NE], I32)
    nb_f = kb.sb("nb_f", [128, NE], F32)
    Cc = kb.sb("Cc", [128, NE], F32)
    ones256 = kb.sb("ones256", [128, NE], F32)
    base = kb.sb("base", [128, NE], F32)
    junkd = kb.sb("junkd", [128, NE], F32)
    be_f = kb.sb("be_f", [128, 6], F32)
    be_i = kb.sb("be_i", [128, 6], I32)
    Dm = kb.sb("Dm", [128, NE], F32)
    dest_f = kb.sb("dest_f", [128, 8], F32)
    h2s = [kb.sb(f"h2s{i}", [128, D], BF16) for i in range(3)]
    kb.op('dve', lambda e: e.memset(ones256[:], 1.0), writes=[ones256])
    kb.op('dve', lambda e: e.tensor_copy(out=cnt_i[:], in_=cnt_b[:]), reads=[cnt_b], writes=[cnt_i])
    kb.op('dve', lambda e: e.tensor_scalar(out=cnt_i[:], in0=cnt_i[:], scalar1=BLK - 1, scalar2=None, op0=ALU.add),
          reads=[cnt_i], writes=[cnt_i])
    kb.op('dve', lambda e: e.tensor_scalar(out=nb_i[:], in0=cnt_i[:], scalar1=8, scalar2=None,
                                           op0=ALU.arith_shift_right), reads=[cnt_i], writes=[nb_i])
    kb.op('dve', lambda e: e.tensor_copy(out=nb_f[:], in_=nb_i[:]), reads=[nb_i], writes=[nb_f])
    kb.op('dve', lambda e: e.tensor_tensor_scan(out=Cc[:], data0=ones256[:], data1=nb_f[:], initial=0.0,
                                                op0=ALU.mult, op1=ALU.add), reads=[ones256, nb_f], writes=[Cc])
    kb.op('dve', lambda e: e.tensor_tensor(out=base[:], in0=Cc[:], in1=nb_f[:], op=ALU.subtract),
          reads=[Cc, nb_f], writes=[base])
    kb.op('dve', lambda e: e.tensor_scalar(out=base[:], in0=base[:], scalar1=float(BLK), scalar2=None, op0=ALU.mult),
          reads=[base], writes=[base])
    for tg in range(NTILE):
        pp = psum[tg % 2]
        pc = psum[2 + tg % 2]
        kb.op('pe', lambda e, tg=tg, pp=pp: e.matmul(pp[:, 0:NE], lhsT=lst[:, :], rhs=M_all[:, tg, :],
                                                     start=True, stop=True), reads=[lst, M_all], writes=[pp])
        kb.op('pe', lambda e, tg=tg, pc=pc: e.matmul(pc[:, 0:NE], lhsT=ones_bf[:, :], rhs=M_all[:, tg, :],
                                                     start=True, stop=True), reads=[ones_bf, M_all], writes=[pc])
        kb.op('dve', lambda e, pp=pp: e.tensor_tensor(out=Dm[:], in0=pp[:, 0:NE], in1=base[:], op=ALU.add),
              reads=[pp, base], writes=[Dm])
        kb.op('dve', lambda e, pc=pc: e.tensor_tensor(out=base[:], in0=pc[:, 0:NE], in1=base[:], op=ALU.add),
              reads=[pc, base], writes=[base])
        for k in range(8):
            kb.op('dve', lambda e, k=k, tg=tg: e.scalar_tensor_tensor(
                out=junkd[:], in0=iota[:], scalar=i8f_all[:, tg, k:k + 1], in1=Dm[:], op0=ALU.is_equal,
                op1=ALU.mult, accum_out=dest_f[:, k:k + 1]), reads=[iota, i8f_all, Dm], writes=[junkd, dest_f])
        kb.op('dve', lambda e, tg=tg: e.tensor_copy(out=dest_all[:, tg, :], in_=dest_f[:]), reads=[dest_f],
              writes=[dest_all])
        hs_ = h2s[tg % 3]
        kb.dma('sp', hs_[:], H2[tg * 128:(tg + 1) * 128, :], reads=[H2], writes=[hs_], sembuf=hs_)
        for k in range(8):
            kb.dma('pool', None, None, reads=[hs_, dest_all], writes=[HS], sembuf=hs_,
                   fn=lambda e, k=k, tg=tg, hs_=hs_: e.indirect_dma_start(
                       out=HS[:, :], out_offset=bass.IndirectOffsetOnAxis(ap=dest_all[:, tg, k:k + 1], axis=0),
                       in_=hs_[:, :], in_offset=None))
    be_bc = kb.sb("be_bc", [128, NBLK], F32)
    for bb in range(NBLK):
        kb.op('dve', lambda e, bb=bb: e.tensor_scalar(out=junkd[:], in0=Cc[:], scalar1=float(bb), scalar2=0.0,
                                                      op0=ALU.is_le, op1=ALU.add, accum_out=be_bc[:, bb:bb + 1]),
              reads=[Cc], writes=[junkd, be_bc])
    same = kb.sb("same", [128, NBLK], F32)
    bex = kb.sb("bex", [128, NBLK], F32)
    for lag, dst in ((3, idx_all), (4, idx4_all)):
        kb.op('dve', lambda e, lag=lag: e.memset(same[:, 0:lag], 0.0), writes=[same])
        kb.op('dve', lambda e, lag=lag: e.tensor_tensor(out=same[:, lag:NBLK], in0=be_bc[:, lag:NBLK],
                                                        in1=be_bc[:, 0:NBLK - lag], op=ALU.is_equal),
              reads=[be_bc], writes=[same])
        kb.op('dve', lambda e: e.scalar_tensor_tensor(out=bex[:], in0=same[:], scalar=float(2 * NE), in1=be_bc[:],
                                                      op0=ALU.mult, op1=ALU.add), reads=[same, be_bc], writes=[bex])
        kb.op('dve', lambda e: e.tensor_scalar(out=bex[:], in0=bex[:], scalar1=128.0, scalar2=bvals[:, 0:1],
                                               op0=ALU.mult, op1=ALU.add), reads=[bex, bvals], writes=[bex])
        kb.op('dve', lambda e, dst=dst: e.tensor_copy(out=dst[:], in_=bex[:]), reads=[bex], writes=[dst])
    phD.__exit__(None, None, None)
    phCD.__exit__(None, None, None)

    phE = kb.phase()
    phE.__enter__()
    wA = [kb.sb(f"wA{i}", [128, 2048], BF16) for i in range(3)]
    wB = [kb.sb(f"wB{i}", [128, 2048], BF16) for i in range(3)]
    wDn = [kb.sb(f"wDn{i}", [128, 2048], BF16) for i in range(4)]
    xb = [kb.sb(f"xb{i}", [128, D], BF16) for i in range(6)]
    xbT = [kb.sb(f"xbT{i}", [128, 8, 256], BF16) for i in range(2)]
    sge = [kb.sb(f"sge{i}", [128, 256], F32) for i in range(4)]
    actk = [kb.sb(f"actk{i}", [128, 256], BF16) for i in range(4)]
    acte = [kb.sb(f"acte{i}", [128, 2, 2, 128], BF16) for i in range(2)]
    yb = [kb.sb(f"yb{i}", [128, 2, D], BF16) for i in range(2)]
    bnd_reg = nc.gpsimd.alloc_register("bnd_reg")
    nc.gpsimd.reg_mov(bnd_reg, NE * 128 - 1)

    def gather(dst, src_d, idxt, sl):
        kb.dma('pool', None, None, reads=[idxt, src_d], writes=[dst], sembuf=dst,
               fn=lambda e: e.indirect_dma_start(
                   out=dst[:, :], out_offset=None, in_=src_d[:, :],
                   in_offset=bass.IndirectOffsetOnAxis(ap=idxt[:, sl:sl + 1], axis=0),
                   bounds_check=bnd_reg, oob_is_err=False))

    def issue_w(sl):
        gather(wA[sl % 3], WAb, idx_all, sl)
        gather(wB[sl % 3], WBb, idx_all, sl)
        gather(wDn[sl % 4], WDb, idx4_all, sl)

    def issue_x(sl):
        for sb in range(2):
            xt_ = xb[(2 * sl + sb) % 6]
            r0 = sl * BLK + sb * 128
            kb.dma('sp', xt_[:], HS[r0:r0 + 128, :], reads=[HS], writes=[xt_], sembuf=xt_)

    def e_T(sl):
        xT = xbT[sl % 2]
        for sb in range(2):
            xt_ = xb[(2 * sl + sb) % 6]
            tp = psum[sb]
            tpv = tp[:].bitcast(BF16)
            for kc in range(8):
                kb.op('pe', lambda e, kc=kc: e.transpose(
                    out=tpv[:, kc * 128:(kc + 1) * 128], in_=xt_[:, kc * 128:(kc + 1) * 128], identity=ident[:]),
                    reads=[xt_, ident], writes=[tp])
            src = tpv.rearrange("p (k t) -> p k t", t=128)
            if sb == 0:
                kb.op('dve', lambda e: e.tensor_copy(out=xT[:, :, 0:128], in_=src), reads=[tp], writes=[xT])
            else:
                kb.op('act', lambda e: e.activation(out=xT[:, :, 128:256], in_=src, func=AF.Copy),
                      reads=[tp], writes=[xT])

    def e_GU(sl):
        xT = xbT[sl % 2]
        for sb in range(2):
            pg = psum[2 + sb]
            for kc in range(8):
                wt_ = wA[sl % 3] if kc < 4 else wB[sl % 3]
                kb.op('pe', lambda e, kc=kc, wt_=wt_: e.matmul(
                    pg[:, :], lhsT=xT[:, kc, sb * 128:(sb + 1) * 128], rhs=wt_[:, (kc % 4) * 512:(kc % 4 + 1) * 512],
                    start=(kc == 0), stop=(kc == 7)), reads=[wt_, xT], writes=[pg])
            sg_ = sge[(2 * sl + sb) % 4]
            ak_ = actk[(2 * sl + sb) % 4]
            kb.op('act', lambda e: e.activation(out=sg_[:], in_=pg[:, 0:256], func=AF.Silu), reads=[pg],
                  writes=[sg_])
            kb.op('dve', lambda e: e.tensor_tensor(out=ak_[:], in0=sg_[:], in1=pg[:, 256:512], op=ALU.mult),
                  reads=[sg_, pg], writes=[ak_])

    def e_AT(sl):
        ta = psum[4]
        tav = ta[:].bitcast(BF16)
        for sb in range(2):
            ak_ = actk[(2 * sl + sb) % 4]
            for hc in range(2):
                c0 = (sb * 2 + hc) * 128
                kb.op('pe', lambda e, hc=hc, c0=c0: e.transpose(
                    out=tav[:, c0:c0 + 128], in_=ak_[:, hc * 128:(hc + 1) * 128], identity=ident[:]),
                    reads=[ak_, ident], writes=[ta])
        ac_ = acte[sl % 2]
        kb.op('dve', lambda e: e.tensor_copy(out=ac_[:].rearrange("p a b t -> p (a b t)"), in_=tav[:, 0:512]),
              reads=[ta], writes=[ac_])

    pocnt = [0]

    def e_DN(sl):
        wd = wDn[sl % 4][:].rearrange("p (h n) -> p h n", n=D)
        ac_ = acte[sl % 2]
        yb_ = yb[sl % 2]
        for sb in range(2):
            for hf in range(2):
                po_ = psum[5 + pocnt[0] % 3]
                pocnt[0] += 1
                for hc in range(2):
                    kb.op('pe', lambda e, hc=hc: e.matmul(
                        po_[:, :], lhsT=ac_[:, sb, hc, :], rhs=wd[:, hc, hf * 512:(hf + 1) * 512],
                        start=(hc == 0), stop=(hc == 1)), reads=[ac_, wDn[sl % 4]], writes=[po_])
                if hf == 0:
                    kb.op('dve', lambda e: e.tensor_copy(out=yb_[:, sb, 0:512], in_=po_[:, :]),
                          reads=[po_], writes=[yb_])
                else:
                    kb.op('act', lambda e: e.activation(out=yb_[:, sb, 512:1024], in_=po_[:, :], func=AF.Copy),
                          reads=[po_], writes=[yb_])
        kb.dma('sp', YS[sl * BLK:(sl + 1) * BLK, :].rearrange("(sb p) n -> p sb n", p=128), yb_[:],
               reads=[yb_], writes=[YS], sembuf=yb_)

    issue_w(0)
    issue_w(1)
    issue_x(0)
    issue_x(1)
    e_T(0)
    for sl in range(NBLK):
        if sl + 2 < NBLK:
            issue_w(sl + 2)
            issue_x(sl + 2)
        if sl + 1 < NBLK:
            e_T(sl + 1)
        if sl > 0:
            e_AT(sl - 1)
        e_GU(sl)
        if sl > 0:
            e_DN(sl - 1)
    e_AT(NBLK - 1)
    e_DN(NBLK - 1)
    phE.__exit__(None, None, None)

    phF = kb.phase()
    phF.__enter__()
    gfin_b = kb.sb("gfin_b", [128, D], F32)
    kb.dma('sp', gfin_b[:], bcast_rows(gfin_d[0:1, :]), writes=[gfin_b], sembuf=gfin_b)
    x2t = [kb.sb(f"x2t{i}", [128, D], F32) for i in range(2)]
    yk = [kb.sb(f"yk{i}", [128, D], BF16) for i in range(4)]
    acc = kb.sb("acc", [128, D], F32)
    tmpf = kb.sb("tmpf", [128, D], F32)
    junkf = kb.sb("junkf", [128, D], BF16)
    ssf = kb.sb("ssf", [128, 1], F32)
    sdf = kb.sb("sdf", [128, 1], F32)
    rsf = kb.sb("rsf", [128, 1], F32)
    ot = [kb.sb(f"ot{i}", [128, D], F32) for i in range(2)]
    gi_ = 0
    for tg in range(NTILE):
        s = tg // NT
        t = tg % NT
        xt = x2t[tg % 2]
        kb.dma('sp', xt[:], X2[tg * 128:(tg + 1) * 128, :], reads=[X2], writes=[xt], sembuf=xt)
        for k in range(8):
            yk_ = yk[gi_ % 4]
            gi_ += 1
            kb.dma('pool', None, None, reads=[YS, dest_all], writes=[yk_], sembuf=yk_,
                   fn=lambda e, k=k, tg=tg, yk_=yk_: e.indirect_dma_start(
                       out=yk_[:, :], out_offset=None, in_=YS[:, :],
                       in_offset=bass.IndirectOffsetOnAxis(ap=dest_all[:, tg, k:k + 1], axis=0)))
            if k == 0:
                kb.op('dve', lambda e, k=k, tg=tg, yk_=yk_: e.tensor_scalar(
                    out=acc[:], in0=yk_[:], scalar1=wts_all[:, tg, k:k + 1], scalar2=None, op0=ALU.mult),
                    reads=[yk_, wts_all], writes=[acc])
            else:
                kb.op('dve', lambda e, k=k, tg=tg, yk_=yk_: e.scalar_tensor_tensor(
                    out=acc[:], in0=yk_[:], scalar=wts_all[:, tg, k:k + 1], in1=acc[:], op0=ALU.mult, op1=ALU.add),
                    reads=[yk_, wts_all, acc], writes=[acc])
        g2 = gate_b[(5, s)]
        kb.op('dve', lambda e, g2=g2: e.tensor_tensor(out=tmpf[:], in0=acc[:], in1=g2[:], op=ALU.mult),
              reads=[acc, g2], writes=[tmpf])
        kb.op('dve', lambda e, xt=xt: e.tensor_tensor(out=xt[:], in0=xt[:], in1=tmpf[:], op=ALU.add),
              reads=[xt, tmpf], writes=[xt])
        kb.op('act', lambda e, xt=xt: e.activation(out=junkf[:], in_=xt[:], func=AF.Square, accum_out=ssf[:]),
              reads=[xt], writes=[junkf, ssf])
        kb.op('act', lambda e: e.activation(out=sdf[:], in_=ssf[:], func=AF.Sqrt, scale=1.0 / D, bias=epsc[:]),
              reads=[ssf, epsc], writes=[sdf])
        kb.op('dve', lambda e: e.reciprocal(out=rsf[:], in_=sdf[:]), reads=[sdf], writes=[rsf])
        o_ = ot[tg % 2]
        kb.op('dve', lambda e, xt=xt, o_=o_: e.scalar_tensor_tensor(
            out=o_[:], in0=xt[:], scalar=rsf[:], in1=gfin_b[:], op0=ALU.mult, op1=ALU.mult),
            reads=[xt, rsf, gfin_b], writes=[o_])
        kb.dma('sp', out_d[s, t * 128:(t + 1) * 128, :], o_[:], reads=[o_], writes=[out_d], sembuf=o_)
    phF.__exit__(None, None, None)
    allb = [out_d]
    for e in ('sp',):
        kb.wait_all(e, allb)
    return nc, kb, es


_CACHE = {}


def relayout_experts(inputs):
    g = inputs["w_exp_gate"][0].reshape(NE, 8, 128, 256).transpose(0, 2, 1, 3)
    u = inputs["w_exp_up"][0].reshape(NE, 8, 128, 256).transpose(0, 2, 1, 3)
    gu = np.concatenate([g, u], axis=3)
    wa = np.ascontiguousarray(gu[:, :, 0:4, :]).reshape(NE * 128, 2048)
    wb = np.ascontiguousarray(gu[:, :, 4:8, :]).reshape(NE * 128, 2048)
    wd = np.ascontiguousarray(inputs["w_exp_down"][0].reshape(NE, 2, 128, D).transpose(0, 2, 1, 3)).reshape(NE * 128, 2048)
    return {"_wa": wa, "_wb": wb, "_wd": wd}


def kernel(**inputs):
    inputs = {k: np.asarray(v) for k, v in inputs.items()}
    if 'nc' not in _CACHE:
        _CACHE['nc'] = build(False)
    nc = _CACHE['nc'][0]
    inputs.update(relayout_experts(inputs))
    in_maps = [make_inputs(c, **inputs) for c in range(8)]
    res = run_bass_kernel_spmd(nc, in_maps, core_ids=list(range(8)))
    out = np.concatenate([np.asarray(r["out"]) for r in res.results], axis=0)
    return out.astype(np.float32)


def _t5_bucket(dist):
    max_exact = 16
    dd = np.maximum(dist, 1).astype(np.float32)
    large = max_exact + (np.log(dd / max_exact) / np.log(2048 / max_exact) * (32 - max_exact)).astype(np.int32)
    large = np.minimum(large, 31)
    return np.where(dist < max_exact, dist, large).astype(np.int32)


def _consts():
    oh = np.zeros((32, 3, 384), np.float32)
    negm = np.full((8, 3, 384), NEG, np.float32)
    for di, (win, d) in enumerate(DIL):
        rel = np.arange(129)
        bk = _t5_bucket(rel * d)
        oh[bk, di, 127 + rel] = 1.0
        negm[:, di, 127:127 + 129] = 0.0
    J = np.zeros((128, 128), np.float32)
    J[np.arange(128), 127 - np.arange(128)] = 1.0
    tri = (np.arange(128)[None, :] >= np.arange(128)[:, None]).astype(ml_dtypes.bfloat16)
    lst = (np.arange(128)[:, None] < np.arange(128)[None, :]).astype(ml_dtypes.bfloat16)
    iota = np.tile(np.arange(NE, dtype=np.float32)[None, :], (128, 1))
    bvals = (np.arange(128)[:, None] + 128 * np.arange(6)[None, :]).astype(np.float32)
    return {"oh_in": oh, "negm_in": negm, "J_in": J, "tri_in": tri, "lst_in": lst, "iota_in": iota,
            "bvals_in": bvals}


CONSTS = _consts()


def make_inputs(core, x, c, w_in, b_forget, g_fox_out, g_dil_out, w_out, w_ada, b_ada, **kw):
    f = np.ascontiguousarray
    xs = f(x[NSEQ * core:NSEQ * (core + 1)])
    cs = c[NSEQ * core:NSEQ * (core + 1)]
    cT = f(cs.T.reshape(8, 128, NSEQ).transpose(1, 0, 2))
    gm = np.concatenate([g_fox_out[0], g_dil_out[0]])
    return {
        "x": xs, "cT": cT, "w_in": f(w_in[0]), "b_forget": f(b_forget[0].reshape(8, 1)),
        "g_mix": f(gm.reshape(8, 128).T), "w_out": f(w_out[0]), "w_ada": f(w_ada[0]),
        "b_ada": f(b_ada[0].reshape(1, -1)), "b_adaT": f(b_ada[0].reshape(48, 128).T),
        "ident_in": np.eye(128, dtype=ml_dtypes.bfloat16),
        "rel_bias": f(kw["rel_bias"]), **CONSTS,
        "w_router": f(kw["w_router"][0]), "router_bias": f(kw["router_bias"][0].reshape(1, -1)),
        "w_sh_gate": f(kw["w_sh_gate"][0]), "w_sh_up": f(kw["w_sh_up"][0]), "w_sh_down": f(kw["w_sh_down"][0]),
        "w_exp_a": kw["_wa"], "w_exp_b": kw["_wb"], "w_exp_d": kw["_wd"],
        "g_final": f(kw["g_final"].reshape(1, -1)),
    }
```

```python
import numpy as np
import ml_dtypes
from contextlib import ExitStack
import concourse.bass as bass
import concourse.mybir as mybir
from concourse.bass_utils import run_bass_kernel_spmd

F32 = mybir.dt.float32
BF16 = mybir.dt.bfloat16
I32 = mybir.dt.int32
U32 = mybir.dt.uint32
AF = mybir.ActivationFunctionType
ALU = mybir.AluOpType

S = 4096
D = 1024
NSEQ = 2
NT = S // 128
NG = S // 512
NTOK = NSEQ * S
NTILE = NTOK // 128
INC = 3080
NE = 256
TOPK = 8
BLK = 256
NBLK = NTOK * TOPK // BLK + NE
RROWS = NBLK * BLK
EPS = 1e-6
NEG = -30000.0
DIL = ((128, 1), (512, 4), (2048, 16))
LIMIT = 30000


class Buf:
    def __init__(self, name, dram=False):
        self.name = name
        self.w = {}
        self.r = {}
        self.sem = None
        self.cnt = 0
        self.dram = dram


class TT:
    def __init__(self, t, name, dram=False):
        self.t = t
        self.buf = Buf(name, dram)

    def __getitem__(self, k):
        return self.t[k]


class KB:
    def __init__(self, nc, es):
        self.nc = nc
        self.es = es
        self.engs = {'pe': nc.tensor, 'dve': nc.vector, 'act': nc.scalar, 'pool': nc.gpsimd, 'sp': nc.sync}
        self.esem = {}
        self.ecnt = {}
        self.known = {e: {} for e in self.engs}
        self.nsem = 0
        self.nins = {e: 0 for e in self.engs}
        self.cur_es = es
        self.dmabufs = []
        self.free_sems = []
        self.phase_bufs = [[]]
        for e in self.engs:
            self._epoch(e)

    def phase(self):
        kb = self

        class _Ph:
            def __enter__(self_):
                self_.es = ExitStack()
                self_.prev = kb.cur_es
                kb.cur_es = self_.es
                kb.phase_bufs.append([])
                return self_

            def __exit__(self_, *a):
                kb.barrier()
                for b in kb.phase_bufs.pop():
                    if b.sem is not None:
                        kb.free_sems.append((b.sem, b.cnt))
                        if b in kb.dmabufs:
                            kb.dmabufs.remove(b)
                        b.sem = None
                kb.cur_es = self_.prev
                self_.es.close()
                return False
        return _Ph()

    def barrier(self):
        evs = {}
        for e in self.engs:
            if self.ecnt[e] > 0:
                evs[self.esem[e]] = self.ecnt[e]
        for b in self.dmabufs:
            if b.sem is not None and b.cnt > 0 and b.name != "precast":
                evs[b.sem] = b.cnt
        for e in self.engs:
            kn = self.known[e]
            for sem, val in evs.items():
                if sem == self.esem[e]:
                    continue
                if kn.get(sem, 0) < val:
                    self.engs[e].wait_ge(sem, val)
                    kn[sem] = val

    def newsem(self, name):
        self.nsem += 1
        return self.es.enter_context(self.nc.semaphore(f"{name}_{self.nsem}"))

    def _epoch(self, e):
        self.esem[e] = self.newsem("e" + e)
        self.ecnt[e] = 0

    def sb(self, name, shape, dt):
        t = TT(self.cur_es.enter_context(self.nc.sbuf_tensor(name, list(shape), dt)), name)
        self.phase_bufs[-1].append(t.buf)
        return t

    def ps(self, name, shape, dt):
        return TT(self.es.enter_context(self.nc.psum_tensor(name, list(shape), dt)), name)

    def dram(self, name, shape, dt, kind="Internal"):
        return TT(self.nc.dram_tensor(name, list(shape), dt, kind=kind), name, dram=True)

    def _wait(self, e, reads, writes):
        deps = {}

        def add(sem, val, src, raw):
            if src == e:
                if e == 'pe' or e == 'sp':
                    return
                if not raw:
                    return
            k = sem
            if deps.get(k, 0) < val:
                deps[k] = val
        for b in reads:
            b = b.buf if isinstance(b, TT) else b
            for sem, (val, src) in b.w.items():
                add(sem, val, src, True)
        for b in writes:
            b = b.buf if isinstance(b, TT) else b
            for sem, (val, src) in b.w.items():
                add(sem, val, src, False)
            for sem, (val, src) in b.r.items():
                add(sem, val, src, False)
        kn = self.known[e]
        for sem, val in deps.items():
            if kn.get(sem, 0) < val:
                self.engs[e].wait_ge(sem, val)
                kn[sem] = val

    def _post(self, ev, reads, writes):
        sem, val, src = ev
        for b in writes:
            b = b.buf if isinstance(b, TT) else b
            if b.dram:
                b.w[sem] = (val, src)
            else:
                b.w = {sem: (val, src)}
                b.r = {}
        for b in reads:
            b = b.buf if isinstance(b, TT) else b
            b.r[sem] = (val, src)

    def op(self, e, fn, reads=(), writes=()):
        self._wait(e, reads, writes)
        if self.ecnt[e] >= LIMIT:
            self._epoch(e)
        ins = fn(self.engs[e])
        self.ecnt[e] += 1
        self.nins[e] += 1
        ins.then_inc(self.esem[e], 1)
        self._post((self.esem[e], self.ecnt[e], e), reads, writes)
        return ins

    def dma(self, q, out, in_, reads=(), writes=(), sembuf=None, fn=None, **kw):
        self._wait(q, reads, writes)
        sb_ = sembuf.buf if isinstance(sembuf, TT) else sembuf
        if sb_.sem is None or sb_.cnt >= LIMIT:
            if sb_.sem is None and self.free_sems and self.free_sems[-1][1] < LIMIT // 2:
                sb_.sem, sb_.cnt = self.free_sems.pop()
            else:
                sb_.sem = self.newsem("d" + sb_.name[:8])
                sb_.cnt = 0
            if sb_ not in self.dmabufs:
                self.dmabufs.append(sb_)
        if fn is None:
            ins = self.engs[q].dma_start(out=out, in_=in_, **kw)
        else:
            ins = fn(self.engs[q])
        sb_.cnt += 16
        self.nins[q] += 1
        ins.then_inc(sb_.sem, 16)
        self._post((sb_.sem, sb_.cnt, 'dma'), reads, writes)
        return ins

    def wait_all(self, e, bufs):
        self._wait(e, bufs, ())


def bcast_rows(ap_row, nparts=128):
    n = ap_row.shape[-1]
    return bass.AP(ap_row.tensor, ap_row.offset, [[0, nparts], [1, n]])


def build(debug=False):
    nc = bass.Bass("TRN2", target_bir_lowering=False)
    es = ExitStack()
    kb = KB(nc, es)
    ein = lambda name, shape, dt=F32: TT(nc.dram_tensor(name, list(shape), dt, kind="ExternalInput"), name, dram=True)
    okind = "ExternalOutput" if debug else "Internal"

    def record(f, *a):
        rec = []
        orig_op, orig_dma = kb.op, kb.dma
        kb.op = lambda *args, **kw: rec.append((orig_op, args, kw))
        kb.dma = lambda *args, **kw: rec.append((orig_dma, args, kw))
        try:
            f(*a)
        finally:
            kb.op, kb.dma = orig_op, orig_dma
        return rec

    def emit_interleaved(A, B):
        ia = ib = 0
        while ia < len(A) or ib < len(B):
            if ib >= len(B) or (ia < len(A) and ia * len(B) <= ib * len(A)):
                fn, args, kw = A[ia]
                ia += 1
            else:
                fn, args, kw = B[ib]
                ib += 1
            fn(*args, **kw)

    def emit_merged(streams):
        streams = [st for st in streams if st]
        pos = [0] * len(streams)
        total = sum(len(st) for st in streams)
        for _ in range(total):
            best = min((i for i in range(len(streams)) if pos[i] < len(streams[i])),
                       key=lambda i: pos[i] / len(streams[i]))
            fn, args, kw = streams[best][pos[best]]
            pos[best] += 1
            fn(*args, **kw)


    x_in = ein("x", [NSEQ, S, D])
    cT_in = ein("cT", [128, 8, NSEQ])
    w_in_d = ein("w_in", [D, INC])
    bfg_in = ein("b_forget", [8, 1])
    gmix_in = ein("g_mix", [128, 8])
    w_out_d = ein("w_out", [D, D])
    w_ada_d = ein("w_ada", [D, 6 * D])
    b_ada_d = ein("b_ada", [1, 6 * D])
    b_adaT_d = ein("b_adaT", [128, 48])
    ident_d = ein("ident_in", [128, 128], BF16)
    relb_d = ein("rel_bias", [32, 8])
    oh_d = ein("oh_in", [32, 3, 384])
    negm_d = ein("negm_in", [8, 3, 384])
    J_d = ein("J_in", [128, 128])
    tri_d = ein("tri_in", [128, 128], BF16)
    w_r_d = ein("w_router", [D, NE])
    rbias_d = ein("router_bias", [1, NE])
    wsg_d = ein("w_sh_gate", [D, 256])
    wsu_d = ein("w_sh_up", [D, 256])
    wsd_d = ein("w_sh_down", [256, D])
    wA_d = ein("w_exp_a", [NE * 128, 2048])
    wB_d = ein("w_exp_b", [NE * 128, 2048])
    wD_d = ein("w_exp_d", [NE * 128, 2048])
    gfin_d = ein("g_final", [1, D])
    lst_d = ein("lst_in", [128, 128], BF16)
    iota_d = ein("iota_in", [128, NE])
    bvals_d = ein("bvals_in", [128, 6])

    out_d = TT(nc.dram_tensor("out", [NSEQ, S, D], F32, kind="ExternalOutput"), "out", dram=True)

    QK = [kb.dram(f"QK{s}", [16, 128, S], BF16, kind=okind) for s in range(NSEQ)]
    VV = [kb.dram(f"VV{s}", [S, 1024], BF16, kind=okind) for s in range(NSEQ)]
    GP = [kb.dram(f"GP{s}", [8, 3, S], BF16, kind=okind) for s in range(NSEQ)]
    EXT = kb.dram("EXT", [8, 3, 384], F32)
    YF = [kb.dram(f"YF{s}", [S, 512], F32, kind=okind) for s in range(NSEQ)]
    OD = [kb.dram(f"OD{s}", [3, S, 8, 65], F32, kind=okind) for s in range(NSEQ)]
    H2 = kb.dram("H2", [NTOK, D], BF16)
    X2 = kb.dram("X2", [NTOK, D], F32, kind=okind)
    HS = kb.dram("HS", [RROWS, D], BF16)
    WGUb = kb.dram("WGUb", [NE * 128, 4096], BF16)
    WDb = kb.dram("WDb", [NE * 128, 2048], BF16)
    YS = kb.dram("YS", [RROWS, D], BF16)

    ident = kb.sb("ident", [128, 128], BF16)
    kb.dma('sp', ident[:], ident_d[:], writes=[ident], sembuf=ident)
    epsc = kb.sb("epsc", [128, 1], F32)
    kb.op('dve', lambda e: e.memset(epsc[:], EPS), writes=[epsc])
    cact = kb.sb("cact", [128, 8, NSEQ], F32)
    kb.dma('sp', cact[:], cT_in[:], writes=[cact], sembuf=cact)
    kb.op('act', lambda e: e.activation(out=cact[:], in_=cact[:], func=AF.Silu), reads=[cact], writes=[cact])
    badT = kb.sb("badT", [128, 48], F32)
    kb.dma('sp', badT[:], b_adaT_d[:], writes=[badT], sembuf=badT)
    bfg = kb.sb("bfg", [8, 1], F32)
    kb.dma('sp', bfg[:], bfg_in[:], writes=[bfg], sembuf=bfg)
    kb.op('dve', lambda e: e.tensor_scalar(out=bfg[:], in0=bfg[:], scalar1=-1.0, scalar2=None, op0=ALU.mult),
          reads=[bfg], writes=[bfg])

    psum = [kb.ps(f"ps{i}", [128, 512], F32) for i in range(8)]

    precast_buf = Buf("precast")
    PCH = 256
    precast_jobs = [(dst, c0, src, r0) for r0 in range(0, NE * 128, PCH)
                    for dst, c0, src in ((WGUb, 0, wA_d), (WGUb, 2048, wB_d), (WDb, 0, wD_d))]

    def precast(n):
        for _ in range(n):
            if precast_jobs:
                dst, c0, src, r0 = precast_jobs.pop(0)
                kb.dma('pool', dst[r0:r0 + PCH, c0:c0 + 2048], src[r0:r0 + PCH, :], writes=[dst],
                       sembuf=precast_buf)

    modT = {m: kb.sb(f"modT{m}", [128, 8, NSEQ], F32) for m in (0, 1)}
    gate_b = {(m, s): kb.sb(f"gate{m}_{s}", [128, 1024], F32) for m in (2, 3, 4, 5) for s in range(NSEQ)}
    ph0 = kb.phase()
    ph0.__enter__()
    wa = [kb.sb(f"wa{i}", [128, 8, 1024], F32) for i in range(2)]
    cbc = kb.sb("cbc", [128, NSEQ, 8, 128], F32)
    for s in range(NSEQ):
        for kc in range(8):
            kb.op('dve', lambda e, s=s, kc=kc: e.tensor_copy(out=cbc[:, s, kc, :],
                                                              in_=cact[:, kc, s:s + 1].to_broadcast([128, 128])),
                  reads=[cact], writes=[cbc])
    wi = 0
    for m in range(6):
        wt = wa[wi % 2]
        wi += 1
        kb.dma('sp', wt[:], w_ada_d[:, m * 1024:(m + 1) * 1024].rearrange("(kc p) n -> p kc n", p=128),
               writes=[wt], sembuf=wt)
        if m in (0, 1):
            pt = psum[wi % 2]
            for ncn in range(8):
                for kc in range(8):
                    kb.op('pe', lambda e, ncn=ncn, kc=kc, wt=wt, pt=pt: e.matmul(
                        pt[:, ncn * 2:(ncn + 1) * 2], lhsT=wt[:, kc, ncn * 128:(ncn + 1) * 128],
                        rhs=cact[:, kc, :], start=(kc == 0), stop=(kc == 7)),
                        reads=[wt, cact], writes=[pt])
            mt = modT[m]
            kb.op('dve', lambda e, pt=pt, mt=mt, m=m: e.tensor_tensor(
                out=mt[:], in0=pt[:, 0:16].rearrange("p (a b) -> p a b", b=NSEQ),
                in1=badT[:, m * 8:(m + 1) * 8].unsqueeze(2).to_broadcast([128, 8, NSEQ]), op=ALU.add),
                reads=[pt, badT], writes=[mt])
            if m == 1:
                kb.op('dve', lambda e, mt=mt: e.tensor_scalar(out=mt[:], in0=mt[:], scalar1=1.0, scalar2=None,
                                                             op0=ALU.add), reads=[mt], writes=[mt])
        else:
            brow = kb.sb(f"brow{m}", [128, 1024], F32)
            kb.dma('sp', brow[:], bcast_rows(b_ada_d[0:1, m * 1024:(m + 1) * 1024]), writes=[brow], sembuf=brow)
            for s in range(NSEQ):
                gb = gate_b[(m, s)]
                for half in range(2):
                    pt = psum[2 + half]
                    for kc in range(8):
                        kb.op('pe', lambda e, kc=kc, wt=wt, pt=pt, s=s, half=half: e.matmul(
                            pt[:, :], lhsT=cbc[:, s, kc, :], rhs=wt[:, kc, half * 512:(half + 1) * 512],
                            start=(kc == 0), stop=(kc == 7)), reads=[wt, cbc], writes=[pt])
                    kb.op('dve', lambda e, pt=pt, gb=gb, half=half, brow=brow: e.tensor_tensor(
                        out=gb[:, half * 512:(half + 1) * 512], in0=pt[:, :],
                        in1=brow[:, half * 512:(half + 1) * 512], op=ALU.add),
                        reads=[pt, brow], writes=[gb])
                if m == 4:
                    kb.op('dve', lambda e, gb=gb: e.tensor_scalar(out=gb[:], in0=gb[:], scalar1=1.0, scalar2=None,
                                                                 op0=ALU.add), reads=[gb], writes=[gb])

    ph0.__exit__(None, None, None)

    phA = kb.phase()
    phA.__enter__()
    w_in_bf = kb.sb("w_in_bf", [128, 8, INC], BF16)
    for kc in range(8):
        for hf in range(2):
            kb.dma('pool', w_in_bf[:, kc, hf * 1540:(hf + 1) * 1540],
                   w_in_d[kc * 128:(kc + 1) * 128, hf * 1540:(hf + 1) * 1540],
                   writes=[w_in_bf], sembuf=w_in_bf)
    xp = [kb.sb(f"xp{i}", [128, D], F32) for i in range(3)]
    junk = kb.sb("junk", [128, D], BF16)
    xn = [kb.sb(f"xn{i}", [128, D], BF16) for i in range(2)]
    ssq = [kb.sb(f"ssq{i}", [128, 1], F32) for i in range(2)]
    std = [kb.sb(f"std{i}", [128, 1], F32) for i in range(2)]
    rstd = [kb.sb(f"rstd{i}", [128, 1], F32) for i in range(2)]
    hT = [kb.sb(f"hT{i}", [128, 8, 512], BF16) for i in range(2)]
    qkst = [kb.sb(f"qkst{i}", [128, 16, 512], BF16) for i in range(2)]
    vst = [kb.sb(f"vst{i}", [128, 4, 1024], BF16) for i in range(2)]
    ef = [kb.sb(f"ef{i}", [8, 512], F32) for i in range(2)]
    lf = [kb.sb(f"lf{i}", [8, 512], F32) for i in range(2)]
    Gt = [kb.sb(f"Gt{i}", [8, 512], F32) for i in range(2)]
    r1 = kb.sb("r1", [8, 512], F32)
    r2 = kb.sb("r2", [8, 512], F32)
    gpt = [kb.sb(f"gpt{i}", [8, 3, 512], BF16) for i in range(2)]
    ones8 = kb.sb("ones8", [8, 512], F32)
    kb.op('dve', lambda e: e.memset(ones8[:], 1.0), writes=[ones8])
    carry = kb.sb("carry", [8, 1], F32)

    qk_cols = [0 + 128 * i for i in range(4)] + [512 + 128 * i for i in range(4)] + \
              [1544 + 128 * i for i in range(4)] + [2056 + 128 * i for i in range(4)]
    qk_isq = [True] * 4 + [False] * 4 + [True] * 4 + [False] * 4
    st = {"tcount": 0, "evac": 0}

    def A1(gi):
        s, g = gi // NG, gi % NG
        gcount = gi
        tcount = gi * 4
        hTg = hT[gcount % 2]
        for j in range(4):
            t = g * 4 + j
            xt = xp[tcount % 3]
            i2 = tcount % 2
            kb.dma('sp', xt[:], x_in[s, t * 128:(t + 1) * 128, :], writes=[xt], sembuf=xt)
            kb.op('act', lambda e, xt=xt, i2=i2: e.activation(out=junk[:], in_=xt[:], func=AF.Square,
                                                               accum_out=ssq[i2][:]),
                  reads=[xt], writes=[junk, ssq[i2]])
            kb.op('act', lambda e, i2=i2: e.activation(out=std[i2][:], in_=ssq[i2][:], func=AF.Sqrt,
                                                       scale=1.0 / D, bias=epsc[:]),
                  reads=[ssq[i2], epsc], writes=[std[i2]])
            kb.op('dve', lambda e, i2=i2: e.reciprocal(out=rstd[i2][:], in_=std[i2][:]),
                  reads=[std[i2]], writes=[rstd[i2]])
            kb.op('act', lambda e, xt=xt, i2=i2: e.activation(out=xn[i2][:], in_=xt[:], func=AF.Identity,
                                                               scale=rstd[i2][:]),
                  reads=[xt, rstd[i2]], writes=[xn[i2]])
            tp = psum[tcount % 2]
            tpv = tp[:].bitcast(BF16)
            for kc in range(8):
                kb.op('pe', lambda e, kc=kc, i2=i2, tpv=tpv: e.transpose(
                    out=tpv[:, kc * 128:(kc + 1) * 128], in_=xn[i2][:, kc * 128:(kc + 1) * 128],
                    identity=ident[:]), reads=[xn[i2], ident], writes=[tp])
            for kc in range(8):
                kb.op('dve', lambda e, kc=kc, tpv=tpv, j=j, hTg=hTg, s=s: e.tensor_scalar(
                    out=hTg[:, kc, j * 128:(j + 1) * 128], in0=tpv[:, kc * 128:(kc + 1) * 128],
                    scalar1=modT[1][:, kc, s:s + 1], scalar2=modT[0][:, kc, s:s + 1],
                    op0=ALU.mult, op1=ALU.add), reads=[tp, modT[1], modT[0]], writes=[hTg])
            tcount += 1

    def A2(gi):
        nonlocal evac
        s, g = gi // NG, gi % NG
        gcount = gi
        hTg = hT[gcount % 2]
        if g == 0:
            kb.op('dve', lambda e: e.memset(carry[:], 0.0), writes=[carry])
        i2 = gcount % 2
        pf = psum[2]
        for kc in range(8):
            kb.op('pe', lambda e, kc=kc, pf=pf, hTg=hTg: e.matmul(
                pf[0:8, :], lhsT=w_in_bf[:, kc, 1536:1544], rhs=hTg[:, kc, :],
                start=(kc == 0), stop=(kc == 7)), reads=[w_in_bf, hTg], writes=[pf])
        kb.op('act', lambda e, pf=pf, i2=i2: e.activation(out=ef[i2][:], in_=pf[0:8, :], func=AF.Exp,
                                                           scale=-1.0, bias=bfg[:]),
              reads=[pf, bfg], writes=[ef[i2]])
        kb.op('act', lambda e, i2=i2: e.activation(out=lf[i2][:], in_=ef[i2][:], func=AF.Ln, bias=1.0),
              reads=[ef[i2]], writes=[lf[i2]])
        kb.op('dve', lambda e, i2=i2: e.tensor_tensor_scan(out=Gt[i2][:], data0=ones8[:], data1=lf[i2][:],
                                                            initial=carry[:], op0=ALU.mult, op1=ALU.add),
              reads=[ones8, lf[i2], carry], writes=[Gt[i2]])
        kb.op('dve', lambda e, i2=i2: e.tensor_copy(out=carry[:], in_=Gt[i2][:, 511:512]),
              reads=[Gt[i2]], writes=[carry])
        gp_ = gpt[i2]
        kb.op('dve', lambda e, i2=i2, gp_=gp_: e.tensor_copy(out=gp_[:, 0, :], in_=Gt[i2][:]),
              reads=[Gt[i2]], writes=[gp_])
        kb.op('dve', lambda e, i2=i2, gp_=gp_: e.tensor_tensor(out=r1[:], in0=Gt[i2][:], in1=gp_[:, 0, :],
                                                                op=ALU.subtract),
              reads=[Gt[i2], gp_], writes=[r1])
        kb.op('dve', lambda e, gp_=gp_: e.tensor_copy(out=gp_[:, 1, :], in_=r1[:]), reads=[r1], writes=[gp_])
        kb.op('dve', lambda e, gp_=gp_: e.tensor_tensor(out=r2[:], in0=r1[:], in1=gp_[:, 1, :],
                                                        op=ALU.subtract), reads=[r1, gp_], writes=[r2])
        kb.op('dve', lambda e, gp_=gp_: e.tensor_copy(out=gp_[:, 2, :], in_=r2[:]), reads=[r2], writes=[gp_])
        kb.dma('pool', GP[s][:, :, g * 512:(g + 1) * 512], gp_[:], reads=[gp_], writes=[GP[s]], sembuf=gp_)
        qs = qkst[gcount % 2]
        for ci in range(16):
            pq = psum[4 + (ci % 4)]
            c0 = qk_cols[ci]
            for kc in range(8):
                kb.op('pe', lambda e, kc=kc, pq=pq, c0=c0, hTg=hTg: e.matmul(
                    pq[:, :], lhsT=w_in_bf[:, kc, c0:c0 + 128], rhs=hTg[:, kc, :],
                    start=(kc == 0), stop=(kc == 7)), reads=[w_in_bf, hTg], writes=[pq])
            sc = 0.125 if qk_isq[ci] else 1.0
            if evac % 2 == 0:
                kb.op('act', lambda e, pq=pq, qs=qs, ci=ci, sc=sc: e.activation(
                    out=qs[:, ci, :], in_=pq[:, :], func=AF.Copy, scale=sc), reads=[pq], writes=[qs])
            else:
                kb.op('dve', lambda e, pq=pq, qs=qs, ci=ci, sc=sc: e.tensor_scalar(
                    out=qs[:, ci, :], in0=pq[:, :], scalar1=sc, scalar2=None, op0=ALU.mult),
                    reads=[pq], writes=[qs])
            evac += 1
        kb.dma('pool', QK[s][:, :, g * 512:(g + 1) * 512].rearrange("c p t -> p c t"), qs[:],
               reads=[qs], writes=[QK[s]], sembuf=qs)
        vs_ = vst[gcount % 2]
        for j in range(4):
            for hf, c0 in enumerate((1024, 2568)):
                pv = psum[2 + ((j * 2 + hf) % 2)] if False else psum[4 + ((j * 2 + hf) % 4)]
                for kc in range(8):
                    kb.op('pe', lambda e, kc=kc, pv=pv, c0=c0, hTg=hTg, j=j: e.matmul(
                        pv[:, :], lhsT=hTg[:, kc, j * 128:(j + 1) * 128], rhs=w_in_bf[:, kc, c0:c0 + 512],
                        start=(kc == 0), stop=(kc == 7)), reads=[w_in_bf, hTg], writes=[pv])
                if evac % 2 == 0:
                    kb.op('act', lambda e, pv=pv, vs_=vs_, j=j, hf=hf: e.activation(
                        out=vs_[:, j, hf * 512:(hf + 1) * 512], in_=pv[:, :], func=AF.Copy),
                        reads=[pv], writes=[vs_])
                else:
                    kb.op('dve', lambda e, pv=pv, vs_=vs_, j=j, hf=hf: e.tensor_copy(
                        out=vs_[:, j, hf * 512:(hf + 1) * 512], in_=pv[:, :]), reads=[pv], writes=[vs_])
                evac += 1
        kb.dma('pool', VV[s][g * 512:(g + 1) * 512, :].rearrange("(j p) n -> p j n", p=128), vs_[:],
               reads=[vs_], writes=[VV[s]], sembuf=vs_)

    evac = 0
    A1(0)
    for gi in range(NSEQ * NG):
        Aa = record(A1, gi + 1) if gi + 1 < NSEQ * NG else []
        Bb = record(A2, gi)
        emit_merged([Aa, Bb])
        precast(4)
    phA.__exit__(None, None, None)

    phB = kb.phase()
    phB.__enter__()
    tri = kb.sb("tri", [128, 128], BF16)
    kb.dma('sp', tri[:], tri_d[:], writes=[tri], sembuf=tri)
    Tt = kb.sb("Tt", [128, 24, 256], F32)
    rb = kb.sb("rb", [32, 8], F32)
    kb.dma('sp', rb[:], relb_d[:], writes=[rb], sembuf=rb)
    oh = kb.sb("oh", [32, 3, 384], F32)
    kb.dma('sp', oh[:], oh_d[:], writes=[oh], sembuf=oh)
    negm = kb.sb("negm", [8, 3, 384], F32)
    kb.dma('sp', negm[:], negm_d[:], writes=[negm], sembuf=negm)
    Jm = kb.sb("Jm", [128, 128], F32)
    kb.dma('sp', Jm[:], J_d[:], writes=[Jm], sembuf=Jm)
    extsb = kb.sb("extsb", [8, 3, 384], F32)
    for di in range(3):
        pt = psum[di % 2]
        kb.op('pe', lambda e, di=di, pt=pt: e.matmul(pt[0:8, 0:384], lhsT=rb[:, :], rhs=oh[:, di, :],
                                                      start=True, stop=True), reads=[rb, oh], writes=[pt])
        kb.op('dve', lambda e, di=di, pt=pt: e.tensor_tensor(out=extsb[:, di, :], in0=pt[0:8, 0:384],
                                                              in1=negm[:, di, :], op=ALU.add),
              reads=[pt, negm], writes=[extsb])
    kb.dma('sp', EXT[:], extsb[:], reads=[extsb], writes=[EXT], sembuf=extsb)
    hk = [kb.sb(f"hk{i}", [128, 256], F32) for i in range(2)]
    for di in range(3):
        for h in range(8):
            hkt = hk[(di * 8 + h) % 2]
            kb.dma('sp', hkt[:], bass.AP(EXT.t, (h * 3 + di) * 384, [[1, 128], [1, 256]]),
                   reads=[EXT], writes=[hkt], sembuf=hkt)
            pt = psum[(di * 8 + h) % 2]
            kb.op('pe', lambda e, hkt=hkt, pt=pt: e.matmul(pt[:, 0:256], lhsT=Jm[:, :], rhs=hkt[:, :],
                                                            start=True, stop=True), reads=[Jm, hkt], writes=[pt])
            kb.op('dve', lambda e, pt=pt, di=di, h=h: e.tensor_copy(out=Tt[:, di * 8 + h, :], in_=pt[:, 0:256]),
                  reads=[pt], writes=[Tt])

    KT = [kb.sb(f"KT{i}", [70, S], BF16) for i in range(2)]
    QT = [kb.sb(f"QT{i}", [70, S], BF16) for i in range(2)]
    KTd = [kb.sb(f"KTd{i}", [64, S], BF16) for i in range(2)]
    QTd = [kb.sb(f"QTd{i}", [64, S], BF16) for i in range(2)]
    Vf = [kb.sb(f"Vf{i}", [128, 32, 65], BF16) for i in range(2)]
    _vd = [kb.sb(f"Vd_{di}", [128, 32, 65], BF16) for di in range(3)]
    Vd = [_vd, _vd]
    for i in range(2):
        kb.op('pool', lambda e, i=i: e.memset(KT[i][64:70, :], -1.0), writes=[KT[i]])
        kb.op('pool', lambda e, i=i: e.memset(QT[i][64:70, :], 1.0), writes=[QT[i]])
        kb.op('pool', lambda e, i=i: e.memset(Vf[i][:, :, 64:65], 1.0), writes=[Vf[i]])
        if i == 0:
            for di in range(3):
                kb.op('pool', lambda e, i=i, di=di: e.memset(Vd[i][di][:, :, 64:65], 1.0), writes=[Vd[i][di]])
    Pt = [kb.sb(f"Pt{i}", [128, 512], BF16) for i in range(3)]
    Ptd = [kb.sb(f"Ptd{i}", [128, 256], BF16) for i in range(3)]
    Ssb = [kb.sb(f"Ssb{i}", [128, 256], F32) for i in range(3)]
    yst = [kb.sb(f"yst{i}", [128, 4, 64], F32) for i in range(2)]
    rcf = [kb.sb(f"rcf{i}", [128, 1], F32) for i in range(4)]
    ost = [kb.sb(f"ost{i}", [128, 32, 65], F32) for i in range(2)]
    hcount = 0
    ycount = 0
    ocount = 0
    for s in range(NSEQ):
        for h in range(8):
            hb = hcount % 2
            hcount += 1
            po = (h % 2) * 64
            kt_, qt_, ktd, qtd, vf = KT[hb], QT[hb], KTd[hb], QTd[hb], Vf[hb]
            kb.dma('sp', kt_[0:64, :], QK[s][4 + h // 2, po:po + 64, :], reads=[QK[s]], writes=[kt_], sembuf=kt_)
            kb.dma('sp', kt_[67:70, :], GP[s][h, :, :], reads=[GP[s]], writes=[kt_], sembuf=kt_)
            kb.dma('sp', qt_[0:64, :], QK[s][h // 2, po:po + 64, :], reads=[QK[s]], writes=[qt_], sembuf=qt_)
            kb.dma('sp', qt_[64:67, :], GP[s][h, :, :], reads=[GP[s]], writes=[qt_], sembuf=qt_)
            kb.dma('sp', vf[:, :, 0:64], VV[s][:, h * 64:(h + 1) * 64].rearrange("(n p) e -> p n e", p=128),
                   reads=[VV[s]], writes=[vf], sembuf=vf)
            kb.dma('sp', ktd[:, :], QK[s][12 + h // 2, po:po + 64, :], reads=[QK[s]], writes=[ktd], sembuf=ktd)
            kb.dma('sp', qtd[:, :], QK[s][8 + h // 2, po:po + 64, :], reads=[QK[s]], writes=[qtd], sembuf=qtd)
            for di, (win, d) in enumerate(DIL):
                NB = 32 // d
                vdt = Vd[hb][di]
                src = VV[s][:, 512 + h * 64:512 + (h + 1) * 64].rearrange("(n p r) e -> r p n e", p=128, r=d)
                for r in range(d):
                    kb.dma('sp', vdt[:, r * NB:(r + 1) * NB, 0:64], src[r], reads=[VV[s]], writes=[vdt], sembuf=vdt)
            def FOX():
                nonlocal ycount
                for qg in range(8):
                    nkt = 4 * (qg + 1)
                    Oq = [psum[4 + i] for i in range(4)]

                    def fox_s(kt, qg=qg):
                        j = kt - 4 * qg
                        c_lo = 128 * j if j > 0 else 0
                        Sp = psum[kt % 2]
                        Pk = Pt[kt % 3]
                        kb.op('pe', lambda e: e.matmul(
                            Sp[:, c_lo:512], lhsT=kt_[0:70, kt * 128:(kt + 1) * 128],
                            rhs=qt_[0:70, qg * 512 + c_lo:(qg + 1) * 512], start=True, stop=True),
                            reads=[kt_, qt_], writes=[Sp])
                        kb.op('act', lambda e: e.activation(
                            out=Pk[:, c_lo:512], in_=Sp[:, c_lo:512], func=AF.Exp), reads=[Sp], writes=[Pk])
                        if j >= 0:
                            kb.op('dve', lambda e: e.tensor_tensor(
                                out=Pk[:, c_lo:c_lo + 128], in0=Pk[:, c_lo:c_lo + 128], in1=tri[:, :], op=ALU.mult),
                                reads=[Pk, tri], writes=[Pk])

                    def fox_pv(kt, qg=qg, Oq=Oq):
                        j = kt - 4 * qg
                        Pk = Pt[kt % 3]
                        for i in range(max(j, 0), 4):
                            kb.op('pe', lambda e, i=i: e.matmul(
                                Oq[i][:, 0:65], lhsT=Pk[:, i * 128:(i + 1) * 128], rhs=vf[:, kt, :],
                                start=(kt == 0), stop=(kt == 4 * qg + i)), reads=[Pk, vf], writes=[Oq[i]])
                    for kt in range(nkt):
                        fox_s(kt)
                        if kt > 0:
                            fox_pv(kt - 1)
                    fox_pv(nkt - 1)
                    ys = yst[ycount % 2]
                    ycount += 1
                    for i in range(4):
                        kb.op('dve', lambda e, i=i: e.reciprocal(out=rcf[i][:], in_=Oq[i][:, 64:65]),
                              reads=[Oq[i]], writes=[rcf[i]])
                        kb.op('dve', lambda e, i=i, ys=ys: e.tensor_scalar(
                            out=ys[:, i, :], in0=Oq[i][:, 0:64], scalar1=rcf[i][:], scalar2=None, op0=ALU.mult),
                            reads=[Oq[i], rcf[i]], writes=[ys])
                    kb.dma('pool', YF[s][qg * 512:(qg + 1) * 512, h * 64:(h + 1) * 64].rearrange("(i p) e -> p i e", p=128),
                           ys[:], reads=[ys], writes=[YF[s]], sembuf=ys)
                    if qg % 2 == 0:
                        precast(1)

            def DILF():
                nonlocal ocount
                units = []
                for di, (win, d) in enumerate(DIL):
                    NB = 32 // d
                    for r in range(d):
                        for n in range(NB):
                            units.append((di, d, NB, r, n))
                OdB = psum[3]
                ostmap = {}

                def dil_s(ui, u):
                    di, d, NB, r, n = u
                    ncol = 256 if n < NB - 1 else 128
                    st = n * 128 * d + r
                    Sp = psum[2]
                    Sb = Ssb[ui % 3]
                    Pk = Ptd[ui % 3]
                    kb.op('pe', lambda e: e.matmul(
                        Sp[:, 0:ncol], lhsT=ktd[0:64, st:st + 127 * d + 1:d], rhs=qtd[0:64, st:st + (ncol - 1) * d + 1:d],
                        start=True, stop=True), reads=[ktd, qtd], writes=[Sp])
                    kb.op('dve', lambda e: e.tensor_tensor(
                        out=Sb[:, 0:ncol], in0=Sp[:, 0:ncol], in1=Tt[:, di * 8 + h, 0:ncol], op=ALU.add),
                        reads=[Sp, Tt], writes=[Sb])
                    kb.op('act', lambda e: e.activation(
                        out=Pk[:, 0:ncol], in_=Sb[:, 0:ncol], func=AF.Exp), reads=[Sb], writes=[Pk])

                def dil_pv(ui, u):
                    nonlocal ocount
                    di, d, NB, r, n = u
                    Pk = Ptd[ui % 3]
                    vdt = Vd[hb][di]
                    if n == 0:
                        ostmap[(di, r)] = ost[ocount % 2]
                        ocount += 1
                    os_ = ostmap[(di, r)]
                    kb.op('pe', lambda e: e.matmul(
                        OdB[:, (n % 2) * 128:(n % 2) * 128 + 65], lhsT=Pk[:, 0:128], rhs=vdt[:, r * NB + n, :],
                        start=(n == 0), stop=True, skip_group_check=True), reads=[Pk, vdt], writes=[OdB])
                    if n < NB - 1:
                        kb.op('pe', lambda e: e.matmul(
                            OdB[:, ((n + 1) % 2) * 128:((n + 1) % 2) * 128 + 65], lhsT=Pk[:, 128:256],
                            rhs=vdt[:, r * NB + n, :], start=True, stop=False, skip_group_check=True),
                            reads=[Pk, vdt], writes=[OdB])
                    kb.op('act', lambda e: e.activation(
                        out=os_[:, n, :], in_=OdB[:, (n % 2) * 128:(n % 2) * 128 + 65], func=AF.Copy), reads=[OdB], writes=[os_])
                    if n == NB - 1:
                        dstv = OD[s][di, :, h, :].rearrange("(n p r) e -> r p n e", p=128, r=d)
                        kb.dma('pool', dstv[r], os_[:, 0:NB, :], reads=[os_], writes=[OD[s]], sembuf=os_)
                        if r % 2 == 0:
                            precast(1)
                for ui, u in enumerate(units):
                    dil_s(ui, u)
                    if ui > 0:
                        dil_pv(ui - 1, units[ui - 1])
                dil_pv(len(units) - 1, units[-1])
            emit_merged([record(FOX), record(DILF)])
    phB.__exit__(None, None, None)

    wts_all = kb.sb("wts_all", [128, NTILE, 8], F32)
    dest_all = kb.sb("dest_all", [128, NTILE, 8], I32)
    cnt_b = kb.sb("cnt_b", [128, NE], F32)
    idx_all = kb.sb("idx_all", [128, NBLK], I32)
    idx4_all = kb.sb("idx4_all", [128, NBLK], I32)
    ones_bf = kb.sb("ones_bf", [128, 128], BF16)
    kb.op('dve', lambda e: e.memset(ones_bf[:], 1.0), writes=[ones_bf])
    BIG = 1.0e4
    phCD = kb.phase()
    phCD.__enter__()
    M_all = kb.sb("M_all", [128, NTILE, NE], BF16)
    i8f_all = kb.sb("i8f_all", [128, NTILE, 8], F32)

    phC = kb.phase()
    phC.__enter__()
    w_out_bf = kb.sb("w_out_bf", [128, 8, D], BF16)
    wr_bf = kb.sb("wr_bf", [128, 8, NE], BF16)
    wsgu_bf = kb.sb("wsgu_bf", [128, 8, 512], BF16)
    wsd_bf = kb.sb("wsd_bf", [128, 2, D], BF16)
    for kc in range(8):
        rs = slice(kc * 128, (kc + 1) * 128)
        kb.dma('pool', w_out_bf[:, kc, :], w_out_d[rs, :], writes=[w_out_bf], sembuf=w_out_bf)
        kb.dma('pool', wr_bf[:, kc, :], w_r_d[rs, :], writes=[wr_bf], sembuf=wr_bf)
        kb.dma('pool', wsgu_bf[:, kc, 0:256], wsg_d[rs, :], writes=[wsgu_bf], sembuf=wsgu_bf)
        kb.dma('pool', wsgu_bf[:, kc, 256:512], wsu_d[rs, :], writes=[wsgu_bf], sembuf=wsgu_bf)
    for hc in range(2):
        kb.dma('pool', wsd_bf[:, hc, :], wsd_d[hc * 128:(hc + 1) * 128, :], writes=[wsd_bf], sembuf=wsd_bf)
    gmix = kb.sb("gmix", [128, 8], F32)
    kb.dma('sp', gmix[:], gmix_in[:], writes=[gmix], sembuf=gmix)
    rbias_b = kb.sb("rbias_b", [128, NE], F32)
    kb.dma('sp', rbias_b[:], bcast_rows(rbias_d[0:1, :]), writes=[rbias_b], sembuf=rbias_b)
    ym = [kb.sb(f"ym{i}", [128, D], F32) for i in range(2)]
    odt = [kb.sb(f"odt{i}", [128, 3, 520], F32) for i in range(2)]
    xq = [kb.sb(f"xq{i}", [128, D], F32) for i in range(3)]
    osum = kb.sb("osum", [128, 520], F32)
    rcd = kb.sb("rcd", [128, 8], F32)
    junkc = kb.sb("junkc", [128, D], BF16)
    junkc2 = kb.sb("junkc2", [128, D], BF16)
    ss2 = kb.sb("ss2", [128, 2], F32)
    sd2 = kb.sb("sd2", [128, 2], F32)
    rs2 = kb.sb("rs2", [128, 2], F32)
    ymn = kb.sb("ymn", [128, D], BF16)
    ymT = kb.sb("ymT", [128, 8, 128], BF16)
    tmpc = kb.sb("tmpc", [128, 512], F32)
    tmpc2 = kb.sb("tmpc2", [128, 512], F32)
    ss1 = kb.sb("ss1", [128, 1], F32)
    sd1 = kb.sb("sd1", [128, 1], F32)
    rs1 = kb.sb("rs1", [128, 1], F32)
    h2f = kb.sb("h2f", [128, D], F32)
    h2tok = [kb.sb(f"h2tok{i}", [128, D], BF16) for i in range(2)]
    h2Ts = [kb.sb(f"h2T{i}", [128, 8, 128], BF16) for i in range(2)]
    scores = kb.sb("scores", [128, NE], F32)
    sel = kb.sb("sel", [128, NE], F32)
    msel = kb.sb("msel", [128, NE], F32)
    wd_ = kb.sb("wd_", [128, NE], F32)
    m8 = kb.sb("m8", [128, 8, 8], F32)
    grp = kb.sb("grp", [128, 8], F32)
    g8 = kb.sb("g8", [128, 8], F32)
    gm = kb.sb("gm", [128, 8], F32)
    pen = kb.sb("pen", [128, 8], F32)
    t8 = kb.sb("t8", [128, 8], F32)
    w8 = kb.sb("w8", [128, 8], F32)
    i8u = kb.sb("i8u", [128, 8], U32)
    wsum = kb.sb("wsum", [128, 1], F32)
    rws = kb.sb("rws", [128, 1], F32)
    sg = kb.sb("sg", [128, 256], F32)
    actT = kb.sb("actT", [128, 256], BF16)
    kb.op('dve', lambda e: e.memset(cnt_b[:], 0.0), writes=[cnt_b])
    def S1a(tg):
        s = tg // NT
        t = tg % NT
        rows = slice(t * 128, (t + 1) * 128)
        grow = slice(tg * 128, (tg + 1) * 128)
        i2 = tg % 2
        h2T_ = h2Ts[i2]
        ymt, odd, xt = ym[i2], odt[i2], xq[tg % 3]
        kb.dma('sp', ymt[:, 0:512], YF[s][rows, :], reads=[YF[s]], writes=[ymt], sembuf=ymt)
        kb.dma('sp', odd[:], OD[s][:, rows, :, :].rearrange("d t h e -> t d (h e)"), reads=[OD[s]], writes=[odd],
               sembuf=odd)
        kb.dma('sp', xt[:], x_in[s, rows, :], writes=[xt], sembuf=xt)
        kb.op('dve', lambda e, odd=odd: e.tensor_tensor(out=osum[:], in0=odd[:, 0, :], in1=odd[:, 1, :], op=ALU.add),
              reads=[odd], writes=[osum])
        kb.op('dve', lambda e, odd=odd: e.tensor_tensor(out=osum[:], in0=osum[:], in1=odd[:, 2, :], op=ALU.add),
              reads=[odd, osum], writes=[osum])
        osv = osum[:].rearrange("p (h e) -> p h e", e=65)
        kb.op('dve', lambda e, osv=osv: e.reciprocal(out=rcd[:], in_=osv[:, :, 64]), reads=[osum], writes=[rcd])
        kb.op('dve', lambda e, osv=osv, ymt=ymt: e.tensor_tensor(
            out=ymt[:, 512:1024].rearrange("p (h e) -> p h e", e=64), in0=osv[:, :, 0:64],
            in1=rcd[:].unsqueeze(2).to_broadcast([128, 8, 64]), op=ALU.mult), reads=[osum, rcd], writes=[ymt])
        for hf in range(2):
            kb.op('act', lambda e, hf=hf, ymt=ymt: e.activation(
                out=junkc[:, 0:512], in_=ymt[:, hf * 512:(hf + 1) * 512], func=AF.Square,
                accum_out=ss2[:, hf:hf + 1]), reads=[ymt], writes=[junkc, ss2])
        kb.op('act', lambda e: e.activation(out=sd2[:], in_=ss2[:], func=AF.Sqrt, scale=1.0 / 512, bias=epsc[:]),
              reads=[ss2, epsc], writes=[sd2])
        kb.op('dve', lambda e: e.reciprocal(out=rs2[:], in_=sd2[:]), reads=[sd2], writes=[rs2])
        for hf in range(2):
            kb.op('act', lambda e, hf=hf, ymt=ymt: e.activation(
                out=ymn[:, hf * 512:(hf + 1) * 512], in_=ymt[:, hf * 512:(hf + 1) * 512], func=AF.Identity,
                scale=rs2[:, hf:hf + 1]), reads=[ymt, rs2], writes=[ymn])
        tp = psum[0]
        tpv = tp[:].bitcast(BF16)
        for kc in range(8):
            kb.op('pe', lambda e, kc=kc, tpv=tpv: e.transpose(
                out=tpv[:, kc * 128:(kc + 1) * 128], in_=ymn[:, kc * 128:(kc + 1) * 128], identity=ident[:]),
                reads=[ymn, ident], writes=[tp])
        kb.op('dve', lambda e, tpv=tpv: e.tensor_tensor(
            out=ymT[:], in0=tpv.rearrange("p (k t) -> p k t", t=128),
            in1=gmix[:].unsqueeze(2).to_broadcast([128, 8, 128]), op=ALU.mult), reads=[tp, gmix], writes=[ymT])
        for hf in range(2):
            po_ = psum[4 + hf]
            for kc in range(8):
                kb.op('pe', lambda e, kc=kc, hf=hf, po_=po_: e.matmul(
                    po_[:, :], lhsT=ymT[:, kc, :], rhs=w_out_bf[:, kc, hf * 512:(hf + 1) * 512],
                    start=(kc == 0), stop=(kc == 7)), reads=[ymT, w_out_bf], writes=[po_])
            g1 = gate_b[(2, s)]
            kb.op('dve', lambda e, hf=hf, po_=po_, g1=g1: e.tensor_tensor(
                out=tmpc[:], in0=po_[:, :], in1=g1[:, hf * 512:(hf + 1) * 512], op=ALU.mult),
                reads=[po_, g1], writes=[tmpc])
            kb.op('dve', lambda e, hf=hf, xt=xt: e.tensor_tensor(
                out=xt[:, hf * 512:(hf + 1) * 512], in0=xt[:, hf * 512:(hf + 1) * 512], in1=tmpc[:], op=ALU.add),
                reads=[xt, tmpc], writes=[xt])

    def S1b(tg):
        s = tg // NT
        grow = slice(tg * 128, (tg + 1) * 128)
        i2 = tg % 2
        xt = xq[tg % 3]
        h2T_ = h2Ts[i2]
        kb.op('act', lambda e, xt=xt: e.activation(out=junkc2[:], in_=xt[:], func=AF.Square, accum_out=ss1[:]),
              reads=[xt], writes=[junkc2, ss1])
        kb.op('act', lambda e: e.activation(out=sd1[:], in_=ss1[:], func=AF.Sqrt, scale=1.0 / D, bias=epsc[:]),
              reads=[ss1, epsc], writes=[sd1])
        kb.op('dve', lambda e: e.reciprocal(out=rs1[:], in_=sd1[:]), reads=[sd1], writes=[rs1])
        sc2, sh2 = gate_b[(4, s)], gate_b[(3, s)]
        kb.op('dve', lambda e, xt=xt, sc2=sc2: e.scalar_tensor_tensor(
            out=h2f[:], in0=xt[:], scalar=rs1[:], in1=sc2[:], op0=ALU.mult, op1=ALU.mult),
            reads=[xt, rs1, sc2], writes=[h2f])
        h2t = h2tok[i2]
        kb.op('dve', lambda e, h2t=h2t, sh2=sh2: e.tensor_tensor(out=h2t[:], in0=h2f[:], in1=sh2[:], op=ALU.add),
              reads=[h2f, sh2], writes=[h2t])
        kb.dma('pool', H2[grow, :], h2t[:], reads=[h2t], writes=[H2], sembuf=h2t)
        tp2 = psum[7]
        tp2v = tp2[:].bitcast(BF16)
        for kc in range(8):
            kb.op('pe', lambda e, kc=kc, tp2v=tp2v, h2t=h2t: e.transpose(
                out=tp2v[:, kc * 128:(kc + 1) * 128], in_=h2t[:, kc * 128:(kc + 1) * 128], identity=ident[:]),
                reads=[h2t, ident], writes=[tp2])
        kb.op('act', lambda e, tp2v=tp2v: e.activation(out=h2T_[:].rearrange("p k t -> p (k t)"), in_=tp2v,
                                                       func=AF.Copy), reads=[tp2], writes=[h2T_])

    def S2(tg):
        s = tg // NT
        grow = slice(tg * 128, (tg + 1) * 128)
        i2 = tg % 2
        xt = xq[tg % 3]
        h2T_ = h2Ts[i2]
        pr = psum[3]
        for kc in range(8):
            kb.op('pe', lambda e, kc=kc, pr=pr: e.matmul(pr[:, 0:NE], lhsT=h2T_[:, kc, :], rhs=wr_bf[:, kc, :],
                                                         start=(kc == 0), stop=(kc == 7)),
                  reads=[h2T_, wr_bf], writes=[pr])
        kb.op('act', lambda e, pr=pr: e.activation(out=scores[:], in_=pr[:, 0:NE], func=AF.Sigmoid),
              reads=[pr], writes=[scores])
        kb.op('dve', lambda e: e.tensor_tensor(out=sel[:], in0=scores[:], in1=rbias_b[:], op=ALU.add),
              reads=[scores, rbias_b], writes=[sel])
        for g in range(8):
            kb.op('dve', lambda e, g=g: e.max(out=m8[:, g, :], in_=sel[:, g * 32:(g + 1) * 32]),
                  reads=[sel], writes=[m8])
        kb.op('dve', lambda e: e.tensor_tensor(out=grp[:], in0=m8[:, :, 0], in1=m8[:, :, 1], op=ALU.add),
              reads=[m8], writes=[grp])
        kb.op('dve', lambda e: e.max(out=g8[:], in_=grp[:]), reads=[grp], writes=[g8])
        kb.op('dve', lambda e: e.tensor_scalar(out=gm[:], in0=grp[:], scalar1=g8[:, 3:4], scalar2=None,
                                               op0=ALU.is_ge), reads=[grp, g8], writes=[gm])
        kb.op('dve', lambda e: e.tensor_scalar(out=pen[:], in0=gm[:], scalar1=BIG, scalar2=-BIG, op0=ALU.mult,
                                               op1=ALU.add), reads=[gm], writes=[pen])
        kb.op('dve', lambda e: e.tensor_tensor(
            out=msel[:].rearrange("p (g e) -> p g e", e=32), in0=sel[:].rearrange("p (g e) -> p g e", e=32),
            in1=gm[:].unsqueeze(2).to_broadcast([128, 8, 32]), op=ALU.mult), reads=[sel, gm], writes=[msel])
        kb.op('dve', lambda e: e.tensor_tensor(
            out=msel[:].rearrange("p (g e) -> p g e", e=32), in0=msel[:].rearrange("p (g e) -> p g e", e=32),
            in1=pen[:].unsqueeze(2).to_broadcast([128, 8, 32]), op=ALU.add), reads=[msel, pen], writes=[msel])
        kb.op('dve', lambda e: e.max(out=t8[:], in_=msel[:]), reads=[msel], writes=[t8])
        kb.op('dve', lambda e, tg=tg: e.tensor_scalar(out=M_all[:, tg, :], in0=msel[:], scalar1=t8[:, 7:8],
                                                      scalar2=None, op0=ALU.is_ge),
              reads=[msel, t8], writes=[M_all])
        kb.op('dve', lambda e, tg=tg: e.tensor_tensor(out=wd_[:], in0=scores[:], in1=M_all[:, tg, :], op=ALU.mult),
              reads=[scores, M_all], writes=[wd_])
        kb.op('dve', lambda e: e.max(out=w8[:], in_=wd_[:]), reads=[wd_], writes=[w8])
        kb.op('dve', lambda e: e.max_index(out=i8u[:], in_max=w8[:], in_values=wd_[:]), reads=[w8, wd_],
              writes=[i8u])
        kb.op('dve', lambda e: e.tensor_reduce(out=wsum[:], in_=w8[:], axis=mybir.AxisListType.X, op=ALU.add),
              reads=[w8], writes=[wsum])
        kb.op('dve', lambda e: e.reciprocal(out=rws[:], in_=wsum[:]), reads=[wsum], writes=[rws])
        kb.op('dve', lambda e, tg=tg: e.tensor_scalar(out=wts_all[:, tg, :], in0=w8[:], scalar1=rws[:],
                                                      scalar2=2.5, op0=ALU.mult, op1=ALU.mult),
              reads=[w8, rws], writes=[wts_all])
        kb.op('dve', lambda e, tg=tg: e.tensor_copy(out=i8f_all[:, tg, :], in_=i8u[:]), reads=[i8u],
              writes=[i8f_all])
        kb.op('pe', lambda e, tg=tg, pr=pr: e.matmul(pr[:, 256:512], lhsT=ones_bf[:, :], rhs=M_all[:, tg, :],
                                                     start=True, stop=True),
              reads=[ones_bf, M_all], writes=[pr])
        kb.op('dve', lambda e, pr=pr: e.tensor_tensor(out=cnt_b[:], in0=cnt_b[:], in1=pr[:, 256:512], op=ALU.add),
              reads=[pr, cnt_b], writes=[cnt_b])
        pg = psum[6]
        for gi in range(4):
            for kc in range(8):
                kb.op('pe', lambda e, gi=gi, kc=kc, pg=pg: e.matmul(
                    pg[:, gi * 128:(gi + 1) * 128], lhsT=wsgu_bf[:, kc, gi * 128:(gi + 1) * 128], rhs=h2T_[:, kc, :],
                    start=(kc == 0), stop=(kc == 7)), reads=[wsgu_bf, h2T_], writes=[pg])
        kb.op('act', lambda e, pg=pg: e.activation(out=sg[:], in_=pg[:, 0:256], func=AF.Silu), reads=[pg],
              writes=[sg])
        kb.op('dve', lambda e, pg=pg: e.tensor_tensor(out=actT[:], in0=sg[:], in1=pg[:, 256:512], op=ALU.mult),
              reads=[sg, pg], writes=[actT])
        for hf in range(2):
            po_ = psum[1 + hf]
            for hc in range(2):
                kb.op('pe', lambda e, hc=hc, hf=hf, po_=po_: e.matmul(
                    po_[:, :], lhsT=actT[:, hc * 128:(hc + 1) * 128], rhs=wsd_bf[:, hc, hf * 512:(hf + 1) * 512],
                    start=(hc == 0), stop=(hc == 1)), reads=[actT, wsd_bf], writes=[po_])
            g2 = gate_b[(5, s)]
            kb.op('dve', lambda e, hf=hf, po_=po_, g2=g2: e.tensor_tensor(
                out=tmpc2[:], in0=po_[:, :], in1=g2[:, hf * 512:(hf + 1) * 512], op=ALU.mult),
                reads=[po_, g2], writes=[tmpc2])
            kb.op('dve', lambda e, hf=hf, xt=xt: e.tensor_tensor(
                out=xt[:, hf * 512:(hf + 1) * 512], in0=xt[:, hf * 512:(hf + 1) * 512], in1=tmpc2[:], op=ALU.add),
                reads=[xt, tmpc2], writes=[xt])
        kb.dma('pool', X2[grow, :], xt[:], reads=[xt], writes=[X2], sembuf=xt)
    S1a(0)
    S1a(1)
    S1b(0)
    for tg in range(NTILE):
        A = record(S1a, tg + 2) if tg + 2 < NTILE else []
        B = record(S1b, tg + 1) if tg + 1 < NTILE else []
        C = record(S2, tg)
        emit_merged([A, B, C])
        precast(2)
    precast(1000)
    phC.__exit__(None, None, None)

    phD = kb.phase()
    phD.__enter__()
    lst = kb.sb("lst", [128, 128], BF16)
    kb.dma('sp', lst[:], lst_d[:], writes=[lst], sembuf=lst)
    iota = kb.sb("iota", [128, NE], F32)
    kb.dma('sp', iota[:], iota_d[:], writes=[iota], sembuf=iota)
    bvals = kb.sb("bvals", [128, 6], F32)
    kb.dma('sp', bvals[:], bvals_d[:], writes=[bvals], sembuf=bvals)
    cnt_i = kb.sb("cnt_i", [128, NE], I32)
    nb_i = kb.sb("nb_i", [128, NE], I32)
    nb_f = kb.sb("nb_f", [128, NE], F32)
    Cc = kb.sb("Cc", [128, NE], F32)
    ones256 = kb.sb("ones256", [128, NE], F32)
    base = kb.sb("base", [128, NE], F32)
    junkd = kb.sb("junkd", [128, NE], F32)
    be_f = kb.sb("be_f", [128, 6], F32)
    be_i = kb.sb("be_i", [128, 6], I32)
    Dm = kb.sb("Dm", [128, NE], F32)
    dest_f = kb.sb("dest_f", [128, 8], F32)
    h2s = [kb.sb(f"h2s{i}", [128, D], BF16) for i in range(3)]
    kb.op('dve', lambda e: e.memset(ones256[:], 1.0), writes=[ones256])
    kb.op('dve', lambda e: e.tensor_copy(out=cnt_i[:], in_=cnt_b[:]), reads=[cnt_b], writes=[cnt_i])
    kb.op('dve', lambda e: e.tensor_scalar(out=cnt_i[:], in0=cnt_i[:], scalar1=BLK - 1, scalar2=None, op0=ALU.add),
          reads=[cnt_i], writes=[cnt_i])
    kb.op('dve', lambda e: e.tensor_scalar(out=nb_i[:], in0=cnt_i[:], scalar1=8, scalar2=None,
                                           op0=ALU.arith_shift_right), reads=[cnt_i], writes=[nb_i])
    kb.op('dve', lambda e: e.tensor_copy(out=nb_f[:], in_=nb_i[:]), reads=[nb_i], writes=[nb_f])
    kb.op('dve', lambda e: e.tensor_tensor_scan(out=Cc[:], data0=ones256[:], data1=nb_f[:], initial=0.0,
                                                op0=ALU.mult, op1=ALU.add), reads=[ones256, nb_f], writes=[Cc])
    kb.op('dve', lambda e: e.tensor_tensor(out=base[:], in0=Cc[:], in1=nb_f[:], op=ALU.subtract),
          reads=[Cc, nb_f], writes=[base])
    kb.op('dve', lambda e: e.tensor_scalar(out=base[:], in0=base[:], scalar1=float(BLK), scalar2=None, op0=ALU.mult),
          reads=[base], writes=[base])
    for tg in range(NTILE):
        pp = psum[tg % 2]
        pc = psum[2 + tg % 2]
        kb.op('pe', lambda e, tg=tg, pp=pp: e.matmul(pp[:, 0:NE], lhsT=lst[:, :], rhs=M_all[:, tg, :],
                                                     start=True, stop=True), reads=[lst, M_all], writes=[pp])
        kb.op('pe', lambda e, tg=tg, pc=pc: e.matmul(pc[:, 0:NE], lhsT=ones_bf[:, :], rhs=M_all[:, tg, :],
                                                     start=True, stop=True), reads=[ones_bf, M_all], writes=[pc])
        kb.op('dve', lambda e, pp=pp: e.tensor_tensor(out=Dm[:], in0=pp[:, 0:NE], in1=base[:], op=ALU.add),
              reads=[pp, base], writes=[Dm])
        kb.op('dve', lambda e, pc=pc: e.tensor_tensor(out=base[:], in0=pc[:, 0:NE], in1=base[:], op=ALU.add),
              reads=[pc, base], writes=[base])
        for k in range(8):
            kb.op('dve', lambda e, k=k, tg=tg: e.scalar_tensor_tensor(
                out=junkd[:], in0=iota[:], scalar=i8f_all[:, tg, k:k + 1], in1=Dm[:], op0=ALU.is_equal,
                op1=ALU.mult, accum_out=dest_f[:, k:k + 1]), reads=[iota, i8f_all, Dm], writes=[junkd, dest_f])
        kb.op('dve', lambda e, tg=tg: e.tensor_copy(out=dest_all[:, tg, :], in_=dest_f[:]), reads=[dest_f],
              writes=[dest_all])
        hs_ = h2s[tg % 3]
        kb.dma('sp', hs_[:], H2[tg * 128:(tg + 1) * 128, :], reads=[H2], writes=[hs_], sembuf=hs_)
        for k in range(8):
            kb.dma('pool', None, None, reads=[hs_, dest_all], writes=[HS], sembuf=hs_,
                   fn=lambda e, k=k, tg=tg, hs_=hs_: e.indirect_dma_start(
                       out=HS[:, :], out_offset=bass.IndirectOffsetOnAxis(ap=dest_all[:, tg, k:k + 1], axis=0),
                       in_=hs_[:, :], in_offset=None))
    be_bc = kb.sb("be_bc", [128, NBLK], F32)
    for bb in range(NBLK):
        kb.op('dve', lambda e, bb=bb: e.tensor_scalar(out=junkd[:], in0=Cc[:], scalar1=float(bb), scalar2=0.0,
                                                      op0=ALU.is_le, op1=ALU.add, accum_out=be_bc[:, bb:bb + 1]),
              reads=[Cc], writes=[junkd, be_bc])
    same = kb.sb("same", [128, NBLK], F32)
    bex = kb.sb("bex", [128, NBLK], F32)
    for lag, dst in ((3, idx_all), (4, idx4_all)):
        kb.op('dve', lambda e, lag=lag: e.memset(same[:, 0:lag], 0.0), writes=[same])
        kb.op('dve', lambda e, lag=lag: e.tensor_tensor(out=same[:, lag:NBLK], in0=be_bc[:, lag:NBLK],
                                                        in1=be_bc[:, 0:NBLK - lag], op=ALU.is_equal),
              reads=[be_bc], writes=[same])
        kb.op('dve', lambda e: e.scalar_tensor_tensor(out=bex[:], in0=same[:], scalar=float(2 * NE), in1=be_bc[:],
                                                      op0=ALU.mult, op1=ALU.add), reads=[same, be_bc], writes=[bex])
        kb.op('dve', lambda e: e.tensor_scalar(out=bex[:], in0=bex[:], scalar1=128.0, scalar2=bvals[:, 0:1],
                                               op0=ALU.mult, op1=ALU.add), reads=[bex, bvals], writes=[bex])
        kb.op('dve', lambda e, dst=dst: e.tensor_copy(out=dst[:], in_=bex[:]), reads=[bex], writes=[dst])
    phD.__exit__(None, None, None)
    phCD.__exit__(None, None, None)

    phE = kb.phase()
    phE.__enter__()
    wGU = [kb.sb(f"wGU{i}", [128, 4096], BF16) for i in range(3)]
    wDn = [kb.sb(f"wDn{i}", [128, 2048], BF16) for i in range(4)]
    xb = [kb.sb(f"xb{i}", [128, D], BF16) for i in range(6)]
    xbT = [kb.sb(f"xbT{i}", [128, 8, 256], BF16) for i in range(2)]
    sge = [kb.sb(f"sge{i}", [128, 256], F32) for i in range(4)]
    actk = [kb.sb(f"actk{i}", [128, 256], BF16) for i in range(4)]
    acte = [kb.sb(f"acte{i}", [128, 2, 2, 128], BF16) for i in range(2)]
    yb = [kb.sb(f"yb{i}", [128, 2, D], BF16) for i in range(2)]
    bnd_reg = nc.gpsimd.alloc_register("bnd_reg")
    nc.gpsimd.reg_mov(bnd_reg, NE * 128 - 1)

    def gather(dst, src_d, idxt, sl):
        kb.dma('pool', None, None, reads=[idxt, src_d], writes=[dst], sembuf=dst,
               fn=lambda e: e.indirect_dma_start(
                   out=dst[:, :], out_offset=None, in_=src_d[:, :],
                   in_offset=bass.IndirectOffsetOnAxis(ap=idxt[:, sl:sl + 1], axis=0),
                   bounds_check=bnd_reg, oob_is_err=False))

    def issue_w(sl):
        gather(wGU[sl % 3], WGUb, idx_all, sl)
        gather(wDn[sl % 4], WDb, idx4_all, sl)

    def issue_x(sl):
        for sb in range(2):
            xt_ = xb[(2 * sl + sb) % 6]
            r0 = sl * BLK + sb * 128
            kb.dma('sp', xt_[:], HS[r0:r0 + 128, :], reads=[HS], writes=[xt_], sembuf=xt_)

    def e_T(sl):
        xT = xbT[sl % 2]
        for sb in range(2):
            xt_ = xb[(2 * sl + sb) % 6]
            tp = psum[sb]
            tpv = tp[:].bitcast(BF16)
            for kc in range(8):
                kb.op('pe', lambda e, kc=kc: e.transpose(
                    out=tpv[:, kc * 128:(kc + 1) * 128], in_=xt_[:, kc * 128:(kc + 1) * 128], identity=ident[:]),
                    reads=[xt_, ident], writes=[tp])
            src = tpv.rearrange("p (k t) -> p k t", t=128)
            if sb == 0:
                kb.op('dve', lambda e: e.tensor_copy(out=xT[:, :, 0:128], in_=src), reads=[tp], writes=[xT])
            else:
                kb.op('act', lambda e: e.activation(out=xT[:, :, 128:256], in_=src, func=AF.Copy),
                      reads=[tp], writes=[xT])

    def e_GU(sl):
        xT = xbT[sl % 2]
        for sb in range(2):
            pg = psum[2 + sb]
            for kc in range(8):
                wt_ = wGU[sl % 3]
                kb.op('pe', lambda e, kc=kc, wt_=wt_: e.matmul(
                    pg[:, :], lhsT=xT[:, kc, sb * 128:(sb + 1) * 128], rhs=wt_[:, kc * 512:(kc + 1) * 512],
                    start=(kc == 0), stop=(kc == 7)), reads=[wt_, xT], writes=[pg])
            sg_ = sge[(2 * sl + sb) % 4]
            ak_ = actk[(2 * sl + sb) % 4]
            kb.op('act', lambda e: e.activation(out=sg_[:], in_=pg[:, 0:256], func=AF.Silu), reads=[pg],
                  writes=[sg_])
            kb.op('dve', lambda e: e.tensor_tensor(out=ak_[:], in0=sg_[:], in1=pg[:, 256:512], op=ALU.mult),
                  reads=[sg_, pg], writes=[ak_])

    def e_AT(sl):
        ta = psum[4]
        tav = ta[:].bitcast(BF16)
        for sb in range(2):
            ak_ = actk[(2 * sl + sb) % 4]
            for hc in range(2):
                c0 = (sb * 2 + hc) * 128
                kb.op('pe', lambda e, hc=hc, c0=c0: e.transpose(
                    out=tav[:, c0:c0 + 128], in_=ak_[:, hc * 128:(hc + 1) * 128], identity=ident[:]),
                    reads=[ak_, ident], writes=[ta])
        ac_ = acte[sl % 2]
        kb.op('dve', lambda e: e.tensor_copy(out=ac_[:].rearrange("p a b t -> p (a b t)"), in_=tav[:, 0:512]),
              reads=[ta], writes=[ac_])

    pocnt = [0]

    def e_DN(sl):
        wd = wDn[sl % 4][:].rearrange("p (h n) -> p h n", n=D)
        ac_ = acte[sl % 2]
        yb_ = yb[sl % 2]
        for sb in range(2):
            for hf in range(2):
                po_ = psum[5 + pocnt[0] % 3]
                pocnt[0] += 1
                for hc in range(2):
                    kb.op('pe', lambda e, hc=hc: e.matmul(
                        po_[:, :], lhsT=ac_[:, sb, hc, :], rhs=wd[:, hc, hf * 512:(hf + 1) * 512],
                        start=(hc == 0), stop=(hc == 1)), reads=[ac_, wDn[sl % 4]], writes=[po_])
                if hf == 0:
                    kb.op('dve', lambda e: e.tensor_copy(out=yb_[:, sb, 0:512], in_=po_[:, :]),
                          reads=[po_], writes=[yb_])
                else:
                    kb.op('act', lambda e: e.activation(out=yb_[:, sb, 512:1024], in_=po_[:, :], func=AF.Copy),
                          reads=[po_], writes=[yb_])
        kb.dma('sp', YS[sl * BLK:(sl + 1) * BLK, :].rearrange("(sb p) n -> p sb n", p=128), yb_[:],
               reads=[yb_], writes=[YS], sembuf=yb_)

    issue_w(0)
    issue_w(1)
    issue_x(0)
    issue_x(1)
    e_T(0)
    for sl in range(NBLK):
        if sl + 2 < NBLK:
            issue_w(sl + 2)
            issue_x(sl + 2)
        if sl + 1 < NBLK:
            e_T(sl + 1)
        if sl > 0:
            e_AT(sl - 1)
        e_GU(sl)
        if sl > 0:
            e_DN(sl - 1)
    e_AT(NBLK - 1)
    e_DN(NBLK - 1)
    phE.__exit__(None, None, None)

    phF = kb.phase()
    phF.__enter__()
    gfin_b = kb.sb("gfin_b", [128, D], F32)
    kb.dma('sp', gfin_b[:], bcast_rows(gfin_d[0:1, :]), writes=[gfin_b], sembuf=gfin_b)
    x2t = [kb.sb(f"x2t{i}", [128, D], F32) for i in range(2)]
    yk = [kb.sb(f"yk{i}", [128, D], BF16) for i in range(4)]
    acc = kb.sb("acc", [128, D], F32)
    tmpf = kb.sb("tmpf", [128, D], F32)
    junkf = kb.sb("junkf", [128, D], BF16)
    ssf = kb.sb("ssf", [128, 1], F32)
    sdf = kb.sb("sdf", [128, 1], F32)
    rsf = kb.sb("rsf", [128, 1], F32)
    ot = [kb.sb(f"ot{i}", [128, D], F32) for i in range(2)]
    gi_ = 0
    for tg in range(NTILE):
        s = tg // NT
        t = tg % NT
        xt = x2t[tg % 2]
        kb.dma('sp', xt[:], X2[tg * 128:(tg + 1) * 128, :], reads=[X2], writes=[xt], sembuf=xt)
        for k in range(8):
            yk_ = yk[gi_ % 4]
            gi_ += 1
            kb.dma('pool', None, None, reads=[YS, dest_all], writes=[yk_], sembuf=yk_,
                   fn=lambda e, k=k, tg=tg, yk_=yk_: e.indirect_dma_start(
                       out=yk_[:, :], out_offset=None, in_=YS[:, :],
                       in_offset=bass.IndirectOffsetOnAxis(ap=dest_all[:, tg, k:k + 1], axis=0)))
            if k == 0:
                kb.op('dve', lambda e, k=k, tg=tg, yk_=yk_: e.tensor_scalar(
                    out=acc[:], in0=yk_[:], scalar1=wts_all[:, tg, k:k + 1], scalar2=None, op0=ALU.mult),
                    reads=[yk_, wts_all], writes=[acc])
            else:
                kb.op('dve', lambda e, k=k, tg=tg, yk_=yk_: e.scalar_tensor_tensor(
                    out=acc[:], in0=yk_[:], scalar=wts_all[:, tg, k:k + 1], in1=acc[:], op0=ALU.mult, op1=ALU.add),
                    reads=[yk_, wts_all, acc], writes=[acc])
        g2 = gate_b[(5, s)]
        kb.op('dve', lambda e, g2=g2: e.tensor_tensor(out=tmpf[:], in0=acc[:], in1=g2[:], op=ALU.mult),
              reads=[acc, g2], writes=[tmpf])
        kb.op('dve', lambda e, xt=xt: e.tensor_tensor(out=xt[:], in0=xt[:], in1=tmpf[:], op=ALU.add),
              reads=[xt, tmpf], writes=[xt])
        kb.op('act', lambda e, xt=xt: e.activation(out=junkf[:], in_=xt[:], func=AF.Square, accum_out=ssf[:]),
              reads=[xt], writes=[junkf, ssf])
        kb.op('act', lambda e: e.activation(out=sdf[:], in_=ssf[:], func=AF.Sqrt, scale=1.0 / D, bias=epsc[:]),
              reads=[ssf, epsc], writes=[sdf])
        kb.op('dve', lambda e: e.reciprocal(out=rsf[:], in_=sdf[:]), reads=[sdf], writes=[rsf])
        o_ = ot[tg % 2]
        kb.op('dve', lambda e, xt=xt, o_=o_: e.scalar_tensor_tensor(
            out=o_[:], in0=xt[:], scalar=rsf[:], in1=gfin_b[:], op0=ALU.mult, op1=ALU.mult),
            reads=[xt, rsf, gfin_b], writes=[o_])
        kb.dma('sp', out_d[s, t * 128:(t + 1) * 128, :], o_[:], reads=[o_], writes=[out_d], sembuf=o_)
    phF.__exit__(None, None, None)
    allb = [out_d]
    for e in ('sp',):
        kb.wait_all(e, allb)
    return nc, kb, es


_CACHE = {}


def relayout_experts(inputs):
    g = inputs["w_exp_gate"][0].reshape(NE, 8, 128, 256).transpose(0, 2, 1, 3)
    u = inputs["w_exp_up"][0].reshape(NE, 8, 128, 256).transpose(0, 2, 1, 3)
    gu = np.concatenate([g, u], axis=3)
    wa = np.ascontiguousarray(gu[:, :, 0:4, :]).reshape(NE * 128, 2048)
    wb = np.ascontiguousarray(gu[:, :, 4:8, :]).reshape(NE * 128, 2048)
    wd = np.ascontiguousarray(inputs["w_exp_down"][0].reshape(NE, 2, 128, D).transpose(0, 2, 1, 3)).reshape(NE * 128, 2048)
    return {"_wa": wa, "_wb": wb, "_wd": wd}


def kernel(**inputs):
    inputs = {k: np.asarray(v) for k, v in inputs.items()}
    if 'nc' not in _CACHE:
        _CACHE['nc'] = build(False)
    nc = _CACHE['nc'][0]
    inputs.update(relayout_experts(inputs))
    in_maps = [make_inputs(c, **inputs) for c in range(8)]
    res = run_bass_kernel_spmd(nc, in_maps, core_ids=list(range(8)))
    out = np.concatenate([np.asarray(r["out"]) for r in res.results], axis=0)
    return out.astype(np.float32)


def _t5_bucket(dist):
    max_exact = 16
    dd = np.maximum(dist, 1).astype(np.float32)
    large = max_exact + (np.log(dd / max_exact) / np.log(2048 / max_exact) * (32 - max_exact)).astype(np.int32)
    large = np.minimum(large, 31)
    return np.where(dist < max_exact, dist, large).astype(np.int32)


def _consts():
    oh = np.zeros((32, 3, 384), np.float32)
    negm = np.full((8, 3, 384), NEG, np.float32)
    for di, (win, d) in enumerate(DIL):
        rel = np.arange(129)
        bk = _t5_bucket(rel * d)
        oh[bk, di, 127 + rel] = 1.0
        negm[:, di, 127:127 + 129] = 0.0
    J = np.zeros((128, 128), np.float32)
    J[np.arange(128), 127 - np.arange(128)] = 1.0
    tri = (np.arange(128)[None, :] >= np.arange(128)[:, None]).astype(ml_dtypes.bfloat16)
    lst = (np.arange(128)[:, None] < np.arange(128)[None, :]).astype(ml_dtypes.bfloat16)
    iota = np.tile(np.arange(NE, dtype=np.float32)[None, :], (128, 1))
    bvals = (np.arange(128)[:, None] + 128 * np.arange(6)[None, :]).astype(np.float32)
    return {"oh_in": oh, "negm_in": negm, "J_in": J, "tri_in": tri, "lst_in": lst, "iota_in": iota,
            "bvals_in": bvals}


CONSTS = _consts()


def make_inputs(core, x, c, w_in, b_forget, g_fox_out, g_dil_out, w_out, w_ada, b_ada, **kw):
    f = np.ascontiguousarray
    xs = f(x[NSEQ * core:NSEQ * (core + 1)])
    cs = c[NSEQ * core:NSEQ * (core + 1)]
    cT = f(cs.T.reshape(8, 128, NSEQ).transpose(1, 0, 2))
    gm = np.concatenate([g_fox_out[0], g_dil_out[0]])
    return {
        "x": xs, "cT": cT, "w_in": f(w_in[0]), "b_forget": f(b_forget[0].reshape(8, 1)),
        "g_mix": f(gm.reshape(8, 128).T), "w_out": f(w_out[0]), "w_ada": f(w_ada[0]),
        "b_ada": f(b_ada[0].reshape(1, -1)), "b_adaT": f(b_ada[0].reshape(48, 128).T),
        "ident_in": np.eye(128, dtype=ml_dtypes.bfloat16),
        "rel_bias": f(kw["rel_bias"]), **CONSTS,
        "w_router": f(kw["w_router"][0]), "router_bias": f(kw["router_bias"][0].reshape(1, -1)),
        "w_sh_gate": f(kw["w_sh_gate"][0]), "w_sh_up": f(kw["w_sh_up"][0]), "w_sh_down": f(kw["w_sh_down"][0]),
        "w_exp_a": kw["_wa"], "w_exp_b": kw["_wb"], "w_exp_d": kw["_wd"],
        "g_final": f(kw["g_final"].reshape(1, -1)),
    }
```
